# Optimizing a Trainium2 kernel written in Bass

```python
import math
import functools
import jax
import jax.numpy as jnp
from jax import lax
import numpy as np

D_MODEL = 1024
BATCH = 2
SEQ = 16384
DEPTH = 4

N_MIXERS = 4
CHUNK = 64
CONV_K = 5
PLE_DIM = 256

GDN_QK_HEADS = 8
GDN_V_HEADS = 16
GDN_HEAD_DIM = 128
GDN_KEY_WIDTH = GDN_QK_HEADS * GDN_HEAD_DIM
GDN_VAL_WIDTH = GDN_V_HEADS * GDN_HEAD_DIM
GDN_CONV_CH = 2 * GDN_KEY_WIDTH + GDN_VAL_WIDTH
GDN_IN = GDN_CONV_CH + GDN_VAL_WIDTH + 4 * GDN_V_HEADS

M2_D_INNER = 2 * D_MODEL
M2_HEAD_DIM = 64
M2_HEADS = M2_D_INNER // M2_HEAD_DIM
M2_GROUPS = 4
M2_HEADS_PER_GROUP = M2_HEADS // M2_GROUPS
M2_STATE = 128
M2_CONV_CH = M2_D_INNER + 2 * M2_GROUPS * M2_STATE
M2_IN = M2_D_INNER + M2_CONV_CH + 2 * M2_HEADS

HG_EXPAND = 128
HG_HEADS = D_MODEL // HG_EXPAND
HG_WIDTH = HG_HEADS * HG_EXPAND
HG_IN = 5 * HG_WIDTH

GLA_HEADS = 4
GLA_KEY_WIDTH = D_MODEL // 2
GLA_VAL_WIDTH = D_MODEL
GLA_K = GLA_KEY_WIDTH // GLA_HEADS
GLA_V = GLA_VAL_WIDTH // GLA_HEADS
GLA_GATE_RANK = 16
GLA_GATE_TEMP = 16.0
GLA_IN = 2 * GLA_KEY_WIDTH + 2 * GLA_VAL_WIDTH + 2 * GLA_GATE_RANK

MOE_GROUPS = 4
MOE_EXPERTS_PER_GROUP = 8
MOE_EXPERTS = MOE_GROUPS * MOE_EXPERTS_PER_GROUP
MOE_TOPK = 2
MOE_HIDDEN = 256

DEEPNORM_ALPHA = (2 * DEPTH) ** 0.25
DEEPNORM_BETA = (8 * DEPTH) ** -0.25
LN_EPS = 1e-5
NORM_EPS = 1e-6
F32 = jnp.float32

kernel_name = "hybrid_bidir_gdn_ssd_hgrn2_gla_hmoe"


def _n_uses(m):
    return (DEPTH - m + N_MIXERS - 1) // N_MIXERS


def _layernorm(t, g, b):
    tf = t.astype(F32)
    mu = jnp.mean(tf, axis=-1, keepdims=True)
    var = jnp.mean(jnp.square(tf - mu), axis=-1, keepdims=True)
    return ((tf - mu) * lax.rsqrt(var + LN_EPS) * g.astype(F32) + b.astype(F32)).astype(t.dtype)


def _rms(t):
    tf = t.astype(F32)
    return tf * lax.rsqrt(jnp.mean(tf * tf, axis=-1, keepdims=True) + NORM_EPS)


def _l2norm(t):
    tf = t.astype(F32)
    return tf * lax.rsqrt(jnp.sum(tf * tf, axis=-1, keepdims=True) + NORM_EPS)


def _dwconv(t, w):
    return lax.conv_general_dilated(
        t, w[:, None, :].astype(t.dtype), window_strides=(1,),
        padding=[(CONV_K // 2, CONV_K // 2)],
        dimension_numbers=("NWC", "WIO", "NWC"),
        feature_group_count=t.shape[-1])


def _rev(t):
    return jnp.flip(t, axis=1)


def _chunks(t):
    bsz, seq = t.shape[:2]
    t = t.reshape(bsz, seq // CHUNK, CHUNK, *t.shape[2:])
    return jnp.swapaxes(jnp.swapaxes(t, 0, 1), 2, 3)


def _unchunks(t):
    t = jnp.swapaxes(jnp.swapaxes(t, 2, 3), 0, 1)
    return t.reshape(t.shape[0], t.shape[1] * t.shape[2], *t.shape[3:])


def _gated_delta_rule(q, k, v, beta, g):
    q, k, v, beta, g = (_chunks(t.astype(F32)) for t in (q, k, v, beta, g))
    gc = jnp.cumsum(g, axis=-1)
    incl = jnp.tril(jnp.ones((CHUNK, CHUNK), dtype=bool))
    decay = jnp.exp(jnp.where(incl, gc[..., :, None] - gc[..., None, :], -jnp.inf))
    eye = jnp.eye(CHUNK, dtype=F32)
    kb = k * beta[..., None]
    lhs = eye + jnp.einsum("nbhik,nbhjk->nbhij", kb, k) * decay * (1.0 - eye)
    solve = functools.partial(lax.linalg.triangular_solve, left_side=True, lower=True,
                              unit_diagonal=True)
    u = solve(lhs, v * beta[..., None])
    w = solve(lhs, kb * jnp.exp(gc)[..., None])
    attn = jnp.einsum("nbhik,nbhjk->nbhij", q, k) * decay
    q_dec = q * jnp.exp(gc)[..., None]
    k_dec = k * jnp.exp(gc[..., -1:] - gc)[..., None]
    g_last = jnp.exp(gc[..., -1])

    def step(state, inp):
        attn_c, q_c, k_c, u_c, w_c, gl = inp
        v_new = u_c - jnp.einsum("bhck,bhkv->bhcv", w_c, state)
        o = jnp.einsum("bhck,bhkv->bhcv", q_c, state) + jnp.einsum("bhij,bhjv->bhiv", attn_c, v_new)
        state = state * gl[..., None, None] + jnp.einsum("bhck,bhcv->bhkv", k_c, v_new)
        return state, o

    s0 = jnp.zeros(q.shape[1:3] + (q.shape[-1], v.shape[-1]), F32)
    _, o = lax.scan(step, s0, (attn, q_dec, k_dec, u, w, g_last))
    return _unchunks(o)


def _ssd_grouped(c, b, u, g):
    c, b, u, g = (_chunks(t.astype(F32)) for t in (c, b, u, g))
    gc = jnp.cumsum(g, axis=3)
    tril = jnp.tril(jnp.ones((CHUNK, CHUNK), dtype=bool))[:, :, None]
    seg = gc[:, :, :, :, None, :] - gc[:, :, :, None, :, :]
    decay = jnp.exp(jnp.where(tril, seg, -jnp.inf))
    scores = jnp.einsum("nbgis,nbgjs->nbgij", c, b)[..., None] * decay
    y_intra = jnp.einsum("nbgijr,nbgjrp->nbgirp", scores, u)

    def step(state, inp):
        c_c, b_c, u_c, gc_c = inp
        y = jnp.exp(gc_c)[..., None] * jnp.einsum("bgis,bgrsp->bgirp", c_c, state)
        g_last = gc_c[:, :, -1]
        u_dec = u_c * jnp.exp(g_last[:, :, None] - gc_c)[..., None]
        state = state * jnp.exp(g_last)[..., None, None] + jnp.einsum("bgjs,bgjrp->bgrsp", b_c, u_dec)
        return state, y

    s0 = jnp.zeros((c.shape[1], c.shape[2], u.shape[4], c.shape[4], u.shape[5]), F32)
    _, y_inter = lax.scan(step, s0, (c, b, u, gc))
    return _unchunks(y_intra + y_inter)


def _gla_chunked(q, k, v, g):
    q, k, v, g = (_chunks(t.astype(F32)) for t in (q, k, v, g))
    gc = jnp.cumsum(g, axis=3)
    tril = jnp.tril(jnp.ones((CHUNK, CHUNK), dtype=bool))[:, :, None]

    def step(state, inp):
        q_c, k_c, v_c, gc_c = inp
        seg = gc_c[:, :, :, None, :] - gc_c[:, :, None, :, :]
        decay = jnp.exp(jnp.where(tril, seg, -jnp.inf))
        attn = jnp.einsum("bhijk,bhjk->bhij", decay * q_c[:, :, :, None, :], k_c)
        g_last = gc_c[:, :, -1]
        o = (jnp.einsum("bhij,bhjv->bhiv", attn, v_c)
             + jnp.einsum("bhik,bhkv->bhiv", q_c * jnp.exp(gc_c), state))
        state = (state * jnp.exp(g_last)[..., None]
                 + jnp.einsum("bhjk,bhjv->bhkv", k_c * jnp.exp(g_last[:, :, None] - gc_c), v_c))
        return state, o

    s0 = jnp.zeros(q.shape[1:3] + (q.shape[-1], v.shape[-1]), F32)
    _, o = lax.scan(step, s0, (q, k, v, gc))
    return _unchunks(o)


def _gdn_mixer(x, w_in, conv_w, a_log, dt_bias, norm_w, w_out):
    bsz, seq, _ = x.shape
    qkv, z, ab = jnp.split(x @ w_in, [GDN_CONV_CH, GDN_CONV_CH + GDN_VAL_WIDTH], axis=-1)
    qkv = jax.nn.silu(_dwconv(qkv, conv_w))
    q, k, v = jnp.split(qkv, [GDN_KEY_WIDTH, 2 * GDN_KEY_WIDTH], axis=-1)
    rep = GDN_V_HEADS // GDN_QK_HEADS
    qk_shape = (bsz, seq, GDN_QK_HEADS, GDN_HEAD_DIM)
    q = jnp.repeat(_l2norm(q.reshape(qk_shape)), rep, axis=2) * GDN_HEAD_DIM ** -0.5
    k = jnp.repeat(_l2norm(k.reshape(qk_shape)), rep, axis=2)
    v = v.reshape(bsz, seq, GDN_V_HEADS, GDN_HEAD_DIM).astype(F32)
    ab = ab.astype(F32).reshape(bsz, seq, 2, 2, GDN_V_HEADS)
    g = -jnp.exp(a_log.astype(F32)) * jax.nn.softplus(ab[:, :, 0] + dt_bias.astype(F32))
    beta = jax.nn.sigmoid(ab[:, :, 1])
    o = (_gated_delta_rule(q, k, v, beta[:, :, 0], g[:, :, 0])
         + _rev(_gated_delta_rule(_rev(q), _rev(k), _rev(v), _rev(beta[:, :, 1]), _rev(g[:, :, 1]))))
    o = _rms(o) * norm_w.astype(F32) * jax.nn.silu(
        z.astype(F32).reshape(bsz, seq, GDN_V_HEADS, GDN_HEAD_DIM))
    return o.reshape(bsz, seq, GDN_VAL_WIDTH).astype(x.dtype) @ w_out


def _mamba2_mixer(x, w_in, conv_w, conv_b, a_log, dt_bias, d_skip, norm_w, w_out):
    bsz, seq, _ = x.shape
    z, xbc, dt = jnp.split(x @ w_in, [M2_D_INNER, M2_D_INNER + M2_CONV_CH], axis=-1)
    xbc = jax.nn.silu(_dwconv(xbc, conv_w) + conv_b).astype(F32)
    xs, b_in, c_out = jnp.split(xbc, [M2_D_INNER, M2_D_INNER + M2_GROUPS * M2_STATE], axis=-1)
    xs = xs.reshape(bsz, seq, M2_GROUPS, M2_HEADS_PER_GROUP, M2_HEAD_DIM)
    b_in = b_in.reshape(bsz, seq, M2_GROUPS, M2_STATE)
    c_out = c_out.reshape(bsz, seq, M2_GROUPS, M2_STATE)
    hshape = (2, M2_GROUPS, M2_HEADS_PER_GROUP)
    dt = jax.nn.softplus(dt.astype(F32).reshape(bsz, seq, *hshape) + dt_bias.astype(F32).reshape(hshape))
    g = -jnp.exp(a_log.astype(F32)).reshape(hshape) * dt
    u_f = xs * dt[:, :, 0, :, :, None]
    u_b = xs * dt[:, :, 1, :, :, None]
    y = (_ssd_grouped(c_out, b_in, u_f, g[:, :, 0])
         + _rev(_ssd_grouped(_rev(c_out), _rev(b_in), _rev(u_b), _rev(g[:, :, 1]))))
    y = y + d_skip.astype(F32).reshape(M2_GROUPS, M2_HEADS_PER_GROUP, 1) * xs
    y = y.reshape(bsz, seq, M2_GROUPS, -1) * jax.nn.silu(z.astype(F32).reshape(bsz, seq, M2_GROUPS, -1))
    y = _rms(y) * norm_w.astype(F32).reshape(M2_GROUPS, -1)
    return y.reshape(bsz, seq, M2_D_INNER).astype(x.dtype) @ w_out


def _hgrn2_mixer(x, w_in, lb, norm_w, w_out):
    bsz, seq, _ = x.shape
    hs = (bsz, seq, HG_HEADS, HG_EXPAND)
    q, f_fwd, f_bwd, i_in, gate = jnp.split((x @ w_in).astype(F32), 5, axis=-1)
    q = jax.nn.silu(q).reshape(hs)
    i_in = i_in.reshape(hs)
    lb = lb.reshape(HG_HEADS, HG_EXPAND)

    def forget(f):
        f = f.reshape(hs)
        log_f = jnp.logaddexp(jnp.log(lb), jnp.log1p(-lb) + jax.nn.log_sigmoid(f))
        return (1.0 - lb) * jax.nn.sigmoid(-f), log_f

    k_f, g_f = forget(f_fwd)
    k_b, g_b = forget(f_bwd)
    o = (_gla_chunked(q, k_f, i_in, g_f)
         + _rev(_gla_chunked(_rev(q), _rev(k_b), _rev(i_in), _rev(g_b))))
    o = _rms(o) * norm_w.astype(F32) * jax.nn.silu(gate.reshape(hs))
    return o.reshape(bsz, seq, HG_WIDTH).astype(x.dtype) @ w_out


def _gla_mixer(x, w_in, w_gk, b_gk, norm_w, w_out):
    bsz, seq, _ = x.shape
    splits = [GLA_KEY_WIDTH, 2 * GLA_KEY_WIDTH, 2 * GLA_KEY_WIDTH + GLA_VAL_WIDTH,
              2 * GLA_KEY_WIDTH + 2 * GLA_VAL_WIDTH]
    q, k, v, gate, r = jnp.split((x @ w_in).astype(F32), splits, axis=-1)
    ks = (bsz, seq, GLA_HEADS, GLA_K)
    vs = (bsz, seq, GLA_HEADS, GLA_V)
    q = q.reshape(ks) * GLA_K ** -0.5
    k = k.reshape(ks)
    v = v.reshape(vs)
    r = r.reshape(bsz, seq, 2, GLA_GATE_RANK)
    gk = jax.nn.log_sigmoid(jnp.einsum("blzr,zrk->blzk", r, w_gk.astype(F32))
                            + b_gk.astype(F32)) / GLA_GATE_TEMP
    g_f = gk[:, :, 0].reshape(ks)
    g_b = gk[:, :, 1].reshape(ks)
    o = (_gla_chunked(q, k, v, g_f)
         + _rev(_gla_chunked(_rev(q), _rev(k), _rev(v), _rev(g_b))))
    o = _rms(o) * norm_w.astype(F32) * jax.nn.silu(gate.reshape(vs))
    return o.reshape(bsz, seq, GLA_VAL_WIDTH).astype(x.dtype) @ w_out


def _hier_moe(x, w_group, b_group, w_router, b_router, w_gate, w_up, w_down):
    bsz, seq, d = x.shape
    xt = x.reshape(bsz * seq, d)
    g_logits = (xt @ w_group).astype(F32) + b_group.astype(F32)
    g_onehot = jax.nn.one_hot(jnp.argmax(g_logits, axis=-1), MOE_GROUPS, dtype=F32)
    g_prob = jnp.sum(jax.nn.softmax(g_logits, axis=-1) * g_onehot, axis=-1)
    e_logits = ((xt @ w_router).astype(F32) + b_router.astype(F32)).reshape(
        -1, MOE_GROUPS, MOE_EXPERTS_PER_GROUP)
    e_logits = jnp.einsum("tge,tg->te", e_logits, g_onehot)
    top_val, top_idx = lax.top_k(e_logits, MOE_TOPK)
    top_w = jax.nn.softmax(top_val, axis=-1) * g_prob[:, None]
    within = jnp.einsum("tk,tke->te", top_w,
                        jax.nn.one_hot(top_idx, MOE_EXPERTS_PER_GROUP, dtype=F32))
    comb = (g_onehot[:, :, None] * within[:, None, :]).astype(x.dtype)
    out = jnp.zeros_like(xt)
    for grp in range(MOE_GROUPS):
        h = (jax.nn.silu(jnp.einsum("td,edf->tef", xt, w_gate[grp]))
             * jnp.einsum("td,edf->tef", xt, w_up[grp]))
        out = out + jnp.einsum("tef,efd->td", h * comb[:, grp, :, None], w_down[grp])
    return out.reshape(bsz, seq, d)


def setup_inputs(seed: int = 0) -> dict:
    key = jax.random.key(seed)
    ks = iter(jax.random.split(key, 48))

    def nrm(shape, scale):
        return jax.random.normal(next(ks), shape, F32) * scale

    def a_log(shape):
        return jnp.log(jax.random.uniform(next(ks), shape, F32, 1.0, 16.0))

    def dt_bias(shape):
        dt = jnp.exp(jax.random.uniform(next(ks), shape, F32, math.log(1e-3), math.log(1e-1)))
        return dt + jnp.log(-jnp.expm1(-dt))

    na, nb, nc, nd = (_n_uses(m) for m in range(N_MIXERS))
    D = D_MODEL
    sd = D ** -0.5
    beta = DEEPNORM_BETA
    return {
        "x": nrm((BATCH, SEQ, D), 1.0),
        "p": nrm((DEPTH, BATCH, SEQ, PLE_DIM), 1.0),
        "gdn_w_in": nrm((na, D, GDN_IN), sd),
        "gdn_conv_w": nrm((na, CONV_K, GDN_CONV_CH), CONV_K ** -0.5),
        "gdn_a_log": a_log((na, 2, GDN_V_HEADS)),
        "gdn_dt_bias": dt_bias((na, 2, GDN_V_HEADS)),
        "gdn_norm_w": 1.0 + nrm((na, GDN_HEAD_DIM), 0.02),
        "gdn_w_out": nrm((na, GDN_VAL_WIDTH, D), GDN_VAL_WIDTH ** -0.5 * beta),
        "m2_w_in": nrm((nb, D, M2_IN), sd),
        "m2_conv_w": nrm((nb, CONV_K, M2_CONV_CH), CONV_K ** -0.5),
        "m2_conv_b": nrm((nb, M2_CONV_CH), 0.02),
        "m2_a_log": a_log((nb, 2, M2_HEADS)),
        "m2_dt_bias": dt_bias((nb, 2, M2_HEADS)),
        "m2_d": 1.0 + nrm((nb, M2_HEADS), 0.1),
        "m2_norm_w": 1.0 + nrm((nb, M2_D_INNER), 0.02),
        "m2_w_out": nrm((nb, M2_D_INNER, D), M2_D_INNER ** -0.5 * beta),
        "hg_w_in": nrm((nc, D, HG_IN), sd),
        "hg_lb_logits": nrm((DEPTH, HG_WIDTH), 0.1),
        "hg_norm_w": 1.0 + nrm((nc, HG_EXPAND), 0.02),
        "hg_w_out": nrm((nc, HG_WIDTH, D), HG_WIDTH ** -0.5 * beta),
        "gla_w_in": nrm((nd, D, GLA_IN), sd),
        "gla_w_gk": nrm((nd, 2, GLA_GATE_RANK, GLA_KEY_WIDTH), GLA_GATE_RANK ** -0.5),
        "gla_b_gk": nrm((nd, 2, GLA_KEY_WIDTH), 0.1),
        "gla_norm_w": 1.0 + nrm((nd, GLA_V), 0.02),
        "gla_w_out": nrm((nd, GLA_VAL_WIDTH, D), GLA_VAL_WIDTH ** -0.5 * beta),
        "ln_g": 1.0 + nrm((DEPTH, 2, D), 0.02),
        "ln_b": nrm((DEPTH, 2, D), 0.02),
        "moe_w_group": nrm((DEPTH, D, MOE_GROUPS), sd),
        "moe_b_group": nrm((DEPTH, MOE_GROUPS), 0.01),
        "moe_w_router": nrm((DEPTH, D, MOE_EXPERTS), sd),
        "moe_b_router": nrm((DEPTH, MOE_EXPERTS), 0.01),
        "moe_w_gate": nrm((DEPTH, MOE_GROUPS, MOE_EXPERTS_PER_GROUP, D, MOE_HIDDEN), sd),
        "moe_w_up": nrm((DEPTH, MOE_GROUPS, MOE_EXPERTS_PER_GROUP, D, MOE_HIDDEN), sd),
        "moe_w_down": nrm((DEPTH, MOE_GROUPS, MOE_EXPERTS_PER_GROUP, MOE_HIDDEN, D),
                          MOE_HIDDEN ** -0.5 * beta),
        "pe_w_gate": nrm((DEPTH, D, D), sd),
        "pe_w_proj": nrm((DEPTH, PLE_DIM, D), PLE_DIM ** -0.5 * beta),
    }


def reference(x, p, gdn_w_in, gdn_conv_w, gdn_a_log, gdn_dt_bias, gdn_norm_w, gdn_w_out,
              m2_w_in, m2_conv_w, m2_conv_b, m2_a_log, m2_dt_bias, m2_d, m2_norm_w, m2_w_out,
              hg_w_in, hg_lb_logits, hg_norm_w, hg_w_out,
              gla_w_in, gla_w_gk, gla_b_gk, gla_norm_w, gla_w_out,
              ln_g, ln_b, moe_w_group, moe_b_group, moe_w_router, moe_b_router,
              moe_w_gate, moe_w_up, moe_w_down, pe_w_gate, pe_w_proj):
    lb_all = jnp.cumsum(jax.nn.softmax(hg_lb_logits.astype(F32), axis=0), axis=0)
    lb_all = lb_all - lb_all[0]
    for i in range(DEPTH):
        m, j = i % N_MIXERS, i // N_MIXERS
        if m == 0:
            h = _gdn_mixer(x, gdn_w_in[j], gdn_conv_w[j], gdn_a_log[j], gdn_dt_bias[j],
                           gdn_norm_w[j], gdn_w_out[j])
        elif m == 1:
            h = _mamba2_mixer(x, m2_w_in[j], m2_conv_w[j], m2_conv_b[j], m2_a_log[j],
                              m2_dt_bias[j], m2_d[j], m2_norm_w[j], m2_w_out[j])
        elif m == 2:
            h = _hgrn2_mixer(x, hg_w_in[j], lb_all[i], hg_norm_w[j], hg_w_out[j])
        else:
            h = _gla_mixer(x, gla_w_in[j], gla_w_gk[j], gla_b_gk[j], gla_norm_w[j], gla_w_out[j])
        x = _layernorm(DEEPNORM_ALPHA * x + h, ln_g[i, 0], ln_b[i, 0])
        h = _hier_moe(x, moe_w_group[i], moe_b_group[i], moe_w_router[i], moe_b_router[i],
                      moe_w_gate[i], moe_w_up[i], moe_w_down[i])
        x = _layernorm(DEEPNORM_ALPHA * x + h, ln_g[i, 1], ln_b[i, 1])
        x = x + jax.nn.sigmoid(x @ pe_w_gate[i]) * (p[i].astype(x.dtype) @ pe_w_proj[i])
    return x
```

```python
import numpy as np
import concourse.bass as bass
import concourse.mybir as mybir

F32 = mybir.dt.float32
BF16 = mybir.dt.bfloat16
AF = mybir.ActivationFunctionType
ALU = mybir.AluOpType
AX = mybir.AxisListType


class View:
    __slots__ = ("tile", "ap")

    def __init__(self, tile, ap):
        self.tile = tile
        self.ap = ap


class Tile:
    def __init__(self, sch, tensor, name):
        self.sch = sch
        self.t = tensor
        self.name = name
        self.w = []
        self.r = []
        self.dsem = None
        self.dcnt = 0
        self.is_dram = False
        self.is_psum = False

    def __getitem__(self, k):
        return View(self, self.t[k])

    def v(self, ap):
        return View(self, ap)


class Sched:
    def __init__(self, nc):
        self.nc = nc
        self.E = {"pe": nc.tensor, "act": nc.scalar, "dve": nc.vector,
                  "pool": nc.gpsimd, "sp": nc.sync}
        self.sem = {k: nc.alloc_semaphore("sem_" + k) for k in ("pe", "act", "dve", "pool")}
        self.cnt = {k: 0 for k in self.sem}
        self.semid = {id(v): k for k, v in self.sem.items()}
        self.seen = {k: {} for k in self.E}
        self.nsem = 4
        self.ntile = 0
        self.last_out_tokens = []

    def sb(self, name, shape, dt):
        self.ntile += 1
        if not hasattr(self, "off"):
            self.off = 16384
            self.cap = 228800
        esz = 4 if dt == F32 else 2
        n = 1
        for d in shape[1:]:
            n *= d
        nbytes = (n * esz + 63) // 64 * 64
        if self.off + nbytes > self.cap:
            raise RuntimeError(f"SBUF overflow allocating {name}: {self.off}+{nbytes}")
        t = self.nc.alloc_sbuf_tensor_at(f"{name}_{self.ntile}", list(shape), dt, offset=self.off)
        self.off += nbytes
        return Tile(self, t, name)

    def mark(self):
        return getattr(self, "off", 16384)

    def reset_to(self, off):
        self.barrier()
        self.off = off
        ph = getattr(self, "phase", 0)
        keep = []
        for t in getattr(self, "_dsems", []):
            if getattr(t, "phase", 0) == ph and ph != 0:
                self._free_dsems.append((t.dsem, t.dcnt))
                t.dsem = None
            else:
                keep.append(t)
        self._dsems = keep
        self._recycled = getattr(self, "_recycled", []) + [x for x in self._free_dsems]
        self.phase = ph + 1

    def start_phases(self):
        self.phase = 1

    def barrier(self):
        dsems = getattr(self, "_dsems", [])
        for k in self.sem:
            pass
        for eng, e in self.E.items():
            seen = self.seen[eng]
            for k, sm in self.sem.items():
                if self.cnt[k] > 0 and seen.get(id(sm), 0) < self.cnt[k]:
                    e.wait_ge(sm, self.cnt[k]); seen[id(sm)] = self.cnt[k]
            for t in dsems:
                if t.dcnt > 0 and seen.get(id(t.dsem), 0) < t.dcnt:
                    e.wait_ge(t.dsem, t.dcnt); seen[id(t.dsem)] = t.dcnt
            for (sm, c) in getattr(self, "_free_dsems", []):
                if c > 0 and seen.get(id(sm), 0) < c:
                    e.wait_ge(sm, c); seen[id(sm)] = c
            if getattr(self, "ccsem", None) is not None and self.cccnt > 0 and seen.get(id(self.ccsem), 0) < self.cccnt:
                e.wait_ge(self.ccsem, self.cccnt); seen[id(self.ccsem)] = self.cccnt

    def ps(self, name, shape, dt):
        self.ntile += 1
        t = Tile(self, self.nc.alloc_psum_tensor(f"{name}_{self.ntile}", list(shape), dt), name)
        t.is_psum = True
        return t

    def dram(self, ap, name):
        t = Tile(self, ap, name)
        t.is_dram = True
        return t

    def _dsem(self, tile):
        if tile.dsem is None:
            if not hasattr(self, "_dsems"):
                self._dsems = []
                self._free_dsems = []
            if self._free_dsems:
                tile.dsem, tile.dcnt = self._free_dsems.pop()
            else:
                tile.dsem = self.nc.alloc_semaphore(f"d_{tile.name}_{self.nsem}")
                self.nsem += 1
                tile.dcnt = 0
            tile.phase = getattr(self, "phase", 0)
            self._dsems.append(tile)
        return tile.dsem

    def _waits(self, eng, outs, ins):
        need = {}
        own = self.sem.get(eng)
        for v in ins:
            for (s, val) in v.tile.w:
                need[id(s)] = (s, max(val, need.get(id(s), (s, 0))[1]))
            if v.tile.is_psum:
                for (s, val) in v.tile.r:
                    if s is not own:
                        need[id(s)] = (s, max(val, need.get(id(s), (s, 0))[1]))
        for v in outs:
            for (s, val) in v.tile.w + v.tile.r:
                need[id(s)] = (s, max(val, need.get(id(s), (s, 0))[1]))
        e = self.E[eng]
        seen = self.seen[eng]
        for sid, (s, val) in need.items():
            k = self.semid.get(sid)
            if k is not None and val > self.cnt[k]:
                if own is not None and s is own:
                    continue
                raise RuntimeError(f"wait on future inc of {k}: {val} > {self.cnt[k]}")
            if seen.get(sid, 0) < val:
                e.wait_ge(s, val)
                seen[sid] = val

    def _commit(self, outs, ins, tok):
        for v in ins:
            v.tile.r.append(tok)
            if len(v.tile.r) > 12:
                d = {}
                for (s, val) in v.tile.r:
                    d[id(s)] = (s, max(val, d.get(id(s), (s, 0))[1]))
                v.tile.r = list(d.values())
        for v in outs:
            if v.tile.is_dram:
                d = {}
                for (s_, val) in v.tile.w + [tok]:
                    d[id(s_)] = (s_, max(val, d.get(id(s_), (s_, 0))[1]))
                v.tile.w = list(d.values())
            else:
                v.tile.w = [tok]
            v.tile.r = []

    def op(self, eng, fn, outs, ins, inc=True):
        import os
        b = os.environ.get("OPBUDGET")
        self.nops = getattr(self, "nops", 0) + 1
        if b is not None and self.nops > int(b):
            return None
        self._waits(eng, outs, ins)
        ins_ = fn()
        s = self.sem[eng]
        if inc:
            ins_.then_inc(s, 1)
            self.cnt[eng] += 1
            tok = (s, self.cnt[eng])
        else:
            tok = (s, self.cnt[eng] + 1)
        self._commit(outs, ins, tok)
        return ins_

    def dma(self, q, out, in_, sbuf_side=None):
        self._waits(q, [out], [in_])
        st = sbuf_side if sbuf_side is not None else (in_.tile if out.tile.is_dram else out.tile)
        s = self._dsem(st)
        ins_ = self.E[q].dma_start(out=out.ap, in_=in_.ap)
        ins_.then_inc(s, 16)
        st.dcnt += 16
        tok = (s, st.dcnt)
        self._commit([out], [in_], tok)
        return tok

    def mm(self, out, lhsT, rhs, start=True, stop=True, inc=None):
        if inc is None:
            inc = stop
        return self.op("pe", lambda: self.nc.tensor.matmul(out.ap, lhsT=lhsT.ap, rhs=rhs.ap, start=start, stop=stop),
                       [out], [lhsT, rhs], inc=inc)

    def tr(self, out, in_, ident, inc=True):
        return self.op("pe", lambda: self.nc.tensor.transpose(out=out.ap, in_=in_.ap, identity=ident.ap),
                       [out], [in_, ident], inc=inc)

    def act(self, out, in_, func, bias=None, scale=None, accum=None):
        ins = [in_]
        kw = {}
        if bias is not None:
            if isinstance(bias, View):
                ins.append(bias); kw["bias"] = bias.ap
            else:
                kw["bias"] = bias
        if scale is not None:
            if isinstance(scale, View):
                ins.append(scale); kw["scale"] = scale.ap
            else:
                kw["scale"] = scale
        outs = [out]
        if accum is not None:
            outs.append(accum); kw["accum_out"] = accum.ap
        return self.op("act", lambda: self.nc.scalar.activation(out=out.ap, in_=in_.ap, func=func, **kw), outs, ins)

    def _e(self, eng):
        return self.E[eng]

    def tt(self, eng, out, a, b, op):
        return self.op(eng, lambda: self._e(eng).tensor_tensor(out=out.ap, in0=a.ap, in1=b.ap, op=op), [out], [a, b])

    def ts(self, eng, out, a, s1, op0, s2=None, op1=None):
        ins = [a]
        a1 = s1.ap if isinstance(s1, View) else s1
        a2 = s2.ap if isinstance(s2, View) else s2
        if isinstance(s1, View): ins.append(s1)
        if isinstance(s2, View): ins.append(s2)
        if op1 is None:
            return self.op(eng, lambda: self._e(eng).tensor_scalar(out=out.ap, in0=a.ap, scalar1=a1, scalar2=None, op0=op0), [out], ins)
        return self.op(eng, lambda: self._e(eng).tensor_scalar(out=out.ap, in0=a.ap, scalar1=a1, scalar2=a2, op0=op0, op1=op1), [out], ins)

    def stt(self, eng, out, a, sc, b, op0, op1):
        ins = [a, b]
        s_ = sc.ap if isinstance(sc, View) else sc
        if isinstance(sc, View): ins.append(sc)
        return self.op(eng, lambda: self._e(eng).scalar_tensor_tensor(out=out.ap, in0=a.ap, scalar=s_, in1=b.ap, op0=op0, op1=op1), [out], ins)

    def cp(self, eng, out, a):
        if eng == "act":
            return self.op("act", lambda: self.nc.scalar.copy(out=out.ap, in_=a.ap), [out], [a])
        return self.op(eng, lambda: self._e(eng).tensor_copy(out=out.ap, in_=a.ap), [out], [a])

    def memset(self, eng, out, val):
        return self.op(eng, lambda: self._e(eng).memset(out.ap, val), [out], [])

    def allreduce(self, out, in_, groups):
        if getattr(self, "ccsem", None) is None:
            self.ccsem = self.nc.alloc_semaphore("ccsem")
            self.cccnt = 0
        self._waits("pool", [out], [in_])
        ins_ = self.nc.gpsimd.collective_compute("AllReduce", ALU.add, replica_groups=groups, ins=[in_.ap], outs=[out.ap])
        ins_.then_inc(self.ccsem)
        self.cccnt += 1
        tok = (self.ccsem, self.cccnt)
        self._commit([out], [in_], tok)

    def finish(self, dram_tiles):
        e = self.E["sp"]
        for t in dram_tiles:
            for (s, val) in t.w:
                e.wait_ge(s, val)

ALPHA = float((2 * 4) ** 0.25)
LN_EPS = 1e-5
DM = 1024
NEG_BIG = -30000.0


def make_consts(s):
    nc = s.nc
    C = {}
    idf = s.sb("identf", [128, 128], F32)
    s.memset("pool", idf[:], 1.0)
    s.op("pool", lambda: nc.gpsimd.affine_select(out=idf.t[:], in_=idf.t[:], pattern=[[-1, 128]],
                                                 compare_op=ALU.is_equal, fill=0.0, base=0, channel_multiplier=1),
         [idf[:]], [idf[:]])
    idb = s.sb("identb", [128, 128], BF16)
    s.cp("pool", idb[:], idf[:])
    C["idf"] = idf
    C["idb"] = idb
    return C


def layernorm_tile(s, y, xn, out, gam, bet, st6, mv, rstd, nmr):
    nc = s.nc
    for hf in range(2):
        s.op("dve", lambda hf=hf: nc.vector.bn_stats(out=st6.t[:, hf * 6:(hf + 1) * 6], in_=y.t[:, hf * 512:(hf + 1) * 512]),
             [st6[:]], [y[:]])
    s.op("dve", lambda: nc.vector.bn_aggr(out=mv.t[:], in_=st6.t[:]), [mv[:]], [st6[:]])
    s.act(rstd[:], mv[:, 1:2], AF.Ln, bias=LN_EPS)
    s.act(rstd[:], rstd[:], AF.Exp, scale=-0.5)
    s.stt("dve", nmr[:], mv[:, 0:1], -1.0, rstd[:], ALU.mult, ALU.mult)
    s.act(xn[:], y[:], AF.Identity, bias=nmr[:], scale=rstd[:])
    s.tt("pool", xn[:], xn[:], gam[:], ALU.mult)
    s.tt("pool", out[:], xn[:], bet[:], ALU.add)


def token_phase(s, C, D, NT, GT, W, last, upto=9, PS=None):
    nc = s.nc
    WC = W // 128
    idf, idb = C["idf"], C["idb"]
    lng = [s.sb(f"lng{i}", [128, DM], F32) for i in range(2)]
    lnb = [s.sb(f"lnb{i}", [128, DM], F32) for i in range(2)]
    for i in range(2):
        s.dma("sp", lng[i][:], D["lng"].v(D["lng"].t[i:i + 1, :].partition_broadcast(128)))
        s.dma("sp", lnb[i][:], D["lnb"].v(D["lnb"].t[i:i + 1, :].partition_broadcast(128)))
    wr = s.sb("wr", [128, 8, 36], F32)
    s.dma("sp", wr[:], D["wrt"][:, :, :])
    wrh = s.sb("wrh", [128, 8, 36], BF16)
    wrl = s.sb("wrl", [128, 8, 36], BF16)
    wrt_ = s.sb("wrt_", [128, 8, 36], F32)
    s.cp("dve", wrh[:], wr[:])
    s.tt("dve", wrt_[:], wr[:], wrh[:], ALU.subtract)
    s.cp("dve", wrl[:], wrt_[:])
    brt = s.sb("brt", [128, 36], F32)
    s.dma("sp", brt[:], D["brt"].v(D["brt"].t[0:1, :].partition_broadcast(128)))
    wbig_n = max(WC * DM, 10 * DM)
    wbig = s.sb("wbig", [128, wbig_n], BF16)
    sel = s.sb("sel", [32, 32, 128], BF16)
    s.memset("pool", sel[:], 1.0)
    s.op("pool", lambda: nc.gpsimd.affine_select(out=sel.t[:], in_=sel.t[:], pattern=[[-1, 32], [0, 128]],
                                                 compare_op=ALU.is_equal, fill=0.0, base=0, channel_multiplier=1),
         [sel[:]], [sel[:]])

    if PS is None:
        PS = [s.ps(f"ps{i}", [128, 512], F32) for i in range(8)]

    xT1 = s.sb("x1T", [128, 8, GT], BF16)
    acc = s.sb("acc", [128, GT // 128, DM], F32)
    combT = [s.sb(f"combT{i}", [32, GT], BF16) for i in range(2)]
    oTb = [s.sb(f"oTb{i}", [128, WC, 256], BF16) for i in range(2)]
    xt = [s.sb(f"xt{i}", [128, DM], F32) for i in range(2)]
    y = s.sb("y", [128, DM], F32)
    xn = s.sb("xn", [128, DM], F32)
    x1 = [s.sb(f"x1_{i}", [128, DM], F32) for i in range(2)]
    xTf = s.sb("xTf", [128, 8, 128], F32)
    xTl = s.sb("xTl", [128, 8, 128], BF16)
    st6 = s.sb("st6", [128, 12], F32)
    mv = s.sb("mv", [128, 2], F32)
    rstd = s.sb("rstd", [128, 1], F32)
    nmr = s.sb("nmr", [128, 1], F32)
    lg = s.sb("lg", [128, 36], F32)
    sm = {k: s.sb("sm_" + k, [128, n], F32) for k, n in
          [("gmax", 1), ("goh", 4), ("gex", 4), ("gsum", 1), ("gp", 1), ("es", 8), ("m1", 1), ("k1", 8),
           ("e2", 8), ("m2", 1), ("k2", 8), ("d", 1), ("w1", 1), ("w2", 1), ("wi", 8), ("comb", 32), ("lo", 32)]}
    combh = s.sb("combh", [128, 32], BF16)
    combl = s.sb("combl", [128, 32], BF16)
    wg = [s.sb(f"wg{i}", [128, 8, 256], BF16) for i in range(2)]
    wu = [s.sb(f"wu{i}", [128, 8, 256], BF16) for i in range(2)]
    wd = [s.sb(f"wd{i}", [128, 2, DM], BF16) for i in range(2)]
    cb = [s.sb(f"cb{i}", [128, 512], F32) for i in range(2)]
    sg = [s.sb(f"sg{i}", [128, 512], F32) for i in range(2)]
    tmp = [s.sb(f"tmp{i}", [128, 512], F32) for i in range(2)]
    hT = [s.sb(f"hT{i}", [128, 2, 512], BF16) for i in range(2)]
    x2 = s.sb("x2", [128, DM], F32)
    x2T = s.sb("x2T", [128, 8, 128], BF16)
    pt = s.sb("pt", [128, 256], F32)
    pT = s.sb("pT", [128, 2, 128], BF16)
    sgt = s.sb("sgt", [128, DM], F32)
    xo = [s.sb(f"xo{i}", [128, DM], F32) for i in range(1)]
    xoT = [s.sb(f"xoT{i}", [128, 8, 128], BF16) for i in range(1)]

    NG = NT // GT
    TPG = GT // 128
    for g in range(NG):
        t0g = g * GT
        wo = wbig.v(wbig.t[:, 0:WC * DM].rearrange("p (c n) -> p c n", c=WC))
        s.dma("pool", wo, D["w_out"].v(D["w_out"].t.rearrange("(c p) n -> p c n", p=128)))
        for tl in range(TPG):
            tok0 = t0g + tl * 128
            if tl % 2 == 0:
                ob = oTb[(tl // 2) % 2]
                nb = min(256, GT - tl * 128)
                s.dma("sp", ob[:, :, 0:nb], D["oT"].v(D["oT"].t[:, tok0:tok0 + nb].rearrange("(c p) t -> p c t", p=128)))
            xc = xt[tl % 2]
            s.dma("sp", xc[:], D["xres"][tok0:tok0 + 128, :])
            o_off = (tl % 2) * 128
            for hf in range(2):
                for wc in range(WC):
                    s.mm(PS[hf][:, :], ob[:, wc, o_off:o_off + 128], wo.tile.v(wo.ap[:, wc, hf * 512:(hf + 1) * 512]),
                         start=(wc == 0), stop=(wc == WC - 1))
            for hf in range(2):
                s.stt("dve", y[:, hf * 512:(hf + 1) * 512], xc[:, hf * 512:(hf + 1) * 512], ALPHA, PS[hf][:, :], ALU.mult, ALU.add)
            x1c = x1[tl % 2]
            layernorm_tile(s, y, xn, x1c, lng[0], lnb[0], st6, mv, rstd, nmr)
            s.dma("sp", D["x1res"][tok0:tok0 + 128, :], x1c[:])
            if upto == 1:
                s.dma("sp", D["xout"][tok0:tok0 + 128, :], x1c[:])
                continue
            for dc in range(8):
                pt_ = PS[2 + dc // 4]
                s.tr(pt_[:, (dc % 4) * 128:(dc % 4 + 1) * 128], x1c[:, dc * 128:(dc + 1) * 128], idf[:])
            for hh in range(2):
                s.cp("act", xTf.v(xTf.t[:, hh * 4:(hh + 1) * 4, :].rearrange("p c t -> p (c t)")), PS[2 + hh][:, :])
            s.cp("dve", xT1[:, :, tl * 128:(tl + 1) * 128], xTf[:])
            if upto == 2:
                s.dma("sp", D["xout"][tok0:tok0 + 128, :], xTf.v(xTf.t[:, :, :].rearrange("p c t -> p (c t)")))
                continue
            xh = xT1[:, :, tl * 128:(tl + 1) * 128]
            s.tt("dve", xTf[:], xTf[:], xh, ALU.subtract)
            s.cp("dve", xTl[:], xTf[:])
            k_ = 0
            for (xa, wa) in ((xh, wrh), (xTl[:], wrh), (xh, wrl)):
                for dc in range(8):
                    s.mm(PS[4][:, 0:36], xa.tile.v(xa.ap[:, dc, :]), wa[:, dc, :], start=(k_ == 0), stop=(k_ == 23))
                    k_ += 1
            s.tt("dve", lg[:], PS[4][:, 0:36], brt[:], ALU.add)
            routing(s, lg, sm)
            if upto == 3:
                s.dma("sp", D["xout"][tok0:tok0 + 128, 0:32], sm["comb"][:])
                continue
            s.cp("dve", combh[:], sm["comb"][:])
            s.tt("dve", sm["lo"][:], sm["comb"][:], combh[:], ALU.subtract)
            s.cp("dve", combl[:], sm["lo"][:])
            pb = PS[5].v(PS[5].t[:, :].bitcast(BF16))
            s.tr(PS[5].v(pb.ap[0:32, 0:128]), combh[:], idb[:])
            s.tr(PS[5].v(pb.ap[0:32, 128:256]), combl[:], idb[:])
            s.cp("act", combT[0][:, tl * 128:(tl + 1) * 128], PS[5].v(pb.ap[0:32, 0:128]))
            s.cp("act", combT[1][:, tl * 128:(tl + 1) * 128], PS[5].v(pb.ap[0:32, 128:256]))
        if upto <= 3:
            continue
        if upto == 4:
            s.dma("sp", D["xout"][0:32, 0:GT], combT[0][:])
            continue
        NB = GT // 512 if GT >= 512 else 1
        BW = min(512, GT)
        for e in range(32):
            b = e % 2
            s.dma("pool", wg[b][:], D["w_gate"].v(D["w_gate"].t[e].rearrange("(c p) f -> p c f", p=128)))
            s.dma("pool", wu[b][:], D["w_up"].v(D["w_up"].t[e].rearrange("(c p) f -> p c f", p=128)))
            s.dma("pool", wd[b][:], D["w_down"].v(D["w_down"].t[e].rearrange("(c p) n -> p c n", p=128)))
            for blk in range(NB):
                k = (e * NB + blk) % 2
                bs = slice(blk * BW, (blk + 1) * BW)
                s.mm(PS[0][:, 0:BW], sel[:, e, :], combT[0][:, bs], start=True, stop=False)
                s.mm(PS[0][:, 0:BW], sel[:, e, :], combT[1][:, bs], start=False, stop=True)
                s.cp("act", cb[k][:, 0:BW], PS[0][:, 0:BW])
                for fc in range(2):
                    gP, uP = PS[1 + 2 * fc], PS[2 + 2 * fc]
                    for dc in range(8):
                        s.mm(gP[:, 0:BW], wg[b][:, dc, fc * 128:(fc + 1) * 128], xT1[:, dc, bs], start=(dc == 0), stop=(dc == 7))
                    for dc in range(8):
                        s.mm(uP[:, 0:BW], wu[b][:, dc, fc * 128:(fc + 1) * 128], xT1[:, dc, bs], start=(dc == 0), stop=(dc == 7))
                    s.act(sg[fc][:, 0:BW], gP[:, 0:BW], AF.Silu)
                    s.tt("dve", tmp[fc][:, 0:BW], sg[fc][:, 0:BW], uP[:, 0:BW], ALU.mult)
                    s.tt("pool", hT[k][:, fc, 0:BW], tmp[fc][:, 0:BW], cb[k][:, 0:BW], ALU.mult)
                for tt_ in range(BW // 128):
                    tl = blk * (BW // 128) + tt_
                    for hf in range(2):
                        dP = PS[5 + (tt_ * 2 + hf) % 3]
                        for fc in range(2):
                            s.mm(dP[:, :], hT[k][:, fc, tt_ * 128:(tt_ + 1) * 128], wd[b][:, fc, hf * 512:(hf + 1) * 512],
                                 start=(fc == 0), stop=(fc == 1))
                        a_ = acc[:, tl, hf * 512:(hf + 1) * 512]
                        if e == 0:
                            s.cp("act", a_, dP[:, :])
                        else:
                            s.tt("dve", a_, a_, dP[:, :], ALU.add)
        if upto == 5:
            for tl in range(TPG):
                s.dma("sp", D["xout"][t0g + tl * 128:t0g + (tl + 1) * 128, :], acc[:, tl, :])
            continue
        wpg = wbig.v(wbig.t[:, 0:8 * DM].rearrange("p (c n) -> p c n", c=8))
        wpp = wbig.v(wbig.t[:, 8 * DM:10 * DM].rearrange("p (c n) -> p c n", c=2))
        s.dma("pool", wpg, D["pe_g"].v(D["pe_g"].t.rearrange("(c p) n -> p c n", p=128)))
        s.dma("pool", wpp, D["pe_p"].v(D["pe_p"].t.rearrange("(c p) n -> p c n", p=128)))
        for tl in range(TPG):
            tok0 = t0g + tl * 128
            xc = xt[tl % 2]
            s.dma("sp", xc[:], D["x1res"][tok0:tok0 + 128, :])
            s.dma("sp", pt[:], D["pl"][tok0:tok0 + 128, :])
            s.stt("dve", y[:], xc[:], ALPHA, acc[:, tl, :], ALU.mult, ALU.add)
            layernorm_tile(s, y, xn, x2, lng[1], lnb[1], st6, mv, rstd, nmr)
            for dc in range(8):
                pt_ = PS[dc // 4]
                s.tr(pt_[:, (dc % 4) * 128:(dc % 4 + 1) * 128], x2[:, dc * 128:(dc + 1) * 128], idf[:])
            for hh in range(2):
                s.cp("act" if hh == 0 else "dve", x2T.v(x2T.t[:, hh * 4:(hh + 1) * 4, :].rearrange("p c t -> p (c t)")), PS[hh][:, :])
            for pc in range(2):
                s.tr(PS[2][:, pc * 128:(pc + 1) * 128], pt[:, pc * 128:(pc + 1) * 128], idf[:])
            s.cp("act", pT.v(pT.t[:, :, :].rearrange("p c t -> p (c t)")), PS[2][:, 0:256])
            for hf in range(2):
                for dc in range(8):
                    s.mm(PS[3 + hf][:, :], x2T[:, dc, :], wbig.v(wpg.ap[:, dc, hf * 512:(hf + 1) * 512]), start=(dc == 0), stop=(dc == 7))
                for pc in range(2):
                    s.mm(PS[5 + hf][:, :], pT[:, pc, :], wbig.v(wpp.ap[:, pc, hf * 512:(hf + 1) * 512]), start=(pc == 0), stop=(pc == 1))
            xoc = xo[0]
            for hf in range(2):
                hs = slice(hf * 512, (hf + 1) * 512)
                s.act(sgt[:, hs], PS[3 + hf][:, :], AF.Sigmoid)
                s.tt("dve", sgt[:, hs], sgt[:, hs], PS[5 + hf][:, :], ALU.mult)
                s.tt("pool", xoc[:, hs], sgt[:, hs], x2[:, hs], ALU.add)
            s.dma("sp", D["xout"][tok0:tok0 + 128, :], xoc[:])
            if not last:
                for dc in range(8):
                    pt_ = PS[dc // 4]
                    s.tr(pt_[:, (dc % 4) * 128:(dc % 4 + 1) * 128], xoc[:, dc * 128:(dc + 1) * 128], idf[:])
                xoTc = xoT[0]
                for hh in range(2):
                    s.cp("act" if hh == 0 else "dve", xoTc.v(xoTc.t[:, hh * 4:(hh + 1) * 4, :].rearrange("p c t -> p (c t)")), PS[hh][:, :])
                s.dma("sp", D["xTout"].v(D["xTout"].t[:, tok0:tok0 + 128].rearrange("(c p) t -> p c t", p=128)), xoTc[:])


def routing(s, lg, sm):
    nc = s.nc
    gl = lg[:, 0:4]
    s.op("dve", lambda: nc.vector.reduce_max(out=sm["gmax"].t[:], in_=lg.t[:, 0:4], axis=AX.X), [sm["gmax"][:]], [lg[:]])
    s.ts("dve", sm["goh"][:], gl, sm["gmax"][:], ALU.is_equal)
    s.ts("dve", sm["gex"][:], gl, sm["gmax"][:], ALU.subtract)
    s.act(sm["gex"][:], sm["gex"][:], AF.Exp)
    s.op("dve", lambda: nc.vector.reduce_sum(out=sm["gsum"].t[:], in_=sm["gex"].t[:], axis=AX.X), [sm["gsum"][:]], [sm["gex"][:]])
    s.op("dve", lambda: nc.vector.reciprocal(out=sm["gp"].t[:], in_=sm["gsum"].t[:]), [sm["gp"][:]], [sm["gsum"][:]])
    s.ts("dve", sm["es"][:], lg[:, 4:12], sm["goh"][:, 0:1], ALU.mult)
    for g in range(1, 4):
        s.stt("dve", sm["es"][:], lg[:, 4 + 8 * g:12 + 8 * g], sm["goh"][:, g:g + 1], sm["es"][:], ALU.mult, ALU.add)
    s.op("dve", lambda: nc.vector.reduce_max(out=sm["m1"].t[:], in_=sm["es"].t[:], axis=AX.X), [sm["m1"][:]], [sm["es"][:]])
    s.ts("dve", sm["k1"][:], sm["es"][:], sm["m1"][:], ALU.is_equal)
    s.stt("dve", sm["e2"][:], sm["k1"][:], NEG_BIG, sm["es"][:], ALU.mult, ALU.add)
    s.op("dve", lambda: nc.vector.reduce_max(out=sm["m2"].t[:], in_=sm["e2"].t[:], axis=AX.X), [sm["m2"][:]], [sm["e2"][:]])
    s.ts("dve", sm["k2"][:], sm["e2"][:], sm["m2"][:], ALU.is_equal)
    s.tt("dve", sm["d"][:], sm["m2"][:], sm["m1"][:], ALU.subtract)
    s.act(sm["d"][:], sm["d"][:], AF.Exp)
    s.ts("dve", sm["d"][:], sm["d"][:], 1.0, ALU.add)
    s.op("dve", lambda: nc.vector.reciprocal(out=sm["w1"].t[:], in_=sm["d"].t[:]), [sm["w1"][:]], [sm["d"][:]])
    s.ts("dve", sm["w2"][:], sm["w1"][:], -1.0, ALU.mult, 1.0, ALU.add)
    s.tt("dve", sm["w1"][:], sm["w1"][:], sm["gp"][:], ALU.mult)
    s.tt("dve", sm["w2"][:], sm["w2"][:], sm["gp"][:], ALU.mult)
    s.ts("dve", sm["wi"][:], sm["k1"][:], sm["w1"][:], ALU.mult)
    s.stt("dve", sm["wi"][:], sm["k2"][:], sm["w2"][:], sm["wi"][:], ALU.mult, ALU.add)
    for g in range(4):
        s.ts("dve", sm["comb"][:, 8 * g:8 * g + 8], sm["wi"][:], sm["goh"][:, g:g + 1], ALU.mult)

NORM_EPS = 1e-6


def xT_view(D, lo, hi):
    t = D["xT"]
    if len(t.t.shape) == 2:
        return t.v(t.t[:, lo:hi].rearrange("(c p) t -> p c t", p=128))
    slot = lo // 4096
    assert (hi - 1) // 4096 == slot
    return t.v(t.t[slot, :, lo - slot * 4096:hi - slot * 4096].rearrange("(c p) t -> p c t", p=128))


def make_masks(s, C):
    nc = s.nc

    def tri(name, pat, cm, op):
        t = s.sb(name, [128, 128], F32)
        s.memset("pool", t[:], 1.0)
        s.op("pool", lambda: nc.gpsimd.affine_select(out=t.t[:], in_=t.t[:], pattern=[[pat, 128]], compare_op=op,
                                                     fill=0.0, base=0, channel_multiplier=cm), [t[:]], [t[:]])
        return t
    C["U128"] = [tri("U128f", 1, -1, ALU.is_ge), tri("U128b", -1, 1, ALU.is_ge)]
    C["SU128"] = [tri("SU128f", -1, 1, ALU.is_gt), tri("SU128b", 1, -1, ALU.is_gt)]
    U64 = [tri("U64f", 1, -1, ALU.is_ge), tri("U64b", -1, 1, ALU.is_ge)]
    SU64 = [tri("SU64f", -1, 1, ALU.is_gt), tri("SU64b", 1, -1, ALU.is_gt)]
    s.memset("pool", U64[0][0:64, 64:128], 0.0)
    s.memset("pool", U64[1][64:128, 0:64], 0.0)
    s.memset("pool", SU64[0][64:128, 0:64], 0.0)
    s.memset("pool", SU64[1][0:64, 64:128], 0.0)
    C["U64"] = U64
    C["SU64"] = SU64
    ones = s.sb("ones", [128, 128], F32)
    s.memset("pool", ones[:], 1.0)
    C["ones"] = ones
    C["U64m"] = U64
    return C


def formA_tile(s, C, d, PSg, PSa, PSo, qT, kT, k_tm, g_tm, v_tm, V, St, W):
    nc = s.nc
    U, SU = C["U64"][d], C["SU64"][d]
    gcT = PSg.tile.v(PSg.ap[:, 0:128])
    dec = PSg.tile.v(PSg.ap[:, 128:256])
    s.mm(gcT, g_tm, U[:])
    s.mm(dec, SU[:], g_tm)
    Eg, Eng, Ed = W["Eg"], W["Eng"], W["Ed"]
    s.act(Eg[:], gcT, AF.Exp)
    s.act(Eng[:], gcT, AF.Exp, scale=-1.0)
    s.act(Ed[:], dec, AF.Exp)
    QdT, KdT, Kl = W["QdT"], W["KdT"], W["Kl"]
    s.tt("dve", QdT[:], qT, Eg[:], ALU.mult)
    s.tt("pool", KdT[:], kT, Eng[:], ALU.mult)
    s.tt("pool", Kl[:], k_tm, Ed[:], ALU.mult)
    AT = PSa.tile.v(PSa.ap[:, 0:128])
    dS = PSa.tile.v(PSa.ap[:, 128:128 + V])
    s.mm(AT, KdT[:], QdT[:])
    ATm = W["ATm"]
    s.tt("dve", ATm[:], AT, U[:], ALU.mult)
    first, second = (slice(0, 64), slice(64, 128)) if d == 0 else (slice(64, 128), slice(0, 64))
    e_first, e_second = (63, 127) if d == 0 else (64, 0)
    S = St["S"]
    Sb = St["Sb"]
    cur = Sb[St["i"] % 2]
    s.mm(PSo.tile.v(PSo.ap[first, :]), ATm[:, first], v_tm, start=True, stop=False)
    s.mm(PSo.tile.v(PSo.ap[first, :]), QdT[:, first], cur[:], start=False, stop=True)
    s.mm(dS, Kl[first, :], v_tm.tile.v(v_tm.ap[first, :]))
    s.stt("dve", S[:], S[:], Eg[:, e_first:e_first + 1], dS, ALU.mult, ALU.add)
    St["i"] += 1
    nxt = Sb[St["i"] % 2]
    s.cp("act", nxt[:], S[:])
    s.mm(PSo.tile.v(PSo.ap[second, :]), ATm[:, second], v_tm, start=True, stop=False)
    s.mm(PSo.tile.v(PSo.ap[second, :]), QdT[:, second], nxt[:], start=False, stop=True)
    s.mm(dS, Kl[second, :], v_tm.tile.v(v_tm.ap[second, :]))
    s.stt("dve", S[:], S[:], Eg[:, e_second:e_second + 1], dS, ALU.mult, ALU.add)
    St["i"] += 1
    nxt2 = Sb[St["i"] % 2]
    s.cp("act", nxt2[:], S[:])


def formA_work(s, V):
    W = {}
    for n in ("Eg", "Eng", "Ed"):
        W[n] = s.sb(n, [128, 128], F32)
    for n in ("QdT", "KdT", "Kl", "ATm"):
        W[n] = s.sb(n, [128, 128], BF16)
    return W


def out_stage(s, C, d, PSo_all, PSt, D, tok0, ncol, nh, V, sgate, normB, Wk, col0=0, pre_gate=False):
    nc = s.nc
    osb = Wk["osb"][Wk["oi"] % 2]
    Wk["oi"] += 1
    if d == 0:
        s.cp("act", osb[:, 0:ncol], PSo_all)
        s.dma("sp", D["of"].v(D["of"].t[tok0:tok0 + 128, col0:col0 + ncol]), osb[:, 0:ncol])
        return
    ofl = Wk["ofl"]
    s.dma("sp", ofl[:, 0:ncol], D["of"].v(D["of"].t[tok0:tok0 + 128, col0:col0 + ncol]))
    s.tt("dve", osb[:, 0:ncol], ofl[:, 0:ncol], PSo_all, ALU.add)
    ss, rs, sq = Wk["ss"], Wk["rs"], Wk["sq"]
    if pre_gate:
        s.tt("dve", osb[:, 0:ncol], osb[:, 0:ncol], sgate, ALU.mult)
    s.memset("pool", ss[:], 0.0)
    for h in range(nh):
        s.act(sq[:, 0:V], osb[:, h * V:(h + 1) * V], AF.Square, accum=ss[:, h:h + 1])
    s.act(rs[:, 0:nh], ss[:, 0:nh], AF.Ln, bias=NORM_EPS, scale=1.0 / V)
    s.act(rs[:, 0:nh], rs[:, 0:nh], AF.Exp, scale=-0.5)
    og = Wk["og"]
    for h in range(nh):
        if pre_gate:
            s.ts("dve", osb[:, h * V:(h + 1) * V], osb[:, h * V:(h + 1) * V], rs[:, h:h + 1], ALU.mult)
        else:
            s.stt("dve", osb[:, h * V:(h + 1) * V], osb[:, h * V:(h + 1) * V], rs[:, h:h + 1], sgate.tile.v(sgate.ap[:, h * V:(h + 1) * V]),
                  ALU.mult, ALU.mult)
    s.tt("pool", og[:, 0:ncol], osb[:, 0:ncol], normB, ALU.mult)
    ptb = PSt.tile.v(PSt.ap.bitcast(BF16))
    nchunk = ncol // 128
    oTs = Wk["oTs"][Wk["oi"] % 2]
    for c in range(nchunk):
        s.tr(PSt.tile.v(ptb.ap[:, c * 128:(c + 1) * 128]), og[:, c * 128:(c + 1) * 128], C["idb"][:])
    s.cp("act", oTs[:, 0:nchunk, :], PSt.tile.v(ptb.ap[:, 0:nchunk * 128].rearrange("p (c t) -> p c t", c=nchunk)))
    s.dma("sp", D["oT"].v(D["oT"].t[col0:col0 + ncol, tok0:tok0 + 128].rearrange("(c p) t -> p c t", p=128)), oTs[:, 0:nchunk, :])


def out_work(s, maxcol):
    Wk = {"oi": 0}
    Wk["osb"] = [s.sb(f"osb{i}", [128, maxcol], F32) for i in range(2)]
    Wk["ofl"] = s.sb("ofl", [128, maxcol], F32)
    Wk["og"] = s.sb("og", [128, maxcol], BF16)
    Wk["sq"] = s.sb("sq", [128, 512], F32)
    Wk["ss"] = s.sb("ss", [128, 8], F32)
    Wk["rs"] = s.sb("rs", [128, 8], F32)
    Wk["oTs"] = [s.sb(f"oTs{i}", [128, maxcol // 128, 128], BF16) for i in range(2)]
    return Wk


def hg_mixer(s, C, D, L, PS):
    nc = s.nc
    nh, V = 2, 128
    win = s.sb("win", [128, 8, 1280], BF16)
    s.dma("pool", win[:], D["w_in"].v(D["w_in"].t.rearrange("(c p) n -> p c n", p=128)))
    def lb_from_logits(src_view, n, nm):
        lgt = s.sb(nm + "lg", [128, 4, n], F32)
        s.dma("sp", lgt[:], src_view)
        s.act(lgt[:], lgt[:], AF.Exp)
        num = s.sb(nm + "num", [128, n], F32)
        den = s.sb(nm + "den", [128, n], F32)
        s.tt("dve", num[:], lgt[:, 1, :], lgt[:, 2, :], ALU.add)
        s.tt("dve", den[:], lgt[:, 0, :], lgt[:, 3, :], ALU.add)
        s.tt("dve", den[:], den[:], num[:], ALU.add)
        s.op("dve", lambda: nc.vector.reciprocal(out=den.t[:], in_=den.t[:]), [den[:]], [den[:]])
        s.tt("dve", num[:], num[:], den[:], ALU.mult)
        return num
    lbB = lb_from_logits(D["lbrow"].v(D["lbrow"].t[0:1, :, :].partition_broadcast(128)), 256, "lbr")
    omlB = s.sb("omlB", [128, 256], F32)
    s.ts("dve", omlB[:], lbB[:], -1.0, ALU.mult, 1.0, ALU.add)
    lbc = lb_from_logits(D["lbcol"][:, :, :], 2, "lbc")
    omlc = s.sb("omlc", [128, 2], F32)
    nomlc = s.sb("nomlc", [128, 2], F32)
    s.ts("dve", omlc[:], lbc[:], -1.0, ALU.mult, 1.0, ALU.add)
    s.ts("dve", nomlc[:], omlc[:], -1.0, ALU.mult)
    normB = s.sb("normB", [128, 2, 128], F32)
    for h in range(2):
        s.dma("sp", normB[:, h, :], D["normw"].v(D["normw"].t[0:1, :].partition_broadcast(128)))
    xTb = [s.sb(f"xTb{i}", [128, 8, 512], BF16) for i in range(2)]
    qTs = [s.sb(f"qTs{h}", [128, 512], F32) for h in range(2)]
    kTs = [s.sb(f"kTs{h}", [128, 512], F32) for h in range(2)]
    sig = s.sb("sig", [128, 256], F32)
    t1 = s.sb("t1", [128, 256], F32)
    t2 = s.sb("t2", [128, 256], F32)
    ktm = s.sb("ktm", [128, 256], F32)
    gtm = s.sb("gtm", [128, 256], F32)
    vtm = s.sb("vtm", [128, 256], BF16)
    sgate = s.sb("sgate", [128, 256], F32)
    W = formA_work(s, V)
    Wk = out_work(s, 256)
    NBLK = L // 512
    for d in range(2):
        St = []
        for h in range(nh):
            S = s.sb(f"S{d}{h}", [128, V], F32)
            Sb = [s.sb(f"Sb{d}{h}{i}", [128, V], BF16) for i in range(2)]
            s.memset("dve", S[:], 0.0)
            s.memset("dve", Sb[0][:], 0.0)
            St.append({"S": S, "Sb": Sb, "i": 0})
        fcol = 256 + 256 * d
        blocks = range(NBLK) if d == 0 else range(NBLK - 1, -1, -1)
        for bi, blk in enumerate(blocks):
            xb = xTb[bi % 2]
            s.dma("sp", xb[:], xT_view(D, blk * 512, (blk + 1) * 512))
            for h in range(nh):
                for dc in range(8):
                    s.mm(PS[0][:, :], win[:, dc, h * 128:(h + 1) * 128], xb[:, dc, :], start=(dc == 0), stop=(dc == 7))
                s.act(qTs[h][:], PS[0][:, :], AF.Silu)
                for dc in range(8):
                    s.mm(PS[1][:, :], win[:, dc, fcol + h * 128:fcol + (h + 1) * 128], xb[:, dc, :], start=(dc == 0), stop=(dc == 7))
                s.act(kTs[h][:], PS[1][:, :], AF.Sigmoid)
                s.ts("dve", kTs[h][:], kTs[h][:], nomlc[:, h:h + 1], ALU.mult, omlc[:, h:h + 1], ALU.add)
            tiles = range(4) if d == 0 else range(3, -1, -1)
            for tl in tiles:
                tsl = slice(tl * 128, (tl + 1) * 128)
                tok0 = blk * 512 + tl * 128
                for dc in range(8):
                    s.mm(PS[2][:, 0:256], xb[:, dc, tsl], win[:, dc, fcol:fcol + 256], start=(dc == 0), stop=(dc == 7))
                for dc in range(8):
                    s.mm(PS[2][:, 256:512], xb[:, dc, tsl], win[:, dc, 768:1024], start=(dc == 0), stop=(dc == 7))
                s.act(sig[:], PS[2][:, 0:256], AF.Sigmoid)
                s.cp("act", vtm[:], PS[2][:, 256:512])
                s.tt("dve", t1[:], sig[:], omlB[:], ALU.mult)
                s.tt("pool", ktm[:], omlB[:], t1[:], ALU.subtract)
                s.tt("pool", t2[:], t1[:], lbB[:], ALU.add)
                s.act(gtm[:], t2[:], AF.Ln)
                if d == 1:
                    for dc in range(8):
                        s.mm(PS[3][:, 0:256], xb[:, dc, tsl], win[:, dc, 1024:1280], start=(dc == 0), stop=(dc == 7))
                    s.act(sgate[:], PS[3][:, 0:256], AF.Silu)
                for h in range(nh):
                    hs = slice(h * 128, (h + 1) * 128)
                    formA_tile(s, C, d, PS[4][:, h * 256:(h + 1) * 256], PS[5][:, h * 256:(h + 1) * 256], PS[6][:, h * V:(h + 1) * V],
                               qTs[h][:, tsl], kTs[h][:, tsl], ktm[:, hs], gtm[:, hs], vtm[:, hs], V, St[h], W)
                out_stage(s, C, d, PS[6][:, 0:256], PS[7][:, :], D, tok0, 256, nh, V, sgate[:],
                          normB.v(normB.t[:, :, :].rearrange("p h v -> p (h v)")), Wk)


def gla_mixer(s, C, D, L, PS):
    nc = s.nc
    nh, V = 1, 256
    win = s.sb("win", [128, 8, 800], BF16)
    s.dma("pool", win[:], D["w_in"].v(D["w_in"].t.rearrange("(c p) n -> p c n", p=128)))
    wgk = s.sb("wgk", [16, 2, 128], F32)
    s.dma("sp", wgk[:], D["wgk"].v(D["wgk"].t.rearrange("z r k -> r z k")))
    bgkB = s.sb("bgkB", [128, 2, 128], F32)
    s.dma("sp", bgkB[:], D["bgk"].v(D["bgk"].t.rearrange("(o z) k -> o z k", o=1).partition_broadcast(128)))
    normB = s.sb("normB", [128, 256], F32)
    s.dma("sp", normB[:], D["normw"].v(D["normw"].t[0:1, :].partition_broadcast(128)))
    xTb = [s.sb(f"xTb{i}", [128, 8, 512], BF16) for i in range(2)]
    qTs = s.sb("qTs", [128, 512], F32)
    kTs = s.sb("kTs", [128, 512], F32)
    rTs = s.sb("rTs", [16, 512], F32)
    z = s.sb("z", [128, 128], F32)
    ktm = s.sb("ktm", [128, 128], F32)
    gtm = s.sb("gtm", [128, 128], F32)
    vtm = s.sb("vtm", [128, 256], BF16)
    sgate = s.sb("sgate", [128, 256], F32)
    W = formA_work(s, V)
    Wk = out_work(s, 256)
    NBLK = L // 512
    for d in range(2):
        S = s.sb(f"S{d}", [128, V], F32)
        Sb = [s.sb(f"Sb{d}{i}", [128, V], BF16) for i in range(2)]
        s.memset("dve", S[:], 0.0)
        s.memset("dve", Sb[0][:], 0.0)
        St = {"S": S, "Sb": Sb, "i": 0}
        rcol = 768 + 16 * d
        blocks = range(NBLK) if d == 0 else range(NBLK - 1, -1, -1)
        for bi, blk in enumerate(blocks):
            xb = xTb[bi % 2]
            s.dma("sp", xb[:], xT_view(D, blk * 512, (blk + 1) * 512))
            for dc in range(8):
                s.mm(PS[0][:, :], win[:, dc, 0:128], xb[:, dc, :], start=(dc == 0), stop=(dc == 7))
            s.act(qTs[:], PS[0][:, :], AF.Copy, scale=float(128 ** -0.5))
            for dc in range(8):
                s.mm(PS[1][:, :], win[:, dc, 128:256], xb[:, dc, :], start=(dc == 0), stop=(dc == 7))
            s.cp("act", kTs[:], PS[1][:, :])
            for dc in range(8):
                s.mm(PS[3][0:16, :], win[:, dc, rcol:rcol + 16], xb[:, dc, :], start=(dc == 0), stop=(dc == 7))
            s.cp("act", rTs[:], PS[3][0:16, :])
            tiles = range(4) if d == 0 else range(3, -1, -1)
            for tl in tiles:
                tsl = slice(tl * 128, (tl + 1) * 128)
                tok0 = blk * 512 + tl * 128
                for dc in range(8):
                    s.mm(PS[2][:, 0:384], xb[:, dc, tsl], win[:, dc, 128:512], start=(dc == 0), stop=(dc == 7))
                s.cp("act", ktm[:], PS[2][:, 0:128])
                s.cp("act", vtm[:], PS[2][:, 128:384])
                s.mm(PS[2][:, 384:512], rTs[:, tsl], wgk[:, d, :])
                s.tt("dve", z[:], PS[2][:, 384:512], bgkB[:, d, :], ALU.add)
                s.act(z[:], z[:], AF.Exp, scale=-1.0)
                s.act(z[:], z[:], AF.Ln, bias=1.0)
                s.ts("dve", gtm[:], z[:], -1.0 / 16.0, ALU.mult)
                if d == 1:
                    for dc in range(8):
                        s.mm(PS[3][:, 0:256], xb[:, dc, tsl], win[:, dc, 512:768], start=(dc == 0), stop=(dc == 7))
                    s.act(sgate[:], PS[3][:, 0:256], AF.Silu)
                formA_tile(s, C, d, PS[4][:, 0:256], PS[5][:, 0:384], PS[6][:, 0:256],
                           qTs[:, tsl], kTs[:, tsl], ktm[:], gtm[:], vtm[:], V, St, W)
                out_stage(s, C, d, PS[6][:, 0:256], PS[7][:, :], D, tok0, 256, nh, V, sgate[:], normB[:], Wk)

MASKBIG = 30000.0


def make_masks_b(s, C):
    C["maskneg"] = []
    for d in range(2):
        m = s.sb(f"maskneg{d}", [128, 128], F32)
        s.ts("pool", m[:], C["U128"][d][:], -1.0, ALU.add, MASKBIG, ALU.mult)
        C["maskneg"].append(m)
    C["maskstrict"] = []
    for d in range(2):
        m = s.sb(f"maskstr{d}", [128, 128], F32)
        s.ts("pool", m[:], C["SU128"][d][:], -1.0, ALU.add, -MASKBIG, ALU.mult)
        C["maskstrict"].append(m)


def load_block_halo(s, xb, D, blk, NBLK, TB=512):
    lo, hi = blk * TB - 2, blk * TB + TB + 2
    a, b = 0, TB + 4
    if blk == 0:
        s.memset("pool", xb[:, :, 0:2], 0.0)
        lo, a = 0, 2
    if blk == NBLK - 1:
        s.memset("pool", xb[:, :, TB + 2:TB + 4], 0.0)
        hi, b = blk * TB + TB, TB + 2
    cur = lo
    while cur < hi:
        nxt = min(hi, (cur // 4096 + 1) * 4096)
        s.dma("sp", xb[:, :, a + (cur - lo):a + (nxt - lo)], xT_view(D, cur, nxt))
        cur = nxt


def conv_chunk(s, PSa, PSb, win, col0, xb, pre, acc, cw, cc, out, bias=None):
    for half, P in ((0, PSa), (1, PSb)):
        for dc in range(8):
            s.mm(P[:, 0:258], win[:, dc, col0:col0 + 128], xb[:, dc, half * 258:(half + 1) * 258], start=(dc == 0), stop=(dc == 7))
        s.cp("act", pre[:, half * 258:(half + 1) * 258], P[:, 0:258])
    s.ts("dve", acc[:], pre[:, 0:512], cw[:, cc, 0:1], ALU.mult)
    for k in range(1, 5):
        s.stt("dve", acc[:], pre[:, k:k + 512], cw[:, cc, k:k + 1], acc[:], ALU.mult, ALU.add)
    if bias is None:
        s.act(out, acc[:], AF.Silu)
    else:
        s.act(out, acc[:], AF.Silu, bias=bias)


def ssd_mixer(s, C, D, L, PS):
    nc = s.nc
    win = s.sb("win", [128, 8, 1296], BF16)
    s.dma("pool", win[:], D["w_in"].v(D["w_in"].t.rearrange("(c p) n -> p c n", p=128)))
    cw = s.sb("cw", [128, 6, 5], F32)
    s.dma("sp", cw[:], D["convw"][:, :, :])
    cbias = s.sb("cbias", [128, 6], F32)
    s.dma("sp", cbias[:], D["convb"][:, :])
    negA = s.sb("negA", [128, 2, 8], F32)
    s.dma("sp", negA[:], D["alog"].v(D["alog"].t.rearrange("(o z) r -> o z r", o=1).partition_broadcast(128)))
    s.act(negA[:], negA[:], AF.Exp)
    s.ts("dve", negA[:], negA[:], -1.0, ALU.mult)
    dtbB = s.sb("dtbB", [128, 2, 8], F32)
    s.dma("sp", dtbB[:], D["dtb"].v(D["dtb"].t.rearrange("(o z) r -> o z r", o=1).partition_broadcast(128)))
    dskB = s.sb("dskB", [128, 8], F32)
    s.dma("sp", dskB[:], D["dsk"].v(D["dsk"].t[0:1, :].partition_broadcast(128)))
    normB = s.sb("normB", [128, 512], F32)
    s.dma("sp", normB[:], D["normw"].v(D["normw"].t[0:1, :].partition_broadcast(128)))
    xTb = [s.sb(f"xTb{i}", [128, 8, 516], BF16) for i in range(2)]
    pre = s.sb("pre", [128, 516], F32)
    acc = s.sb("cacc", [128, 512], F32)
    xsT = [s.sb(f"xsT{c}", [128, 512], F32) for c in range(4)]
    BT = s.sb("BT", [128, 512], BF16)
    CT = s.sb("CT", [128, 512], BF16)
    dt = s.sb("dt", [128, 8], F32)
    g = s.sb("g", [128, 8], F32)
    gB = s.sb("gB", [128, 8, 128], F32)
    ones3 = s.sb("ones3", [128, 8, 128], F32)
    s.memset("pool", ones3[:], 1.0)
    E24 = s.sb("E24", [128, 24], F32)
    ngc = s.sb("ngc", [128, 8], F32)
    xs_tm = s.sb("xs_tm", [128, 8, 64], F32)
    u = s.sb("u", [128, 8, 64], F32)
    u_bf = s.sb("u_bf", [128, 512], BF16)
    udec = s.sb("udec", [128, 8, 64], BF16)
    B_tm = s.sb("B_tm", [128, 128], BF16)
    Er = [s.sb(f"Er{i}", [128, 128], F32) for i in range(2)]
    STr = [s.sb(f"STr{i}", [128, 128], BF16) for i in range(2)]
    tmpB = s.sb("tmpB", [128, 8, 64], F32)
    ysb = s.sb("ysb", [128, 512], F32)
    skip = s.sb("skip", [128, 8, 64], F32)
    sgate = s.sb("sgate", [128, 512], F32)
    Wk = out_work(s, 512)
    NBLK = L // 512
    idf = C["idf"]
    for d in range(2):
        S = s.sb(f"S{d}", [128, 8, 64], F32)
        Sb = [s.sb(f"Sb{d}{i}", [128, 512], BF16) for i in range(2)]
        s.memset("dve", S[:], 0.0)
        s.memset("dve", Sb[0][:], 0.0)
        si = 0
        U, SU, mneg = C["U128"][d], C["SU128"][d], C["maskneg"][d]
        dtcol = 1280 + 8 * d
        blocks = range(NBLK) if d == 0 else range(NBLK - 1, -1, -1)
        for bi, blk in enumerate(blocks):
            xb = xTb[bi % 2]
            load_block_halo(s, xb, D, blk, NBLK)
            for cc in range(6):
                out = xsT[cc][:] if cc < 4 else (BT[:] if cc == 4 else CT[:])
                conv_chunk(s, PS[0], PS[1], win, cc * 128, xb, pre, acc, cw, cc, out, bias=cbias[:, cc:cc + 1])
            tiles = range(4) if d == 0 else range(3, -1, -1)
            for tl in tiles:
                tsl = slice(tl * 128, (tl + 1) * 128)
                xsl = slice(2 + tl * 128, 2 + (tl + 1) * 128)
                tok0 = blk * 512 + tl * 128
                for dc in range(8):
                    s.mm(PS[3][:, 0:8], xb[:, dc, xsl], win[:, dc, dtcol:dtcol + 8], start=(dc == 0), stop=(dc == 7))
                s.tt("dve", dt[:], PS[3][:, 0:8], dtbB[:, d, :], ALU.add)
                s.act(dt[:], dt[:], AF.Exp)
                s.act(dt[:], dt[:], AF.Ln, bias=1.0)
                s.tt("dve", g[:], dt[:], negA[:, d, :], ALU.mult)
                s.tt("pool", gB[:], ones3[:], g.v(g.t[:, :].unsqueeze(2).to_broadcast([128, 8, 128])), ALU.mult)
                if d == 1:
                    for dc in range(8):
                        s.mm(PS[2][:, :], xb[:, dc, xsl], win[:, dc, 768:1280], start=(dc == 0), stop=(dc == 7))
                    s.act(sgate[:], PS[2][:, :], AF.Silu)
                for c in range(4):
                    s.tr(PS[4][:, c * 128:(c + 1) * 128], xsT[c][:, tsl], idf[:])
                s.cp("act", xs_tm.v(xs_tm.t[:, :, :].rearrange("p r q -> p (r q)")), PS[4][:, :])
                p5b = PS[5].v(PS[5].t[:, :].bitcast(BF16))
                s.tr(PS[5].v(p5b.ap[:, 768:896]), BT[:, tsl], C["idb"][:])
                s.cp("act", B_tm[:], PS[5].v(p5b.ap[:, 768:896]))
                dtb_ = dt.v(dt.t[:, :].unsqueeze(2).to_broadcast([128, 8, 64]))
                s.tt("dve", u[:], xs_tm[:], dtb_, ALU.mult)
                s.cp("pool", u_bf.v(u_bf.t[:, :].rearrange("p (r q) -> p r q", r=8)), u[:])
                s.mm(PS[3][:, 32:40], U[:], g[:])
                s.mm(PS[3][:, 40:48], SU[:], g[:])
                s.mm(PS[3][:, 48:56], C["ones"][:], g[:])
                s.act(E24[:], PS[3][:, 32:56], AF.Exp)
                s.act(ngc[:], PS[3][:, 32:40], AF.Copy, scale=-1.0)
                s.mm(PS[5][:, 0:128], BT[:, tsl], CT[:, tsl])
                for r in range(8):
                    Pr = PS[5][:, 128 + (r % 2) * 128:256 + (r % 2) * 128]
                    s.mm(Pr, gB[:, r, :], U[:], start=True, stop=False)
                    s.mm(Pr, idf[:], mneg[:], start=False, stop=True)
                    s.act(Er[r % 2][:], Pr, AF.Exp, bias=ngc[:, r:r + 1])
                    s.tt("dve", STr[r % 2][:], PS[5][:, 0:128], Er[r % 2][:], ALU.mult)
                    s.mm(PS[6][:, r * 64:(r + 1) * 64], STr[r % 2][:], u_bf[:, r * 64:(r + 1) * 64])
                cur = Sb[si % 2]
                s.mm(PS[7][:, :], CT[:, tsl], cur[:])
                s.cp("act", tmpB.v(tmpB.t[:, :, :].rearrange("p r q -> p (r q)")), PS[7][:, :])
                egc_b = E24.v(E24.t[:, 0:8].unsqueeze(2).to_broadcast([128, 8, 64]))
                s.tt("dve", tmpB[:], tmpB[:], egc_b, ALU.mult)
                s.tt("dve", ysb[:], tmpB.v(tmpB.t[:, :, :].rearrange("p r q -> p (r q)")), PS[6][:, :], ALU.add)
                edec_b = E24.v(E24.t[:, 8:16].unsqueeze(2).to_broadcast([128, 8, 64]))
                s.tt("pool", udec[:], u[:], edec_b, ALU.mult)
                s.mm(PS[0][:, :], B_tm[:], udec.v(udec.t[:, :, :].rearrange("p r q -> p (r q)")))
                egl_b = E24.v(E24.t[:, 16:24].unsqueeze(2).to_broadcast([128, 8, 64]))
                s.tt("dve", S[:], S[:], egl_b, ALU.mult)
                s.tt("dve", S.v(S.t[:, :, :].rearrange("p r q -> p (r q)")), S.v(S.t[:, :, :].rearrange("p r q -> p (r q)")), PS[0][:, :], ALU.add)
                si += 1
                s.cp("act", Sb[si % 2][:], S.v(S.t[:, :, :].rearrange("p r q -> p (r q)")))
                if d == 1:
                    dsk_b = dskB.v(dskB.t[:, :].unsqueeze(2).to_broadcast([128, 8, 64]))
                    s.tt("pool", skip[:], xs_tm[:], dsk_b, ALU.mult)
                    s.tt("pool", ysb[:], ysb[:], skip.v(skip.t[:, :, :].rearrange("p r q -> p (r q)")), ALU.add)
                out_stage(s, C, d, ysb[:], PS[1][:, :], D, tok0, 512, 1, 512, sgate[:], normB[:], Wk, pre_gate=True)


class Reg:
    def __init__(self, tile, a, b):
        self.tile, self.a, self.b = tile, a, b
        self.t = tile.t[:, a:b]

    def __getitem__(self, k):
        return View(self.tile, self.t[k])

    def v(self, ap):
        return View(self.tile, ap)


def gdn_mixer(s, C, D, L, PS):
    nc = s.nc
    idf, idb = C["idf"], C["idb"]
    win = s.sb("win", [128, 8, 1552], BF16)
    s.dma("pool", win[:], D["w_in"].v(D["w_in"].t.rearrange("(c p) n -> p c n", p=128)))
    cw = s.sb("cw", [128, 8, 5], F32)
    s.dma("sp", cw[:], D["convw"][:, :, :])
    negA = s.sb("negA", [128, 2, 4], F32)
    s.dma("sp", negA[:], D["alog"].v(D["alog"].t.rearrange("(o z) r -> o z r", o=1).partition_broadcast(128)))
    s.act(negA[:], negA[:], AF.Exp)
    s.ts("dve", negA[:], negA[:], -1.0, ALU.mult)
    dtbB = s.sb("dtbB", [128, 2, 4], F32)
    s.dma("sp", dtbB[:], D["dtb"].v(D["dtb"].t.rearrange("(o z) r -> o z r", o=1).partition_broadcast(128)))
    normB = s.sb("normB", [128, 4, 128], F32)
    for h in range(4):
        s.dma("sp", normB[:, h, :], D["normw"].v(D["normw"].t[0:1, :].partition_broadcast(128)))
    xTb = [s.sb(f"xTb{i}", [128, 8, 516], BF16) for i in range(2)]
    pre = s.sb("pre", [128, 516], F32)
    acc = s.sb("cacc", [128, 512], F32)
    qT = [s.sb(f"qT{c}", [128, 512], F32) for c in range(2)]
    kT = [s.sb(f"kT{c}", [128, 512], F32) for c in range(2)]
    vT = [s.sb(f"vT{c}", [128, 512], F32) for c in range(4)]
    qTb = [s.sb(f"qTb{c}", [128, 512], BF16) for c in range(2)]
    kTb = [s.sb(f"kTb{c}", [128, 512], BF16) for c in range(2)]
    sqt = s.sb("sqt", [128, 512], F32)
    rn = s.sb("rn", [128, 512], F32)
    ab = s.sb("ab", [128, 8], F32)
    g = s.sb("g", [128, 4], F32)
    beta = s.sb("beta", [128, 4], F32)
    nbeta = s.sb("nbeta", [128, 4], F32)
    bg = s.sb("bg", [128, 4], F32)
    gB = s.sb("gB", [128, 4, 128], F32)
    ones3 = s.sb("ones3", [128, 4, 128], F32)
    s.memset("pool", ones3[:], 1.0)
    E12 = s.sb("E12", [128, 12], F32)
    ngc = s.sb("ngc", [128, 4], F32)
    gcs = s.sb("gcs", [128, 4], F32)
    k_tm = s.sb("k_tm", [128, 2, 128], F32)
    v_tm = s.sb("v_tm", [128, 4, 128], F32)
    sgate = s.sb("sgate", [128, 512], F32)
    osb_all = s.sb("osb_all", [128, 512], F32)
    def dbl(name, shape, dt):
        return [s.sb(f"{name}{i}", shape, dt) for i in range(2)]
    Einc = dbl("Einc", [128, 128], F32); En = dbl("En", [128, 128], F32)
    attnT = dbl("attnT", [128, 128], BF16)
    Mb = [dbl(f"Mb{i}", [128, 128], BF16) for i in range(2)]
    MTb = [dbl(f"MTb{i}", [128, 128], BF16) for i in range(2)]
    P32 = dbl("P32", [128, 128], F32); Pbf = dbl("Pbf", [128, 128], BF16)
    kbg = dbl("kbg", [128, 128], BF16); vb = dbl("vb", [128, 128], BF16); kdec = dbl("kdec", [128, 128], BF16)
    u_sb = dbl("u_sb", [128, 128], F32); wTb = dbl("wTb", [128, 128], BF16)
    vnew = dbl("vnew", [128, 128], BF16); oAs = dbl("oAs", [128, 128], F32)
    def reg(bank, a, b, nm):
        return Reg(PS[bank], a, b)
    pG = [reg(5, 0, 128, "pG0"), reg(5, 128, 256, "pG1")]
    pQK = [reg(5, 256, 384, "pQK0"), reg(5, 384, 512, "pQK1")]
    pP1 = reg(6, 0, 128, "pP1"); pP2 = reg(6, 128, 256, "pP2")
    pTb = reg(7, 0, 128, "pTb"); pM = reg(7, 128, 256, "pM"); pMT = reg(7, 256, 384, "pMT")
    pX = reg(0, 0, 128, "pX"); pU = reg(0, 128, 256, "pU"); pW = reg(0, 256, 384, "pW")
    pV = reg(1, 0, 128, "pV"); pOB = reg(1, 128, 256, "pOB"); pOA = reg(1, 256, 384, "pOA"); pDS = reg(1, 384, 512, "pDS")
    Wk = out_work(s, 512)
    NBLK = L // 512
    for d in range(2):
        S = [s.sb(f"S{d}{h}", [128, 128], F32) for h in range(4)]
        Sb = [[s.sb(f"Sb{d}{h}{i}", [128, 128], BF16) for i in range(2)] for h in range(4)]
        si = [0] * 4
        for h in range(4):
            s.memset("dve", S[h][:], 0.0)
            s.memset("dve", Sb[h][0][:], 0.0)
        U, SU, mneg, mstr = C["U128"][d], C["SU128"][d], C["maskneg"][d], C["maskstrict"][d]
        abcol = 1536 + 8 * d
        blocks = range(NBLK) if d == 0 else range(NBLK - 1, -1, -1)
        for bi, blk in enumerate(blocks):
            xb = xTb[bi % 2]
            load_block_halo(s, xb, D, blk, NBLK)
            for cc in range(8):
                out = (qT[cc] if cc < 2 else kT[cc - 2] if cc < 4 else vT[cc - 4])[:]
                conv_chunk(s, PS[2], PS[3], win, cc * 128, xb, pre, acc, cw, cc, out)
            for (src, dstb, scl) in ((qT[0], qTb[0], float(128 ** -0.5)), (qT[1], qTb[1], float(128 ** -0.5)), (kT[0], kTb[0], 1.0), (kT[1], kTb[1], 1.0)):
                s.act(sqt[:], src[:], AF.Square)
                s.mm(PS[2][:, :], C["ones"][:], sqt[:])
                s.act(rn[:], PS[2][:, :], AF.Ln, bias=NORM_EPS)
                s.act(rn[:], rn[:], AF.Exp, scale=-0.5)
                s.stt("dve", src[:], src[:], scl, rn[:], ALU.mult, ALU.mult)
                s.cp("act", dstb[:], src[:])
            tiles = range(4) if d == 0 else range(3, -1, -1)
            for tl in tiles:
                tsl = slice(tl * 128, (tl + 1) * 128)
                xsl = slice(2 + tl * 128, 2 + (tl + 1) * 128)
                tok0 = blk * 512 + tl * 128
                for dc in range(8):
                    s.mm(PS[3][:, 0:8], xb[:, dc, xsl], win[:, dc, abcol:abcol + 8], start=(dc == 0), stop=(dc == 7))
                s.cp("act", ab[:], PS[3][:, 0:8])
                s.tt("dve", g[:], ab[:, 0:4], dtbB[:, d, :], ALU.add)
                s.act(g[:], g[:], AF.Exp)
                s.act(g[:], g[:], AF.Ln, bias=1.0)
                s.tt("dve", g[:], g[:], negA[:, d, :], ALU.mult)
                s.act(beta[:], ab[:, 4:8], AF.Exp, scale=-1.0)
                s.ts("dve", beta[:], beta[:], 1.0, ALU.add)
                s.op("dve", lambda: nc.vector.reciprocal(out=beta.t[:], in_=beta.t[:]), [beta[:]], [beta[:]])
                s.ts("dve", nbeta[:], beta[:], -1.0, ALU.mult)
                s.tt("pool", gB[:], ones3[:], g.v(g.t[:, :].unsqueeze(2).to_broadcast([128, 4, 128])), ALU.mult)
                if d == 1:
                    for dc in range(8):
                        s.mm(PS[2][:, :], xb[:, dc, xsl], win[:, dc, 1024:1536], start=(dc == 0), stop=(dc == 7))
                    s.act(sgate[:], PS[2][:, :], AF.Silu)
                for c in range(2):
                    s.tr(PS[3][:, 64 + c * 128:192 + c * 128], kT[c][:, tsl], idf[:])
                s.cp("act", k_tm.v(k_tm.t[:, :, :].rearrange("p c k -> p (c k)")), PS[3][:, 64:320])
                for c in range(4):
                    s.tr(PS[4][:, c * 128:(c + 1) * 128], vT[c][:, tsl], idf[:])
                s.cp("act", v_tm.v(v_tm.t[:, :, :].rearrange("p c k -> p (c k)")), PS[4][:, :])
                s.mm(PS[3][:, 32:36], U[:], g[:])
                s.mm(PS[3][:, 36:40], SU[:], g[:])
                s.mm(PS[3][:, 40:44], C["ones"][:], g[:])
                s.act(E12[:], PS[3][:, 32:44], AF.Exp)
                s.act(ngc[:], PS[3][:, 32:36], AF.Copy, scale=-1.0)
                s.act(gcs[:], PS[3][:, 32:36], AF.Copy)
                s.tt("dve", bg[:], beta[:], E12[:, 0:4], ALU.mult)
                for hq in range(2):
                    s.mm(pG[hq][:, :], kTb[hq][:, tsl], kTb[hq][:, tsl])
                    s.mm(pQK[hq][:, :], kTb[hq][:, tsl], qTb[hq][:, tsl])
                for hv in range(4):
                    hq, b = hv // 2, hv % 2
                    s.mm(pP1[:, :], gB[:, hv, :], U[:], start=True, stop=False)
                    s.mm(pP1[:, :], idf[:], mneg[:], start=False, stop=True)
                    s.act(Einc[b][:], pP1[:, :], AF.Exp, bias=ngc[:, hv:hv + 1])
                    s.tt("dve", attnT[b][:], pQK[hq][:, :], Einc[b][:], ALU.mult)
                    s.mm(pP2[:, :], gB[:, hv, :], U[:], start=True, stop=False)
                    s.mm(pP2[:, :], idf[:], mstr[:], start=False, stop=True)
                    s.act(En[b][:], pP2[:, :], AF.Exp, bias=gcs[:, hv:hv + 1], scale=-1.0)
                    M, MT = Mb[0][b], MTb[0][b]
                    s.stt("dve", M[:], En[b][:], nbeta[:, hv:hv + 1], pG[hq][:, :], ALU.mult, ALU.mult)
                    ptb = pTb.v(pTb.t[:, :].bitcast(BF16))
                    s.tr(pTb.v(ptb.ap[:, 0:128]), M[:], idb[:])
                    s.cp("act", MT[:], pTb.v(ptb.ap[:, 0:128]))
                    s.tt("dve", P32[b][:], idf[:], MT[:], ALU.add)
                    s.cp("pool", Pbf[b][:], P32[b][:])
                    for l in range(1, 7):
                        Mn, MTn = Mb[l % 2][b], MTb[l % 2][b]
                        s.mm(pM[:, :], MT[:], M[:])
                        s.cp("act", Mn[:], pM[:, :])
                        if l < 6:
                            s.mm(pMT[:, :], M[:], MT[:])
                            s.cp("act", MTn[:], pMT[:, :])
                        s.mm(pX[:, :], Mn[:], Pbf[b][:])
                        s.tt("dve", P32[b][:], P32[b][:], pX[:, :], ALU.add)
                        s.cp("pool", Pbf[b][:], P32[b][:])
                        M, MT = Mn, MTn
                    s.ts("pool", vb[b][:], v_tm[:, hv, :], beta[:, hv:hv + 1], ALU.mult)
                    s.ts("pool", kbg[b][:], k_tm[:, hq, :], bg[:, hv:hv + 1], ALU.mult)
                    s.ts("pool", kdec[b][:], k_tm[:, hq, :], E12[:, 4 + hv:5 + hv], ALU.mult)
                    s.mm(pU[:, :], Pbf[b][:], vb[b][:])
                    s.cp("act", u_sb[b][:], pU[:, :])
                    s.mm(pW[:, :], kbg[b][:], Pbf[b][:])
                    s.cp("act", wTb[b][:], pW[:, :])
                    cur = Sb[hv][si[hv] % 2]
                    s.mm(pV[:, :], wTb[b][:], cur[:])
                    s.tt("dve", vnew[b][:], u_sb[b][:], pV[:, :], ALU.subtract)
                    s.mm(pOB[:, :], qTb[hq][:, tsl], cur[:])
                    s.mm(pOA[:, :], attnT[b][:], vnew[b][:])
                    s.cp("act", oAs[b][:], pOA[:, :])
                    s.stt("dve", osb_all[:, hv * 128:(hv + 1) * 128], pOB[:, :], E12[:, hv:hv + 1], oAs[b][:], ALU.mult, ALU.add)
                    s.mm(pDS[:, :], kdec[b][:], vnew[b][:])
                    s.stt("dve", S[hv][:], S[hv][:], E12[:, 8 + hv:9 + hv], pDS[:, :], ALU.mult, ALU.add)
                    si[hv] += 1
                    s.cp("act", Sb[hv][si[hv] % 2][:], S[hv][:])
                out_stage(s, C, d, osb_all[:], PS[4][:, :], D, tok0, 512, 4, 128, sgate[:],
                          normB.v(normB.t[:, :, :].rearrange("p h v -> p (h v)")), Wk)


def PS1_to_sb(s, P, scratch):
    return P[:, :]
import ml_dtypes
from concourse.bass_utils import run_bass_kernel_spmd

NCORE = 8
_DEBUG_HOOK = None
SEQ = 16384
NT = 4096
GT_TOK = 512
BF = ml_dtypes.bfloat16


def _run(build, in_maps, outs):
    nc = bass.Bass("TRN2", target_bir_lowering=False)
    s = Sched(nc)
    D = {}
    for k, v in in_maps[0].items():
        dt = BF16 if v.dtype == BF else F32
        D[k] = s.dram(nc.dram_tensor(k, list(v.shape), dt, kind="ExternalInput").ap(), k)
    for k, (shape, dt) in outs.items():
        D[k] = s.dram(nc.dram_tensor(k, list(shape), dt, kind="ExternalOutput").ap(), k)
    build(s, nc, D)
    s.finish([D[k] for k in outs])
    res = run_bass_kernel_spmd(nc, in_maps, core_ids=list(range(len(in_maps))))
    return res.results


def prep_phase(s, nc, D, C, PS, nt):
    idf = C["idf"]
    xt = [s.sb(f"pxt{i}", [128, 1024], F32) for i in range(2)]
    xoT = [s.sb(f"pxoT{i}", [128, 8, 128], BF16) for i in range(2)]
    for tl in range(nt // 128):
        tok0 = tl * 128
        xc = xt[tl % 2]
        s.dma("sp", xc[:], D["xres"][tok0:tok0 + 128, :])
        for dc in range(8):
            s.tr(PS[dc // 4][:, (dc % 4) * 128:(dc % 4 + 1) * 128], xc[:, dc * 128:(dc + 1) * 128], idf[:])
        xo = xoT[tl % 2]
        for hh in range(2):
            s.cp("act" if hh == 0 else "dve", xo[:, hh * 4:(hh + 1) * 4, :], PS[hh].v(PS[hh].t[:, :].rearrange("p (c t) -> p c t", c=4)))
        s.dma("sp", D["xTout"].v(D["xTout"].t[:, tok0:tok0 + 128].rearrange("(c p) t -> p c t", p=128)), xo[:])


def _c(a):
    return np.ascontiguousarray(a)


def _mixer_inputs(layer, inp, xT_seq):
    maps = []
    for c in range(NCORE):
        sq, q = c // 4, c % 4
        m = {"xT": xT_seq[sq]}
        if layer == 0:
            w = inp["gdn_w_in"][0]
            qc = np.arange(q * 256, (q + 1) * 256); kc = 1024 + qc
            vc = 2048 + np.arange(q * 512, (q + 1) * 512); zc = 4096 + np.arange(q * 512, (q + 1) * 512)
            hv = np.arange(q * 4, q * 4 + 4)
            abc = 6144 + np.concatenate([hv, 32 + hv, 16 + hv, 48 + hv])
            cols = np.concatenate([qc, kc, vc, zc, abc]); ccols = np.concatenate([qc, kc, vc])
            m.update({"w_in": _c(w[:, cols]),
                      "convw": _c(inp["gdn_conv_w"][0][:, ccols].reshape(5, 8, 128).transpose(2, 1, 0)),
                      "alog": _c(inp["gdn_a_log"][0][:, hv]), "dtb": _c(inp["gdn_dt_bias"][0][:, hv]),
                      "normw": _c(inp["gdn_norm_w"][0][None])})
        elif layer == 1:
            w = inp["m2_w_in"][0]; g_ = q; x0 = 2048
            cols = np.concatenate([np.arange(x0 + g_ * 512, x0 + (g_ + 1) * 512), np.arange(x0 + 2048 + g_ * 128, x0 + 2048 + (g_ + 1) * 128),
                                   np.arange(x0 + 2560 + g_ * 128, x0 + 2560 + (g_ + 1) * 128), np.arange(g_ * 512, (g_ + 1) * 512),
                                   np.arange(5120 + g_ * 8, 5120 + g_ * 8 + 8), np.arange(5152 + g_ * 8, 5152 + g_ * 8 + 8)])
            ccols = np.concatenate([np.arange(g_ * 512, (g_ + 1) * 512), np.arange(2048 + g_ * 128, 2048 + (g_ + 1) * 128),
                                    np.arange(2560 + g_ * 128, 2560 + (g_ + 1) * 128)])
            m.update({"w_in": _c(w[:, cols]),
                      "convw": _c(inp["m2_conv_w"][0][:, ccols].reshape(5, 6, 128).transpose(2, 1, 0)),
                      "convb": _c(inp["m2_conv_b"][0][ccols].reshape(6, 128).T),
                      "alog": _c(inp["m2_a_log"][0][:, g_ * 8:(g_ + 1) * 8]), "dtb": _c(inp["m2_dt_bias"][0][:, g_ * 8:(g_ + 1) * 8]),
                      "dsk": _c(inp["m2_d"][0][None, g_ * 8:(g_ + 1) * 8]), "normw": _c(inp["m2_norm_w"][0][None, g_ * 512:(g_ + 1) * 512])})
        elif layer == 2:
            w = inp["hg_w_in"][0]; cs = slice(q * 256, (q + 1) * 256)
            lg = inp["hg_lb_logits"][:, cs]
            m.update({"w_in": _c(np.concatenate([w[:, k * 1024:(k + 1) * 1024][:, cs] for k in range(5)], axis=1)),
                      "lbrow": _c(lg[None]), "lbcol": _c(lg.reshape(4, 2, 128).transpose(2, 0, 1)),
                      "normw": _c(inp["hg_norm_w"][0][None])})
        else:
            w = inp["gla_w_in"][0]; ks = slice(q * 128, (q + 1) * 128); vs = slice(q * 256, (q + 1) * 256)
            m.update({"w_in": _c(np.concatenate([w[:, 0:512][:, ks], w[:, 512:1024][:, ks], w[:, 1024:2048][:, vs],
                                                 w[:, 2048:3072][:, vs], w[:, 3072:3104]], axis=1)),
                      "wgk": _c(inp["gla_w_gk"][0][:, :, ks]), "bgk": _c(inp["gla_b_gk"][0][:, ks]),
                      "normw": _c(inp["gla_norm_w"][0][None])})
        maps.append(m)
    return maps


MIX_W = [2048, 2048, 1024, 1024]
MIX_WOUT = ["gdn_w_out", "m2_w_out", "hg_w_out", "gla_w_out"]


def _token_inputs(layer, inp, oT_tok, xres):
    maps = []
    p = inp["p"][layer].reshape(2 * SEQ, 256)
    wrt = _c(np.concatenate([inp["moe_w_group"][layer], inp["moe_w_router"][layer]], axis=1).reshape(8, 128, 36).transpose(1, 0, 2))
    brt = _c(np.concatenate([inp["moe_b_group"][layer], inp["moe_b_router"][layer]])[None])
    shared = {"w_out": _c(inp[MIX_WOUT[layer]][0]), "lng": _c(inp["ln_g"][layer]), "lnb": _c(inp["ln_b"][layer]),
              "wrt": wrt, "brt": brt,
              "w_gate": _c(inp["moe_w_gate"][layer].reshape(32, 1024, 256)), "w_up": _c(inp["moe_w_up"][layer].reshape(32, 1024, 256)),
              "w_down": _c(inp["moe_w_down"][layer].reshape(32, 256, 1024)),
              "pe_g": _c(inp["pe_w_gate"][layer]), "pe_p": _c(inp["pe_w_proj"][layer])}
    for c in range(NCORE):
        m = {"oT": oT_tok[c], "xres": xres[c], "pl": _c(p[c * NT:(c + 1) * NT])}
        m.update(shared)
        maps.append(m)
    return maps


def kernel_unfused(**inp):
    inp = {k: np.asarray(v) for k, v in inp.items()}
    x = inp["x"].reshape(2 * SEQ, 1024)
    xres = [_c(x[c * NT:(c + 1) * NT]) for c in range(NCORE)]

    def b_prep(s, nc, D):
        C = make_consts(s)
        PS = [s.ps(f"ps{i}", [128, 512], F32) for i in range(8)]
        prep_phase(s, nc, D, C, PS, NT)
    r = _run(b_prep, [{"xres": xres[c]} for c in range(NCORE)], {"xTout": ([1024, NT], BF16)})
    xT = [np.asarray(r[c]["xTout"]) for c in range(NCORE)]

    for layer in range(4):
        xT_seq = [_c(np.concatenate(xT[4 * sq:4 * sq + 4], axis=1)) for sq in range(2)]
        Wq = MIX_W[layer] // 4

        def b_mix(s, nc, D, layer=layer, Wq=Wq):
            C = make_consts(s)
            make_masks(s, C)
            PS = [s.ps(f"ps{i}", [128, 512], F32) for i in range(8)]
            D["of"] = s.dram(nc.dram_tensor("of", [SEQ, Wq], F32, kind="Internal").ap(), "of")
            if layer == 0:
                make_masks_b(s, C); gdn_mixer(s, C, D, SEQ, PS)
            elif layer == 1:
                make_masks_b(s, C); ssd_mixer(s, C, D, SEQ, PS)
            elif layer == 2:
                hg_mixer(s, C, D, SEQ, PS)
            else:
                gla_mixer(s, C, D, SEQ, PS)
        r = _run(b_mix, _mixer_inputs(layer, inp, xT_seq), {"oT": ([Wq, SEQ], BF16)})
        oT = [np.asarray(r[c]["oT"]) for c in range(NCORE)]
        oT_tok = []
        for c in range(NCORE):
            sq, q = c // 4, c % 4
            oT_tok.append(_c(np.concatenate([oT[4 * sq + qq][:, q * NT:(q + 1) * NT] for qq in range(4)], axis=0)))
        last = layer == 3

        def b_tok(s, nc, D, layer=layer, last=last):
            C = make_consts(s)
            D["x1res"] = s.dram(nc.dram_tensor("x1res", [NT, 1024], F32, kind="Internal").ap(), "x1res")
            token_phase(s, C, D, NT, GT_TOK, MIX_W[layer], last)
        outs = {"xout": ([NT, 1024], F32)}
        if not last:
            outs["xTout"] = ([1024, NT], BF16)
        r = _run(b_tok, _token_inputs(layer, inp, oT_tok, xres), outs)
        xres = [np.asarray(r[c]["xout"]) for c in range(NCORE)]
        if _DEBUG_HOOK is not None:
            _DEBUG_HOOK(layer, xres, oT)
        if not last:
            xT = [np.asarray(r[c]["xTout"]) for c in range(NCORE)]
    out = np.concatenate(xres, axis=0).reshape(2, SEQ, 1024).astype(np.float32)
    return out

GROUPS = [[0, 1, 2, 3], [4, 5, 6, 7]]


def zero_dram(s, dtile, zt):
    n = 1
    for d in dtile.t.shape:
        n *= d
    k = n // (128 * 8192)
    names = " ".join(f"d{i}" for i in range(len(dtile.t.shape)))
    flat = dtile.t.rearrange(f"{names} -> ({names})").rearrange("(k p n) -> k p n", p=128, n=8192)
    for i in range(k):
        s.dma("sp", dtile.v(flat[i]), zt[:, :])


def build_fused(s, nc, D, Lz):
    C = make_consts(s)
    make_masks(s, C)
    make_masks_b(s, C)
    PS = [s.ps(f"ps{i}", [128, 512], F32) for i in range(8)]
    rv = nc.sync.partition_id() % 4

    def dr(name, shape, dt):
        return s.dram(nc.dram_tensor(name, list(shape), dt).ap(), name)
    xTm = dr("xTm", [1024, NT], BF16)
    obx = dr("obx", [4, 1024, NT], BF16)
    x1res = dr("x1res", [NT, 1024], F32)
    xres_pp = [dr("xresA", [NT, 1024], F32), dr("xresB", [NT, 1024], F32)]
    CC_BYTES = 4 * 1024 * 1024

    def make_exch(name, R, N):
        Rc = CC_BYTES // (4 * N * 2)
        nch = R // Rc
        return {"Rc": Rc, "n": nch, "ib": [dr(f"{name}_ib{c}", [4, Rc, N], BF16) for c in range(nch)],
                "ob": [dr(f"{name}_ob{c}", [4, Rc, N], BF16) for c in range(nch)]}
    EX = {"x": make_exch("ex", 1024, NT), 512: make_exch("eo512", 512, SEQ), 256: make_exch("eo256", 256, SEQ)}
    mark = s.mark()
    s.start_phases()
    zt = s.sb("zt", [128, 8192], BF16)
    s.memset("pool", zt[:], 0.0)
    for e in EX.values():
        for t in e["ib"]:
            zero_dram(s, t, zt)
    s.reset_to(mark)

    def exchange(e, src):
        Rc = e["Rc"]
        for c in range(e["n"]):
            ib, ob = e["ib"][c], e["ob"][c]
            s.dma("sp", ib.v(ib.t[bass.ds(rv, 1), :, :]), src.v(src.t[c * Rc:(c + 1) * Rc, :].rearrange("(o r) n -> o r n", o=1)))
            s.allreduce(ob[:, :, :], ib[:, :, :], GROUPS)

    def exchange_x():
        e = EX["x"]
        exchange(e, xTm)
        for c in range(e["n"]):
            s.dma("sp", obx[:, c * e["Rc"]:(c + 1) * e["Rc"], :], e["ob"][c][:, :, :])

    prep_phase(s, nc, {"xres": D["xres"], "xTout": xTm}, C, PS, NT)
    exchange_x()
    if "dbg_obx" in D:
        s.dma("sp", D["dbg_obx"][:, :, :], obx[:, :, :])
    s.reset_to(mark)
    cur_x = D["xres"]
    for layer in range(4):
        W = MIX_W[layer]
        Wq = W // 4
        last = layer == 3
        oTloc = dr(f"oTloc{layer}", [Wq, SEQ], BF16)
        of = dr(f"of{layer}", [SEQ, Wq], F32)
        oTtok = dr(f"oTtok{layer}", [W, NT], BF16)
        Dm = {k[len(f"M{layer}_"):]: v for k, v in D.items() if k.startswith(f"M{layer}_")}
        Dm.update({"xT": obx, "of": of, "oT": oTloc})
        [gdn_mixer, ssd_mixer, hg_mixer, gla_mixer][layer](s, C, Dm, SEQ, PS)
        e = EX[Wq]
        exchange(e, oTloc)
        ot3 = oTtok.t.rearrange("(q w) t -> q w t", q=4)
        for c in range(e["n"]):
            s.dma("sp", oTtok.v(ot3[:, c * e["Rc"]:(c + 1) * e["Rc"], :]), e["ob"][c].v(e["ob"][c].t[:, :, bass.ds(rv * NT, NT)]))
        if layer == 1 and "dbg_oTtok0" in D:
            s.dma("sp", D["dbg_oTtok0"][:, :], oTtok[:, :])
            s.dma("sp", D["dbg_oTloc0"][:, :], oTloc[:, :])
        s.reset_to(mark)
        Dt = {k[len(f"T{layer}_"):]: v for k, v in D.items() if k.startswith(f"T{layer}_")}
        nxt = D["out"] if last else xres_pp[layer % 2]
        Dt.update({"oT": oTtok, "xres": cur_x, "x1res": x1res, "xout": nxt, "xTout": xTm})
        token_phase(s, C, Dt, NT, GT_TOK, W, last, PS=PS)
        cur_x = nxt
        if layer == 1 and "dbg_x0" in D:
            s.dma("sp", D["dbg_x0"][:, :], nxt[:, :])
        if not last:
            exchange_x()
        s.reset_to(mark)


def kernel(**inp):
    inp = {k: np.asarray(v) for k, v in inp.items()}
    x = inp["x"].reshape(2 * SEQ, 1024)
    maps = [{"xres": _c(x[c * NT:(c + 1) * NT])} for c in range(NCORE)]
    for layer in range(4):
        mm = _mixer_inputs(layer, inp, [None, None])
        tm = _token_inputs(layer, inp, [None] * NCORE, [None] * NCORE)
        for c in range(NCORE):
            for k, v in mm[c].items():
                if k != "xT":
                    maps[c][f"M{layer}_{k}"] = v
            for k, v in tm[c].items():
                if k not in ("oT", "xres"):
                    maps[c][f"T{layer}_{k}"] = v

    def b(s, nc, D):
        build_fused(s, nc, D, None)
    outs = {"out": ([NT, 1024], F32)}
    if _DEBUG_HOOK is not None:
        outs.update({"dbg_obx": ([4, 1024, NT], BF16), "dbg_oTtok0": ([2048, NT], BF16), "dbg_oTloc0": ([512, SEQ], BF16), "dbg_x0": ([NT, 1024], F32)})
    r = _run(b, maps, outs)
    if _DEBUG_HOOK is not None:
        _DEBUG_HOOK(r)
    out = np.concatenate([np.asarray(r[c]["out"]) for c in range(NCORE)], axis=0).reshape(2, SEQ, 1024).astype(np.float32)
    return out
```

```python
import numpy as np
import concourse.bass as bass
import concourse.mybir as mybir

F32 = mybir.dt.float32
BF16 = mybir.dt.bfloat16
AF = mybir.ActivationFunctionType
ALU = mybir.AluOpType
AX = mybir.AxisListType


class View:
    __slots__ = ("tile", "ap")

    def __init__(self, tile, ap):
        self.tile = tile
        self.ap = ap


class Tile:
    def __init__(self, sch, tensor, name):
        self.sch = sch
        self.t = tensor
        self.name = name
        self.w = []
        self.r = []
        self.dsem = None
        self.dcnt = 0
        self.is_dram = False
        self.is_psum = False

    def __getitem__(self, k):
        return View(self, self.t[k])

    def v(self, ap):
        return View(self, ap)


class Sched:
    def __init__(self, nc):
        self.nc = nc
        self.E = {"pe": nc.tensor, "act": nc.scalar, "dve": nc.vector,
                  "pool": nc.gpsimd, "sp": nc.sync}
        self.sem = {k: nc.alloc_semaphore("sem_" + k) for k in ("pe", "act", "dve", "pool")}
        self.cnt = {k: 0 for k in self.sem}
        self.semid = {id(v): k for k, v in self.sem.items()}
        self.seen = {k: {} for k in self.E}
        self.nsem = 4
        self.ntile = 0
        self.last_out_tokens = []

    def sb(self, name, shape, dt):
        self.ntile += 1
        if not hasattr(self, "off"):
            self.off = 16384
            self.cap = 228800
        esz = 4 if dt == F32 else 2
        n = 1
        for d in shape[1:]:
            n *= d
        nbytes = (n * esz + 63) // 64 * 64
        if self.off + nbytes > self.cap:
            raise RuntimeError(f"SBUF overflow allocating {name}: {self.off}+{nbytes}")
        t = self.nc.alloc_sbuf_tensor_at(f"{name}_{self.ntile}", list(shape), dt, offset=self.off)
        self.off += nbytes
        return Tile(self, t, name)

    def mark(self):
        return getattr(self, "off", 16384)

    def reset_to(self, off):
        self.barrier()
        self.off = off
        ph = getattr(self, "phase", 0)
        keep = []
        for t in getattr(self, "_dsems", []):
            if getattr(t, "phase", 0) == ph and ph != 0:
                self._free_dsems.append((t.dsem, t.dcnt))
                t.dsem = None
            else:
                keep.append(t)
        self._dsems = keep
        self._recycled = getattr(self, "_recycled", []) + [x for x in self._free_dsems]
        self.phase = ph + 1

    def start_phases(self):
        self.phase = 1

    def barrier(self):
        dsems = getattr(self, "_dsems", [])
        for k in self.sem:
            pass
        for eng, e in self.E.items():
            seen = self.seen[eng]
            for k, sm in self.sem.items():
                if self.cnt[k] > 0 and seen.get(id(sm), 0) < self.cnt[k]:
                    e.wait_ge(sm, self.cnt[k]); seen[id(sm)] = self.cnt[k]
            for t in dsems:
                if t.dcnt > 0 and seen.get(id(t.dsem), 0) < t.dcnt:
                    e.wait_ge(t.dsem, t.dcnt); seen[id(t.dsem)] = t.dcnt
            for (sm, c) in getattr(self, "_free_dsems", []):
                if c > 0 and seen.get(id(sm), 0) < c:
                    e.wait_ge(sm, c); seen[id(sm)] = c
            if getattr(self, "ccsem", None) is not None and self.cccnt > 0 and seen.get(id(self.ccsem), 0) < self.cccnt:
                e.wait_ge(self.ccsem, self.cccnt); seen[id(self.ccsem)] = self.cccnt

    def ps(self, name, shape, dt):
        self.ntile += 1
        t = Tile(self, self.nc.alloc_psum_tensor(f"{name}_{self.ntile}", list(shape), dt), name)
        t.is_psum = True
        return t

    def dram(self, ap, name):
        t = Tile(self, ap, name)
        t.is_dram = True
        return t

    def _dsem(self, tile):
        if tile.dsem is None:
            if not hasattr(self, "_dsems"):
                self._dsems = []
                self._free_dsems = []
            if self._free_dsems:
                tile.dsem, tile.dcnt = self._free_dsems.pop()
            else:
                tile.dsem = self.nc.alloc_semaphore(f"d_{tile.name}_{self.nsem}")
                self.nsem += 1
                tile.dcnt = 0
            tile.phase = getattr(self, "phase", 0)
            self._dsems.append(tile)
        return tile.dsem

    def _waits(self, eng, outs, ins):
        need = {}
        own = self.sem.get(eng)
        for v in ins:
            for (s, val) in v.tile.w:
                need[id(s)] = (s, max(val, need.get(id(s), (s, 0))[1]))
            if v.tile.is_psum:
                for (s, val) in v.tile.r:
                    if s is not own:
                        need[id(s)] = (s, max(val, need.get(id(s), (s, 0))[1]))
        for v in outs:
            for (s, val) in v.tile.w + v.tile.r:
                need[id(s)] = (s, max(val, need.get(id(s), (s, 0))[1]))
        e = self.E[eng]
        seen = self.seen[eng]
        for sid, (s, val) in need.items():
            k = self.semid.get(sid)
            if k is not None and val > self.cnt[k]:
                if own is not None and s is own:
                    continue
                raise RuntimeError(f"wait on future inc of {k}: {val} > {self.cnt[k]}")
            if seen.get(sid, 0) < val:
                e.wait_ge(s, val)
                seen[sid] = val

    def _commit(self, outs, ins, tok):
        for v in ins:
            v.tile.r.append(tok)
            if len(v.tile.r) > 12:
                d = {}
                for (s, val) in v.tile.r:
                    d[id(s)] = (s, max(val, d.get(id(s), (s, 0))[1]))
                v.tile.r = list(d.values())
        for v in outs:
            if v.tile.is_dram:
                d = {}
                for (s_, val) in v.tile.w + [tok]:
                    d[id(s_)] = (s_, max(val, d.get(id(s_), (s_, 0))[1]))
                v.tile.w = list(d.values())
            else:
                v.tile.w = [tok]
            v.tile.r = []

    def op(self, eng, fn, outs, ins, inc=True):
        import os
        b = os.environ.get("OPBUDGET")
        self.nops = getattr(self, "nops", 0) + 1
        if b is not None and self.nops > int(b):
            return None
        self._waits(eng, outs, ins)
        ins_ = fn()
        s = self.sem[eng]
        if inc:
            ins_.then_inc(s, 1)
            self.cnt[eng] += 1
            tok = (s, self.cnt[eng])
        else:
            tok = (s, self.cnt[eng] + 1)
        self._commit(outs, ins, tok)
        return ins_

    def dma(self, q, out, in_, sbuf_side=None):
        self._waits(q, [out], [in_])
        st = sbuf_side if sbuf_side is not None else (in_.tile if out.tile.is_dram else out.tile)
        s = self._dsem(st)
        ins_ = self.E[q].dma_start(out=out.ap, in_=in_.ap)
        ins_.then_inc(s, 16)
        st.dcnt += 16
        tok = (s, st.dcnt)
        self._commit([out], [in_], tok)
        return tok

    def mm(self, out, lhsT, rhs, start=True, stop=True, inc=None):
        if inc is None:
            inc = stop
        return self.op("pe", lambda: self.nc.tensor.matmul(out.ap, lhsT=lhsT.ap, rhs=rhs.ap, start=start, stop=stop),
                       [out], [lhsT, rhs], inc=inc)

    def tr(self, out, in_, ident, inc=True):
        return self.op("pe", lambda: self.nc.tensor.transpose(out=out.ap, in_=in_.ap, identity=ident.ap),
                       [out], [in_, ident], inc=inc)

    def act(self, out, in_, func, bias=None, scale=None, accum=None):
        ins = [in_]
        kw = {}
        if bias is not None:
            if isinstance(bias, View):
                ins.append(bias); kw["bias"] = bias.ap
            else:
                kw["bias"] = bias
        if scale is not None:
            if isinstance(scale, View):
                ins.append(scale); kw["scale"] = scale.ap
            else:
                kw["scale"] = scale
        outs = [out]
        if accum is not None:
            outs.append(accum); kw["accum_out"] = accum.ap
        return self.op("act", lambda: self.nc.scalar.activation(out=out.ap, in_=in_.ap, func=func, **kw), outs, ins)

    def _e(self, eng):
        return self.E[eng]

    def tt(self, eng, out, a, b, op):
        return self.op(eng, lambda: self._e(eng).tensor_tensor(out=out.ap, in0=a.ap, in1=b.ap, op=op), [out], [a, b])

    def ts(self, eng, out, a, s1, op0, s2=None, op1=None):
        ins = [a]
        a1 = s1.ap if isinstance(s1, View) else s1
        a2 = s2.ap if isinstance(s2, View) else s2
        if isinstance(s1, View): ins.append(s1)
        if isinstance(s2, View): ins.append(s2)
        if op1 is None:
            return self.op(eng, lambda: self._e(eng).tensor_scalar(out=out.ap, in0=a.ap, scalar1=a1, scalar2=None, op0=op0), [out], ins)
        return self.op(eng, lambda: self._e(eng).tensor_scalar(out=out.ap, in0=a.ap, scalar1=a1, scalar2=a2, op0=op0, op1=op1), [out], ins)

    def stt(self, eng, out, a, sc, b, op0, op1):
        ins = [a, b]
        s_ = sc.ap if isinstance(sc, View) else sc
        if isinstance(sc, View): ins.append(sc)
        return self.op(eng, lambda: self._e(eng).scalar_tensor_tensor(out=out.ap, in0=a.ap, scalar=s_, in1=b.ap, op0=op0, op1=op1), [out], ins)

    def cp(self, eng, out, a):
        if eng == "act":
            return self.op("act", lambda: self.nc.scalar.copy(out=out.ap, in_=a.ap), [out], [a])
        return self.op(eng, lambda: self._e(eng).tensor_copy(out=out.ap, in_=a.ap), [out], [a])

    def memset(self, eng, out, val):
        return self.op(eng, lambda: self._e(eng).memset(out.ap, val), [out], [])

    def allreduce(self, out, in_, groups):
        if getattr(self, "ccsem", None) is None:
            self.ccsem = self.nc.alloc_semaphore("ccsem")
            self.cccnt = 0
        self._waits("pool", [out], [in_])
        ins_ = self.nc.gpsimd.collective_compute("AllReduce", ALU.add, replica_groups=groups, ins=[in_.ap], outs=[out.ap])
        ins_.then_inc(self.ccsem)
        self.cccnt += 1
        tok = (self.ccsem, self.cccnt)
        self._commit([out], [in_], tok)

    def finish(self, dram_tiles):
        e = self.E["sp"]
        for t in dram_tiles:
            for (s, val) in t.w:
                e.wait_ge(s, val)

ALPHA = float((2 * 4) ** 0.25)
LN_EPS = 1e-5
DM = 1024
NEG_BIG = -30000.0


def make_consts(s):
    nc = s.nc
    C = {}
    idf = s.sb("identf", [128, 128], F32)
    s.memset("pool", idf[:], 1.0)
    s.op("pool", lambda: nc.gpsimd.affine_select(out=idf.t[:], in_=idf.t[:], pattern=[[-1, 128]],
                                                 compare_op=ALU.is_equal, fill=0.0, base=0, channel_multiplier=1),
         [idf[:]], [idf[:]])
    idb = s.sb("identb", [128, 128], BF16)
    s.cp("pool", idb[:], idf[:])
    C["idf"] = idf
    C["idb"] = idb
    return C


def layernorm_tile(s, y, xn, out, gam, bet, st6, mv, rstd, nmr):
    nc = s.nc
    for hf in range(2):
        s.op("dve", lambda hf=hf: nc.vector.bn_stats(out=st6.t[:, hf * 6:(hf + 1) * 6], in_=y.t[:, hf * 512:(hf + 1) * 512]),
             [st6[:]], [y[:]])
    s.op("dve", lambda: nc.vector.bn_aggr(out=mv.t[:], in_=st6.t[:]), [mv[:]], [st6[:]])
    s.act(rstd[:], mv[:, 1:2], AF.Ln, bias=LN_EPS)
    s.act(rstd[:], rstd[:], AF.Exp, scale=-0.5)
    s.stt("dve", nmr[:], mv[:, 0:1], -1.0, rstd[:], ALU.mult, ALU.mult)
    s.act(xn[:], y[:], AF.Identity, bias=nmr[:], scale=rstd[:])
    s.tt("pool", xn[:], xn[:], gam[:], ALU.mult)
    s.tt("pool", out[:], xn[:], bet[:], ALU.add)


def token_phase(s, C, D, NT, GT, W, last, upto=9, PS=None):
    nc = s.nc
    WC = W // 128
    idf, idb = C["idf"], C["idb"]
    lng = [s.sb(f"lng{i}", [128, DM], F32) for i in range(2)]
    lnb = [s.sb(f"lnb{i}", [128, DM], F32) for i in range(2)]
    for i in range(2):
        s.dma("sp", lng[i][:], D["lng"].v(D["lng"].t[i:i + 1, :].partition_broadcast(128)))
        s.dma("sp", lnb[i][:], D["lnb"].v(D["lnb"].t[i:i + 1, :].partition_broadcast(128)))
    wr = s.sb("wr", [128, 8, 36], F32)
    s.dma("sp", wr[:], D["wrt"][:, :, :])
    wrh = s.sb("wrh", [128, 8, 36], BF16)
    wrl = s.sb("wrl", [128, 8, 36], BF16)
    wrt_ = s.sb("wrt_", [128, 8, 36], F32)
    s.cp("dve", wrh[:], wr[:])
    s.tt("dve", wrt_[:], wr[:], wrh[:], ALU.subtract)
    s.cp("dve", wrl[:], wrt_[:])
    brt = s.sb("brt", [128, 36], F32)
    s.dma("sp", brt[:], D["brt"].v(D["brt"].t[0:1, :].partition_broadcast(128)))
    wbig_n = max(WC * DM, 10 * DM)
    wbig = s.sb("wbig", [128, wbig_n], BF16)
    sel = s.sb("sel", [32, 32, 128], BF16)
    s.memset("pool", sel[:], 1.0)
    s.op("pool", lambda: nc.gpsimd.affine_select(out=sel.t[:], in_=sel.t[:], pattern=[[-1, 32], [0, 128]],
                                                 compare_op=ALU.is_equal, fill=0.0, base=0, channel_multiplier=1),
         [sel[:]], [sel[:]])

    if PS is None:
        PS = [s.ps(f"ps{i}", [128, 512], F32) for i in range(8)]

    xT1 = s.sb("x1T", [128, 8, GT], BF16)
    acc = s.sb("acc", [128, GT // 128, DM], F32)
    combT = [s.sb(f"combT{i}", [32, GT], BF16) for i in range(2)]
    oTb = [s.sb(f"oTb{i}", [128, WC, 256], BF16) for i in range(2)]
    xt = [s.sb(f"xt{i}", [128, DM], F32) for i in range(2)]
    y = s.sb("y", [128, DM], F32)
    xn = s.sb("xn", [128, DM], F32)
    x1 = [s.sb(f"x1_{i}", [128, DM], F32) for i in range(2)]
    xTf = s.sb("xTf", [128, 8, 128], F32)
    xTl = s.sb("xTl", [128, 8, 128], BF16)
    st6 = s.sb("st6", [128, 12], F32)
    mv = s.sb("mv", [128, 2], F32)
    rstd = s.sb("rstd", [128, 1], F32)
    nmr = s.sb("nmr", [128, 1], F32)
    lg = s.sb("lg", [128, 36], F32)
    sm = {k: s.sb("sm_" + k, [128, n], F32) for k, n in
          [("gmax", 1), ("goh", 4), ("gex", 4), ("gsum", 1), ("gp", 1), ("es", 8), ("m1", 1), ("k1", 8),
           ("e2", 8), ("m2", 1), ("k2", 8), ("d", 1), ("w1", 1), ("w2", 1), ("wi", 8), ("comb", 32), ("lo", 32)]}
    combh = s.sb("combh", [128, 32], BF16)
    combl = s.sb("combl", [128, 32], BF16)
    wg = [s.sb(f"wg{i}", [128, 8, 256], BF16) for i in range(2)]
    wu = [s.sb(f"wu{i}", [128, 8, 256], BF16) for i in range(2)]
    wd = [s.sb(f"wd{i}", [128, 2, DM], BF16) for i in range(2)]
    cb = [s.sb(f"cb{i}", [128, 512], F32) for i in range(2)]
    sg = [s.sb(f"sg{i}", [128, 512], F32) for i in range(2)]
    tmp = [s.sb(f"tmp{i}", [128, 512], F32) for i in range(2)]
    hT = [s.sb(f"hT{i}", [128, 2, 512], BF16) for i in range(2)]
    x2 = s.sb("x2", [128, DM], F32)
    x2T = s.sb("x2T", [128, 8, 128], BF16)
    pt = s.sb("pt", [128, 256], F32)
    pT = s.sb("pT", [128, 2, 128], BF16)
    sgt = s.sb("sgt", [128, DM], F32)
    xo = [s.sb(f"xo{i}", [128, DM], F32) for i in range(1)]
    xoT = [s.sb(f"xoT{i}", [128, 8, 128], BF16) for i in range(1)]

    NG = NT // GT
    TPG = GT // 128
    for g in range(NG):
        t0g = g * GT
        wo = wbig.v(wbig.t[:, 0:WC * DM].rearrange("p (c n) -> p c n", c=WC))
        s.dma("pool", wo, D["w_out"].v(D["w_out"].t.rearrange("(c p) n -> p c n", p=128)))
        for tl in range(TPG):
            tok0 = t0g + tl * 128
            if tl % 2 == 0:
                ob = oTb[(tl // 2) % 2]
                nb = min(256, GT - tl * 128)
                s.dma("sp", ob[:, :, 0:nb], D["oT"].v(D["oT"].t[:, tok0:tok0 + nb].rearrange("(c p) t -> p c t", p=128)))
            xc = xt[tl % 2]
            s.dma("sp", xc[:], D["xres"][tok0:tok0 + 128, :])
            o_off = (tl % 2) * 128
            for hf in range(2):
                for wc in range(WC):
                    s.mm(PS[hf][:, :], ob[:, wc, o_off:o_off + 128], wo.tile.v(wo.ap[:, wc, hf * 512:(hf + 1) * 512]),
                         start=(wc == 0), stop=(wc == WC - 1))
            for hf in range(2):
                s.stt("dve", y[:, hf * 512:(hf + 1) * 512], xc[:, hf * 512:(hf + 1) * 512], ALPHA, PS[hf][:, :], ALU.mult, ALU.add)
            x1c = x1[tl % 2]
            layernorm_tile(s, y, xn, x1c, lng[0], lnb[0], st6, mv, rstd, nmr)
            s.dma("sp", D["x1res"][tok0:tok0 + 128, :], x1c[:])
            if upto == 1:
                s.dma("sp", D["xout"][tok0:tok0 + 128, :], x1c[:])
                continue
            for dc in range(8):
                pt_ = PS[2 + dc // 4]
                s.tr(pt_[:, (dc % 4) * 128:(dc % 4 + 1) * 128], x1c[:, dc * 128:(dc + 1) * 128], idf[:])
            for hh in range(2):
                s.cp("act", xTf.v(xTf.t[:, hh * 4:(hh + 1) * 4, :].rearrange("p c t -> p (c t)")), PS[2 + hh][:, :])
            s.cp("dve", xT1[:, :, tl * 128:(tl + 1) * 128], xTf[:])
            if upto == 2:
                s.dma("sp", D["xout"][tok0:tok0 + 128, :], xTf.v(xTf.t[:, :, :].rearrange("p c t -> p (c t)")))
                continue
            xh = xT1[:, :, tl * 128:(tl + 1) * 128]
            s.tt("dve", xTf[:], xTf[:], xh, ALU.subtract)
            s.cp("dve", xTl[:], xTf[:])
            k_ = 0
            for (xa, wa) in ((xh, wrh), (xTl[:], wrh), (xh, wrl)):
                for dc in range(8):
                    s.mm(PS[4][:, 0:36], xa.tile.v(xa.ap[:, dc, :]), wa[:, dc, :], start=(k_ == 0), stop=(k_ == 23))
                    k_ += 1
            s.tt("dve", lg[:], PS[4][:, 0:36], brt[:], ALU.add)
            routing(s, lg, sm)
            if upto == 3:
                s.dma("sp", D["xout"][tok0:tok0 + 128, 0:32], sm["comb"][:])
                continue
            s.cp("dve", combh[:], sm["comb"][:])
            s.tt("dve", sm["lo"][:], sm["comb"][:], combh[:], ALU.subtract)
            s.cp("dve", combl[:], sm["lo"][:])
            pb = PS[5].v(PS[5].t[:, :].bitcast(BF16))
            s.tr(PS[5].v(pb.ap[0:32, 0:128]), combh[:], idb[:])
            s.tr(PS[5].v(pb.ap[0:32, 128:256]), combl[:], idb[:])
            s.cp("act", combT[0][:, tl * 128:(tl + 1) * 128], PS[5].v(pb.ap[0:32, 0:128]))
            s.cp("act", combT[1][:, tl * 128:(tl + 1) * 128], PS[5].v(pb.ap[0:32, 128:256]))
        if upto <= 3:
            continue
        if upto == 4:
            s.dma("sp", D["xout"][0:32, 0:GT], combT[0][:])
            continue
        NB = GT // 512 if GT >= 512 else 1
        BW = min(512, GT)
        for e in range(32):
            b = e % 2
            s.dma("pool", wg[b][:], D["w_gate"].v(D["w_gate"].t[e].rearrange("(c p) f -> p c f", p=128)))
            s.dma("pool", wu[b][:], D["w_up"].v(D["w_up"].t[e].rearrange("(c p) f -> p c f", p=128)))
            s.dma("pool", wd[b][:], D["w_down"].v(D["w_down"].t[e].rearrange("(c p) n -> p c n", p=128)))
            for blk in range(NB):
                k = (e * NB + blk) % 2
                bs = slice(blk * BW, (blk + 1) * BW)
                s.mm(PS[0][:, 0:BW], sel[:, e, :], combT[0][:, bs], start=True, stop=False)
                s.mm(PS[0][:, 0:BW], sel[:, e, :], combT[1][:, bs], start=False, stop=True)
                s.cp("act", cb[k][:, 0:BW], PS[0][:, 0:BW])
                for fc in range(2):
                    gP, uP = PS[1 + 2 * fc], PS[2 + 2 * fc]
                    for dc in range(8):
                        s.mm(gP[:, 0:BW], wg[b][:, dc, fc * 128:(fc + 1) * 128], xT1[:, dc, bs], start=(dc == 0), stop=(dc == 7))
                    for dc in range(8):
                        s.mm(uP[:, 0:BW], wu[b][:, dc, fc * 128:(fc + 1) * 128], xT1[:, dc, bs], start=(dc == 0), stop=(dc == 7))
                    s.act(sg[fc][:, 0:BW], gP[:, 0:BW], AF.Silu)
                    s.tt("dve", tmp[fc][:, 0:BW], sg[fc][:, 0:BW], uP[:, 0:BW], ALU.mult)
                    s.tt("pool", hT[k][:, fc, 0:BW], tmp[fc][:, 0:BW], cb[k][:, 0:BW], ALU.mult)
                for tt_ in range(BW // 128):
                    tl = blk * (BW // 128) + tt_
                    for hf in range(2):
                        dP = PS[5 + (tt_ * 2 + hf) % 3]
                        for fc in range(2):
                            s.mm(dP[:, :], hT[k][:, fc, tt_ * 128:(tt_ + 1) * 128], wd[b][:, fc, hf * 512:(hf + 1) * 512],
                                 start=(fc == 0), stop=(fc == 1))
                        a_ = acc[:, tl, hf * 512:(hf + 1) * 512]
                        if e == 0:
                            s.cp("act", a_, dP[:, :])
                        else:
                            s.tt("dve", a_, a_, dP[:, :], ALU.add)
        if upto == 5:
            for tl in range(TPG):
                s.dma("sp", D["xout"][t0g + tl * 128:t0g + (tl + 1) * 128, :], acc[:, tl, :])
            continue
        wpg = wbig.v(wbig.t[:, 0:8 * DM].rearrange("p (c n) -> p c n", c=8))
        wpp = wbig.v(wbig.t[:, 8 * DM:10 * DM].rearrange("p (c n) -> p c n", c=2))
        s.dma("pool", wpg, D["pe_g"].v(D["pe_g"].t.rearrange("(c p) n -> p c n", p=128)))
        s.dma("pool", wpp, D["pe_p"].v(D["pe_p"].t.rearrange("(c p) n -> p c n", p=128)))
        for tl in range(TPG):
            tok0 = t0g + tl * 128
            xc = xt[tl % 2]
            s.dma("sp", xc[:], D["x1res"][tok0:tok0 + 128, :])
            s.dma("sp", pt[:], D["pl"][tok0:tok0 + 128, :])
            s.stt("dve", y[:], xc[:], ALPHA, acc[:, tl, :], ALU.mult, ALU.add)
            layernorm_tile(s, y, xn, x2, lng[1], lnb[1], st6, mv, rstd, nmr)
            for dc in range(8):
                pt_ = PS[dc // 4]
                s.tr(pt_[:, (dc % 4) * 128:(dc % 4 + 1) * 128], x2[:, dc * 128:(dc + 1) * 128], idf[:])
            for hh in range(2):
                s.cp("act" if hh == 0 else "dve", x2T.v(x2T.t[:, hh * 4:(hh + 1) * 4, :].rearrange("p c t -> p (c t)")), PS[hh][:, :])
            for pc in range(2):
                s.tr(PS[2][:, pc * 128:(pc + 1) * 128], pt[:, pc * 128:(pc + 1) * 128], idf[:])
            s.cp("act", pT.v(pT.t[:, :, :].rearrange("p c t -> p (c t)")), PS[2][:, 0:256])
            for hf in range(2):
                for dc in range(8):
                    s.mm(PS[3 + hf][:, :], x2T[:, dc, :], wbig.v(wpg.ap[:, dc, hf * 512:(hf + 1) * 512]), start=(dc == 0), stop=(dc == 7))
                for pc in range(2):
                    s.mm(PS[5 + hf][:, :], pT[:, pc, :], wbig.v(wpp.ap[:, pc, hf * 512:(hf + 1) * 512]), start=(pc == 0), stop=(pc == 1))
            xoc = xo[0]
            for hf in range(2):
                hs = slice(hf * 512, (hf + 1) * 512)
                s.act(sgt[:, hs], PS[3 + hf][:, :], AF.Sigmoid)
                s.tt("dve", sgt[:, hs], sgt[:, hs], PS[5 + hf][:, :], ALU.mult)
                s.tt("pool", xoc[:, hs], sgt[:, hs], x2[:, hs], ALU.add)
            s.dma("sp", D["xout"][tok0:tok0 + 128, :], xoc[:])
            if not last:
                for dc in range(8):
                    pt_ = PS[dc // 4]
                    s.tr(pt_[:, (dc % 4) * 128:(dc % 4 + 1) * 128], xoc[:, dc * 128:(dc + 1) * 128], idf[:])
                xoTc = xoT[0]
                for hh in range(2):
                    s.cp("act" if hh == 0 else "dve", xoTc.v(xoTc.t[:, hh * 4:(hh + 1) * 4, :].rearrange("p c t -> p (c t)")), PS[hh][:, :])
                s.dma("sp", D["xTout"].v(D["xTout"].t[:, tok0:tok0 + 128].rearrange("(c p) t -> p c t", p=128)), xoTc[:])


def routing(s, lg, sm):
    nc = s.nc
    gl = lg[:, 0:4]
    s.op("dve", lambda: nc.vector.reduce_max(out=sm["gmax"].t[:], in_=lg.t[:, 0:4], axis=AX.X), [sm["gmax"][:]], [lg[:]])
    s.ts("dve", sm["goh"][:], gl, sm["gmax"][:], ALU.is_equal)
    s.ts("dve", sm["gex"][:], gl, sm["gmax"][:], ALU.subtract)
    s.act(sm["gex"][:], sm["gex"][:], AF.Exp)
    s.op("dve", lambda: nc.vector.reduce_sum(out=sm["gsum"].t[:], in_=sm["gex"].t[:], axis=AX.X), [sm["gsum"][:]], [sm["gex"][:]])
    s.op("dve", lambda: nc.vector.reciprocal(out=sm["gp"].t[:], in_=sm["gsum"].t[:]), [sm["gp"][:]], [sm["gsum"][:]])
    s.ts("dve", sm["es"][:], lg[:, 4:12], sm["goh"][:, 0:1], ALU.mult)
    for g in range(1, 4):
        s.stt("dve", sm["es"][:], lg[:, 4 + 8 * g:12 + 8 * g], sm["goh"][:, g:g + 1], sm["es"][:], ALU.mult, ALU.add)
    s.op("dve", lambda: nc.vector.reduce_max(out=sm["m1"].t[:], in_=sm["es"].t[:], axis=AX.X), [sm["m1"][:]], [sm["es"][:]])
    s.ts("dve", sm["k1"][:], sm["es"][:], sm["m1"][:], ALU.is_equal)
    s.stt("dve", sm["e2"][:], sm["k1"][:], NEG_BIG, sm["es"][:], ALU.mult, ALU.add)
    s.op("dve", lambda: nc.vector.reduce_max(out=sm["m2"].t[:], in_=sm["e2"].t[:], axis=AX.X), [sm["m2"][:]], [sm["e2"][:]])
    s.ts("dve", sm["k2"][:], sm["e2"][:], sm["m2"][:], ALU.is_equal)
    s.tt("dve", sm["d"][:], sm["m2"][:], sm["m1"][:], ALU.subtract)
    s.act(sm["d"][:], sm["d"][:], AF.Exp)
    s.ts("dve", sm["d"][:], sm["d"][:], 1.0, ALU.add)
    s.op("dve", lambda: nc.vector.reciprocal(out=sm["w1"].t[:], in_=sm["d"].t[:]), [sm["w1"][:]], [sm["d"][:]])
    s.ts("dve", sm["w2"][:], sm["w1"][:], -1.0, ALU.mult, 1.0, ALU.add)
    s.tt("dve", sm["w1"][:], sm["w1"][:], sm["gp"][:], ALU.mult)
    s.tt("dve", sm["w2"][:], sm["w2"][:], sm["gp"][:], ALU.mult)
    s.ts("dve", sm["wi"][:], sm["k1"][:], sm["w1"][:], ALU.mult)
    s.stt("dve", sm["wi"][:], sm["k2"][:], sm["w2"][:], sm["wi"][:], ALU.mult, ALU.add)
    for g in range(4):
        s.ts("dve", sm["comb"][:, 8 * g:8 * g + 8], sm["wi"][:], sm["goh"][:, g:g + 1], ALU.mult)

NORM_EPS = 1e-6


def xT_view(D, lo, hi):
    t = D["xT"]
    if len(t.t.shape) == 2:
        return t.v(t.t[:, lo:hi].rearrange("(c p) t -> p c t", p=128))
    slot = lo // 4096
    assert (hi - 1) // 4096 == slot
    return t.v(t.t[slot, :, lo - slot * 4096:hi - slot * 4096].rearrange("(c p) t -> p c t", p=128))


def make_masks(s, C):
    nc = s.nc

    def tri(name, pat, cm, op):
        t = s.sb(name, [128, 128], F32)
        s.memset("pool", t[:], 1.0)
        s.op("pool", lambda: nc.gpsimd.affine_select(out=t.t[:], in_=t.t[:], pattern=[[pat, 128]], compare_op=op,
                                                     fill=0.0, base=0, channel_multiplier=cm), [t[:]], [t[:]])
        return t
    C["U128"] = [tri("U128f", 1, -1, ALU.is_ge), tri("U128b", -1, 1, ALU.is_ge)]
    C["SU128"] = [tri("SU128f", -1, 1, ALU.is_gt), tri("SU128b", 1, -1, ALU.is_gt)]
    U64 = [tri("U64f", 1, -1, ALU.is_ge), tri("U64b", -1, 1, ALU.is_ge)]
    SU64 = [tri("SU64f", -1, 1, ALU.is_gt), tri("SU64b", 1, -1, ALU.is_gt)]
    s.memset("pool", U64[0][0:64, 64:128], 0.0)
    s.memset("pool", U64[1][64:128, 0:64], 0.0)
    s.memset("pool", SU64[0][64:128, 0:64], 0.0)
    s.memset("pool", SU64[1][0:64, 64:128], 0.0)
    C["U64"] = U64
    C["SU64"] = SU64
    ones = s.sb("ones", [128, 128], F32)
    s.memset("pool", ones[:], 1.0)
    C["ones"] = ones
    C["U64m"] = U64
    return C


def formA_tile(s, C, d, PSg, PSa, PSo, qT, kT, k_tm, g_tm, v_tm, V, St, W):
    nc = s.nc
    U, SU = C["U64"][d], C["SU64"][d]
    gcT = PSg.tile.v(PSg.ap[:, 0:128])
    dec = PSg.tile.v(PSg.ap[:, 128:256])
    s.mm(gcT, g_tm, U[:])
    s.mm(dec, SU[:], g_tm)
    Eg, Eng, Ed = W["Eg"], W["Eng"], W["Ed"]
    s.act(Eg[:], gcT, AF.Exp)
    s.act(Eng[:], gcT, AF.Exp, scale=-1.0)
    s.act(Ed[:], dec, AF.Exp)
    QdT, KdT, Kl = W["QdT"], W["KdT"], W["Kl"]
    s.tt("dve", QdT[:], qT, Eg[:], ALU.mult)
    s.tt("pool", KdT[:], kT, Eng[:], ALU.mult)
    s.tt("pool", Kl[:], k_tm, Ed[:], ALU.mult)
    AT = PSa.tile.v(PSa.ap[:, 0:128])
    dS = PSa.tile.v(PSa.ap[:, 128:128 + V])
    s.mm(AT, KdT[:], QdT[:])
    ATm = W["ATm"]
    s.tt("dve", ATm[:], AT, U[:], ALU.mult)
    first, second = (slice(0, 64), slice(64, 128)) if d == 0 else (slice(64, 128), slice(0, 64))
    e_first, e_second = (63, 127) if d == 0 else (64, 0)
    S = St["S"]
    Sb = St["Sb"]
    cur = Sb[St["i"] % 2]
    s.mm(PSo.tile.v(PSo.ap[first, :]), ATm[:, first], v_tm, start=True, stop=False)
    s.mm(PSo.tile.v(PSo.ap[first, :]), QdT[:, first], cur[:], start=False, stop=True)
    s.mm(dS, Kl[first, :], v_tm.tile.v(v_tm.ap[first, :]))
    s.stt("dve", S[:], S[:], Eg[:, e_first:e_first + 1], dS, ALU.mult, ALU.add)
    St["i"] += 1
    nxt = Sb[St["i"] % 2]
    s.cp("act", nxt[:], S[:])
    s.mm(PSo.tile.v(PSo.ap[second, :]), ATm[:, second], v_tm, start=True, stop=False)
    s.mm(PSo.tile.v(PSo.ap[second, :]), QdT[:, second], nxt[:], start=False, stop=True)
    s.mm(dS, Kl[second, :], v_tm.tile.v(v_tm.ap[second, :]))
    s.stt("dve", S[:], S[:], Eg[:, e_second:e_second + 1], dS, ALU.mult, ALU.add)
    St["i"] += 1
    nxt2 = Sb[St["i"] % 2]
    s.cp("act", nxt2[:], S[:])


def formA_work(s, V):
    W = {}
    for n in ("Eg", "Eng", "Ed"):
        W[n] = s.sb(n, [128, 128], F32)
    for n in ("QdT", "KdT", "Kl", "ATm"):
        W[n] = s.sb(n, [128, 128], BF16)
    return W


def out_stage(s, C, d, PSo_all, PSt, D, tok0, ncol, nh, V, sgate, normB, Wk, col0=0, pre_gate=False):
    nc = s.nc
    osb = Wk["osb"][Wk["oi"] % 2]
    Wk["oi"] += 1
    if d == 0:
        s.cp("act", osb[:, 0:ncol], PSo_all)
        s.dma("sp", D["of"].v(D["of"].t[tok0:tok0 + 128, col0:col0 + ncol]), osb[:, 0:ncol])
        return
    ofl = Wk["ofl"]
    s.dma("sp", ofl[:, 0:ncol], D["of"].v(D["of"].t[tok0:tok0 + 128, col0:col0 + ncol]))
    s.tt("dve", osb[:, 0:ncol], ofl[:, 0:ncol], PSo_all, ALU.add)
    ss, rs, sq = Wk["ss"], Wk["rs"], Wk["sq"]
    if pre_gate:
        s.tt("dve", osb[:, 0:ncol], osb[:, 0:ncol], sgate, ALU.mult)
    s.memset("pool", ss[:], 0.0)
    for h in range(nh):
        s.act(sq[:, 0:V], osb[:, h * V:(h + 1) * V], AF.Square, accum=ss[:, h:h + 1])
    s.act(rs[:, 0:nh], ss[:, 0:nh], AF.Ln, bias=NORM_EPS, scale=1.0 / V)
    s.act(rs[:, 0:nh], rs[:, 0:nh], AF.Exp, scale=-0.5)
    og = Wk["og"]
    for h in range(nh):
        if pre_gate:
            s.ts("dve", osb[:, h * V:(h + 1) * V], osb[:, h * V:(h + 1) * V], rs[:, h:h + 1], ALU.mult)
        else:
            s.stt("dve", osb[:, h * V:(h + 1) * V], osb[:, h * V:(h + 1) * V], rs[:, h:h + 1], sgate.tile.v(sgate.ap[:, h * V:(h + 1) * V]),
                  ALU.mult, ALU.mult)
    s.tt("pool", og[:, 0:ncol], osb[:, 0:ncol], normB, ALU.mult)
    ptb = PSt.tile.v(PSt.ap.bitcast(BF16))
    nchunk = ncol // 128
    oTs = Wk["oTs"][Wk["oi"] % 2]
    for c in range(nchunk):
        s.tr(PSt.tile.v(ptb.ap[:, c * 128:(c + 1) * 128]), og[:, c * 128:(c + 1) * 128], C["idb"][:])
    s.cp("act", oTs[:, 0:nchunk, :], PSt.tile.v(ptb.ap[:, 0:nchunk * 128].rearrange("p (c t) -> p c t", c=nchunk)))
    s.dma("sp", D["oT"].v(D["oT"].t[col0:col0 + ncol, tok0:tok0 + 128].rearrange("(c p) t -> p c t", p=128)), oTs[:, 0:nchunk, :])


def out_work(s, maxcol):
    Wk = {"oi": 0}
    Wk["osb"] = [s.sb(f"osb{i}", [128, maxcol], F32) for i in range(2)]
    Wk["ofl"] = s.sb("ofl", [128, maxcol], F32)
    Wk["og"] = s.sb("og", [128, maxcol], BF16)
    Wk["sq"] = s.sb("sq", [128, 512], F32)
    Wk["ss"] = s.sb("ss", [128, 8], F32)
    Wk["rs"] = s.sb("rs", [128, 8], F32)
    Wk["oTs"] = [s.sb(f"oTs{i}", [128, maxcol // 128, 128], BF16) for i in range(2)]
    return Wk


def hg_mixer(s, C, D, L, PS):
    nc = s.nc
    nh, V = 2, 128
    win = s.sb("win", [128, 8, 1280], BF16)
    s.dma("pool", win[:], D["w_in"].v(D["w_in"].t.rearrange("(c p) n -> p c n", p=128)))
    def lb_from_logits(src_view, n, nm):
        lgt = s.sb(nm + "lg", [128, 4, n], F32)
        s.dma("sp", lgt[:], src_view)
        s.act(lgt[:], lgt[:], AF.Exp)
        num = s.sb(nm + "num", [128, n], F32)
        den = s.sb(nm + "den", [128, n], F32)
        s.tt("dve", num[:], lgt[:, 1, :], lgt[:, 2, :], ALU.add)
        s.tt("dve", den[:], lgt[:, 0, :], lgt[:, 3, :], ALU.add)
        s.tt("dve", den[:], den[:], num[:], ALU.add)
        s.op("dve", lambda: nc.vector.reciprocal(out=den.t[:], in_=den.t[:]), [den[:]], [den[:]])
        s.tt("dve", num[:], num[:], den[:], ALU.mult)
        return num
    lbB = lb_from_logits(D["lbrow"].v(D["lbrow"].t[0:1, :, :].partition_broadcast(128)), 256, "lbr")
    omlB = s.sb("omlB", [128, 256], F32)
    s.ts("dve", omlB[:], lbB[:], -1.0, ALU.mult, 1.0, ALU.add)
    lbc = lb_from_logits(D["lbcol"][:, :, :], 2, "lbc")
    omlc = s.sb("omlc", [128, 2], F32)
    nomlc = s.sb("nomlc", [128, 2], F32)
    s.ts("dve", omlc[:], lbc[:], -1.0, ALU.mult, 1.0, ALU.add)
    s.ts("dve", nomlc[:], omlc[:], -1.0, ALU.mult)
    normB = s.sb("normB", [128, 2, 128], F32)
    for h in range(2):
        s.dma("sp", normB[:, h, :], D["normw"].v(D["normw"].t[0:1, :].partition_broadcast(128)))
    xTb = [s.sb(f"xTb{i}", [128, 8, 512], BF16) for i in range(2)]
    qTs = [s.sb(f"qTs{h}", [128, 512], F32) for h in range(2)]
    kTs = [s.sb(f"kTs{h}", [128, 512], F32) for h in range(2)]
    sig = s.sb("sig", [128, 256], F32)
    t1 = s.sb("t1", [128, 256], F32)
    t2 = s.sb("t2", [128, 256], F32)
    ktm = s.sb("ktm", [128, 256], F32)
    gtm = s.sb("gtm", [128, 256], F32)
    vtm = s.sb("vtm", [128, 256], BF16)
    sgate = s.sb("sgate", [128, 256], F32)
    W = formA_work(s, V)
    Wk = out_work(s, 256)
    NBLK = L // 512
    for d in range(2):
        St = []
        for h in range(nh):
            S = s.sb(f"S{d}{h}", [128, V], F32)
            Sb = [s.sb(f"Sb{d}{h}{i}", [128, V], BF16) for i in range(2)]
            s.memset("dve", S[:], 0.0)
            s.memset("dve", Sb[0][:], 0.0)
            St.append({"S": S, "Sb": Sb, "i": 0})
        fcol = 256 + 256 * d
        blocks = range(NBLK) if d == 0 else range(NBLK - 1, -1, -1)
        for bi, blk in enumerate(blocks):
            xb = xTb[bi % 2]
            s.dma("sp", xb[:], xT_view(D, blk * 512, (blk + 1) * 512))
            for h in range(nh):
                for dc in range(8):
                    s.mm(PS[0][:, :], win[:, dc, h * 128:(h + 1) * 128], xb[:, dc, :], start=(dc == 0), stop=(dc == 7))
                s.act(qTs[h][:], PS[0][:, :], AF.Silu)
                for dc in range(8):
                    s.mm(PS[1][:, :], win[:, dc, fcol + h * 128:fcol + (h + 1) * 128], xb[:, dc, :], start=(dc == 0), stop=(dc == 7))
                s.act(kTs[h][:], PS[1][:, :], AF.Sigmoid)
                s.ts("dve", kTs[h][:], kTs[h][:], nomlc[:, h:h + 1], ALU.mult, omlc[:, h:h + 1], ALU.add)
            tiles = range(4) if d == 0 else range(3, -1, -1)
            for tl in tiles:
                tsl = slice(tl * 128, (tl + 1) * 128)
                tok0 = blk * 512 + tl * 128
                for dc in range(8):
                    s.mm(PS[2][:, 0:256], xb[:, dc, tsl], win[:, dc, fcol:fcol + 256], start=(dc == 0), stop=(dc == 7))
                for dc in range(8):
                    s.mm(PS[2][:, 256:512], xb[:, dc, tsl], win[:, dc, 768:1024], start=(dc == 0), stop=(dc == 7))
                s.act(sig[:], PS[2][:, 0:256], AF.Sigmoid)
                s.cp("act", vtm[:], PS[2][:, 256:512])
                s.tt("dve", t1[:], sig[:], omlB[:], ALU.mult)
                s.tt("pool", ktm[:], omlB[:], t1[:], ALU.subtract)
                s.tt("pool", t2[:], t1[:], lbB[:], ALU.add)
                s.act(gtm[:], t2[:], AF.Ln)
                if d == 1:
                    for dc in range(8):
                        s.mm(PS[3][:, 0:256], xb[:, dc, tsl], win[:, dc, 1024:1280], start=(dc == 0), stop=(dc == 7))
                    s.act(sgate[:], PS[3][:, 0:256], AF.Silu)
                for h in range(nh):
                    hs = slice(h * 128, (h + 1) * 128)
                    formA_tile(s, C, d, PS[4][:, h * 256:(h + 1) * 256], PS[5][:, h * 256:(h + 1) * 256], PS[6][:, h * V:(h + 1) * V],
                               qTs[h][:, tsl], kTs[h][:, tsl], ktm[:, hs], gtm[:, hs], vtm[:, hs], V, St[h], W)
                out_stage(s, C, d, PS[6][:, 0:256], PS[7][:, :], D, tok0, 256, nh, V, sgate[:],
                          normB.v(normB.t[:, :, :].rearrange("p h v -> p (h v)")), Wk)


def gla_mixer(s, C, D, L, PS):
    nc = s.nc
    nh, V = 1, 256
    win = s.sb("win", [128, 8, 800], BF16)
    s.dma("pool", win[:], D["w_in"].v(D["w_in"].t.rearrange("(c p) n -> p c n", p=128)))
    wgk = s.sb("wgk", [16, 2, 128], F32)
    s.dma("sp", wgk[:], D["wgk"].v(D["wgk"].t.rearrange("z r k -> r z k")))
    bgkB = s.sb("bgkB", [128, 2, 128], F32)
    s.dma("sp", bgkB[:], D["bgk"].v(D["bgk"].t.rearrange("(o z) k -> o z k", o=1).partition_broadcast(128)))
    normB = s.sb("normB", [128, 256], F32)
    s.dma("sp", normB[:], D["normw"].v(D["normw"].t[0:1, :].partition_broadcast(128)))
    xTb = [s.sb(f"xTb{i}", [128, 8, 512], BF16) for i in range(2)]
    qTs = s.sb("qTs", [128, 512], F32)
    kTs = s.sb("kTs", [128, 512], F32)
    rTs = s.sb("rTs", [16, 512], F32)
    z = s.sb("z", [128, 128], F32)
    ktm = s.sb("ktm", [128, 128], F32)
    gtm = s.sb("gtm", [128, 128], F32)
    vtm = s.sb("vtm", [128, 256], BF16)
    sgate = s.sb("sgate", [128, 256], F32)
    W = formA_work(s, V)
    Wk = out_work(s, 256)
    NBLK = L // 512
    for d in range(2):
        S = s.sb(f"S{d}", [128, V], F32)
        Sb = [s.sb(f"Sb{d}{i}", [128, V], BF16) for i in range(2)]
        s.memset("dve", S[:], 0.0)
        s.memset("dve", Sb[0][:], 0.0)
        St = {"S": S, "Sb": Sb, "i": 0}
        rcol = 768 + 16 * d
        blocks = range(NBLK) if d == 0 else range(NBLK - 1, -1, -1)
        for bi, blk in enumerate(blocks):
            xb = xTb[bi % 2]
            s.dma("sp", xb[:], xT_view(D, blk * 512, (blk + 1) * 512))
            for dc in range(8):
                s.mm(PS[0][:, :], win[:, dc, 0:128], xb[:, dc, :], start=(dc == 0), stop=(dc == 7))
            s.act(qTs[:], PS[0][:, :], AF.Copy, scale=float(128 ** -0.5))
            for dc in range(8):
                s.mm(PS[1][:, :], win[:, dc, 128:256], xb[:, dc, :], start=(dc == 0), stop=(dc == 7))
            s.cp("act", kTs[:], PS[1][:, :])
            for dc in range(8):
                s.mm(PS[3][0:16, :], win[:, dc, rcol:rcol + 16], xb[:, dc, :], start=(dc == 0), stop=(dc == 7))
            s.cp("act", rTs[:], PS[3][0:16, :])
            tiles = range(4) if d == 0 else range(3, -1, -1)
            for tl in tiles:
                tsl = slice(tl * 128, (tl + 1) * 128)
                tok0 = blk * 512 + tl * 128
                for dc in range(8):
                    s.mm(PS[2][:, 0:384], xb[:, dc, tsl], win[:, dc, 128:512], start=(dc == 0), stop=(dc == 7))
                s.cp("act", ktm[:], PS[2][:, 0:128])
                s.cp("act", vtm[:], PS[2][:, 128:384])
                s.mm(PS[2][:, 384:512], rTs[:, tsl], wgk[:, d, :])
                s.tt("dve", z[:], PS[2][:, 384:512], bgkB[:, d, :], ALU.add)
                s.act(z[:], z[:], AF.Exp, scale=-1.0)
                s.act(z[:], z[:], AF.Ln, bias=1.0)
                s.ts("dve", gtm[:], z[:], -1.0 / 16.0, ALU.mult)
                if d == 1:
                    for dc in range(8):
                        s.mm(PS[3][:, 0:256], xb[:, dc, tsl], win[:, dc, 512:768], start=(dc == 0), stop=(dc == 7))
                    s.act(sgate[:], PS[3][:, 0:256], AF.Silu)
                formA_tile(s, C, d, PS[4][:, 0:256], PS[5][:, 0:384], PS[6][:, 0:256],
                           qTs[:, tsl], kTs[:, tsl], ktm[:], gtm[:], vtm[:], V, St, W)
                out_stage(s, C, d, PS[6][:, 0:256], PS[7][:, :], D, tok0, 256, nh, V, sgate[:], normB[:], Wk)

MASKBIG = 30000.0


def make_masks_b(s, C):
    C["maskneg"] = []
    for d in range(2):
        m = s.sb(f"maskneg{d}", [128, 128], F32)
        s.ts("pool", m[:], C["U128"][d][:], -1.0, ALU.add, MASKBIG, ALU.mult)
        C["maskneg"].append(m)
    C["maskstrict"] = []
    for d in range(2):
        m = s.sb(f"maskstr{d}", [128, 128], F32)
        s.ts("pool", m[:], C["SU128"][d][:], -1.0, ALU.add, -MASKBIG, ALU.mult)
        C["maskstrict"].append(m)


def load_block_halo(s, xb, D, blk, NBLK, TB=512):
    lo, hi = blk * TB - 2, blk * TB + TB + 2
    a, b = 0, TB + 4
    if blk == 0:
        s.memset("pool", xb[:, :, 0:2], 0.0)
        lo, a = 0, 2
    if blk == NBLK - 1:
        s.memset("pool", xb[:, :, TB + 2:TB + 4], 0.0)
        hi, b = blk * TB + TB, TB + 2
    cur = lo
    while cur < hi:
        nxt = min(hi, (cur // 4096 + 1) * 4096)
        s.dma("sp", xb[:, :, a + (cur - lo):a + (nxt - lo)], xT_view(D, cur, nxt))
        cur = nxt


def conv_chunk(s, PSa, PSb, win, col0, xb, pre, acc, cw, cc, out, bias=None):
    for half, P in ((0, PSa), (1, PSb)):
        for dc in range(8):
            s.mm(P[:, 0:258], win[:, dc, col0:col0 + 128], xb[:, dc, half * 258:(half + 1) * 258], start=(dc == 0), stop=(dc == 7))
        s.cp("act", pre[:, half * 258:(half + 1) * 258], P[:, 0:258])
    s.ts("dve", acc[:], pre[:, 0:512], cw[:, cc, 0:1], ALU.mult)
    for k in range(1, 5):
        s.stt("dve", acc[:], pre[:, k:k + 512], cw[:, cc, k:k + 1], acc[:], ALU.mult, ALU.add)
    if bias is None:
        s.act(out, acc[:], AF.Silu)
    else:
        s.act(out, acc[:], AF.Silu, bias=bias)


def ssd_mixer(s, C, D, L, PS):
    nc = s.nc
    win = s.sb("win", [128, 8, 1296], BF16)
    s.dma("pool", win[:], D["w_in"].v(D["w_in"].t.rearrange("(c p) n -> p c n", p=128)))
    cw = s.sb("cw", [128, 6, 5], F32)
    s.dma("sp", cw[:], D["convw"][:, :, :])
    cbias = s.sb("cbias", [128, 6], F32)
    s.dma("sp", cbias[:], D["convb"][:, :])
    negA = s.sb("negA", [128, 2, 8], F32)
    s.dma("sp", negA[:], D["alog"].v(D["alog"].t.rearrange("(o z) r -> o z r", o=1).partition_broadcast(128)))
    s.act(negA[:], negA[:], AF.Exp)
    s.ts("dve", negA[:], negA[:], -1.0, ALU.mult)
    dtbB = s.sb("dtbB", [128, 2, 8], F32)
    s.dma("sp", dtbB[:], D["dtb"].v(D["dtb"].t.rearrange("(o z) r -> o z r", o=1).partition_broadcast(128)))
    dskB = s.sb("dskB", [128, 8], F32)
    s.dma("sp", dskB[:], D["dsk"].v(D["dsk"].t[0:1, :].partition_broadcast(128)))
    normB = s.sb("normB", [128, 512], F32)
    s.dma("sp", normB[:], D["normw"].v(D["normw"].t[0:1, :].partition_broadcast(128)))
    xTb = [s.sb(f"xTb{i}", [128, 8, 516], BF16) for i in range(2)]
    pre = s.sb("pre", [128, 516], F32)
    acc = s.sb("cacc", [128, 512], F32)
    xsT = [s.sb(f"xsT{c}", [128, 512], F32) for c in range(4)]
    BT = s.sb("BT", [128, 512], BF16)
    CT = s.sb("CT", [128, 512], BF16)
    dt = s.sb("dt", [128, 8], F32)
    g = s.sb("g", [128, 8], F32)
    gB = s.sb("gB", [128, 8, 128], F32)
    ones3 = s.sb("ones3", [128, 8, 128], F32)
    s.memset("pool", ones3[:], 1.0)
    E24 = s.sb("E24", [128, 24], F32)
    ngc = s.sb("ngc", [128, 8], F32)
    xs_tm = s.sb("xs_tm", [128, 8, 64], F32)
    u = s.sb("u", [128, 8, 64], F32)
    u_bf = s.sb("u_bf", [128, 512], BF16)
    udec = s.sb("udec", [128, 8, 64], BF16)
    B_tm = s.sb("B_tm", [128, 128], BF16)
    Er = [s.sb(f"Er{i}", [128, 128], F32) for i in range(2)]
    STr = [s.sb(f"STr{i}", [128, 128], BF16) for i in range(2)]
    tmpB = s.sb("tmpB", [128, 8, 64], F32)
    ysb = s.sb("ysb", [128, 512], F32)
    skip = s.sb("skip", [128, 8, 64], F32)
    sgate = s.sb("sgate", [128, 512], F32)
    Wk = out_work(s, 512)
    NBLK = L // 512
    idf = C["idf"]
    for d in range(2):
        S = s.sb(f"S{d}", [128, 8, 64], F32)
        Sb = [s.sb(f"Sb{d}{i}", [128, 512], BF16) for i in range(2)]
        s.memset("dve", S[:], 0.0)
        s.memset("dve", Sb[0][:], 0.0)
        si = 0
        U, SU, mneg = C["U128"][d], C["SU128"][d], C["maskneg"][d]
        dtcol = 1280 + 8 * d
        blocks = range(NBLK) if d == 0 else range(NBLK - 1, -1, -1)
        for bi, blk in enumerate(blocks):
            xb = xTb[bi % 2]
            load_block_halo(s, xb, D, blk, NBLK)
            for cc in range(6):
                out = xsT[cc][:] if cc < 4 else (BT[:] if cc == 4 else CT[:])
                conv_chunk(s, PS[0], PS[1], win, cc * 128, xb, pre, acc, cw, cc, out, bias=cbias[:, cc:cc + 1])
            tiles = range(4) if d == 0 else range(3, -1, -1)
            for tl in tiles:
                tsl = slice(tl * 128, (tl + 1) * 128)
                xsl = slice(2 + tl * 128, 2 + (tl + 1) * 128)
                tok0 = blk * 512 + tl * 128
                for dc in range(8):
                    s.mm(PS[3][:, 0:8], xb[:, dc, xsl], win[:, dc, dtcol:dtcol + 8], start=(dc == 0), stop=(dc == 7))
                s.tt("dve", dt[:], PS[3][:, 0:8], dtbB[:, d, :], ALU.add)
                s.act(dt[:], dt[:], AF.Exp)
                s.act(dt[:], dt[:], AF.Ln, bias=1.0)
                s.tt("dve", g[:], dt[:], negA[:, d, :], ALU.mult)
                s.tt("pool", gB[:], ones3[:], g.v(g.t[:, :].unsqueeze(2).to_broadcast([128, 8, 128])), ALU.mult)
                if d == 1:
                    for dc in range(8):
                        s.mm(PS[2][:, :], xb[:, dc, xsl], win[:, dc, 768:1280], start=(dc == 0), stop=(dc == 7))
                    s.act(sgate[:], PS[2][:, :], AF.Silu)
                for c in range(4):
                    s.tr(PS[4][:, c * 128:(c + 1) * 128], xsT[c][:, tsl], idf[:])
                s.cp("act", xs_tm.v(xs_tm.t[:, :, :].rearrange("p r q -> p (r q)")), PS[4][:, :])
                p5b = PS[5].v(PS[5].t[:, :].bitcast(BF16))
                s.tr(PS[5].v(p5b.ap[:, 768:896]), BT[:, tsl], C["idb"][:])
                s.cp("act", B_tm[:], PS[5].v(p5b.ap[:, 768:896]))
                dtb_ = dt.v(dt.t[:, :].unsqueeze(2).to_broadcast([128, 8, 64]))
                s.tt("dve", u[:], xs_tm[:], dtb_, ALU.mult)
                s.cp("pool", u_bf.v(u_bf.t[:, :].rearrange("p (r q) -> p r q", r=8)), u[:])
                s.mm(PS[3][:, 32:40], U[:], g[:])
                s.mm(PS[3][:, 40:48], SU[:], g[:])
                s.mm(PS[3][:, 48:56], C["ones"][:], g[:])
                s.act(E24[:], PS[3][:, 32:56], AF.Exp)
                s.act(ngc[:], PS[3][:, 32:40], AF.Copy, scale=-1.0)
                s.mm(PS[5][:, 0:128], BT[:, tsl], CT[:, tsl])
                for r in range(8):
                    Pr = PS[5][:, 128 + (r % 2) * 128:256 + (r % 2) * 128]
                    s.mm(Pr, gB[:, r, :], U[:], start=True, stop=False)
                    s.mm(Pr, idf[:], mneg[:], start=False, stop=True)
                    s.act(Er[r % 2][:], Pr, AF.Exp, bias=ngc[:, r:r + 1])
                    s.tt("dve", STr[r % 2][:], PS[5][:, 0:128], Er[r % 2][:], ALU.mult)
                    s.mm(PS[6][:, r * 64:(r + 1) * 64], STr[r % 2][:], u_bf[:, r * 64:(r + 1) * 64])
                cur = Sb[si % 2]
                s.mm(PS[7][:, :], CT[:, tsl], cur[:])
                s.cp("act", tmpB.v(tmpB.t[:, :, :].rearrange("p r q -> p (r q)")), PS[7][:, :])
                egc_b = E24.v(E24.t[:, 0:8].unsqueeze(2).to_broadcast([128, 8, 64]))
                s.tt("dve", tmpB[:], tmpB[:], egc_b, ALU.mult)
                s.tt("dve", ysb[:], tmpB.v(tmpB.t[:, :, :].rearrange("p r q -> p (r q)")), PS[6][:, :], ALU.add)
                edec_b = E24.v(E24.t[:, 8:16].unsqueeze(2).to_broadcast([128, 8, 64]))
                s.tt("pool", udec[:], u[:], edec_b, ALU.mult)
                s.mm(PS[0][:, :], B_tm[:], udec.v(udec.t[:, :, :].rearrange("p r q -> p (r q)")))
                egl_b = E24.v(E24.t[:, 16:24].unsqueeze(2).to_broadcast([128, 8, 64]))
                s.tt("dve", S[:], S[:], egl_b, ALU.mult)
                s.tt("dve", S.v(S.t[:, :, :].rearrange("p r q -> p (r q)")), S.v(S.t[:, :, :].rearrange("p r q -> p (r q)")), PS[0][:, :], ALU.add)
                si += 1
                s.cp("act", Sb[si % 2][:], S.v(S.t[:, :, :].rearrange("p r q -> p (r q)")))
                if d == 1:
                    dsk_b = dskB.v(dskB.t[:, :].unsqueeze(2).to_broadcast([128, 8, 64]))
                    s.tt("pool", skip[:], xs_tm[:], dsk_b, ALU.mult)
                    s.tt("pool", ysb[:], ysb[:], skip.v(skip.t[:, :, :].rearrange("p r q -> p (r q)")), ALU.add)
                out_stage(s, C, d, ysb[:], PS[1][:, :], D, tok0, 512, 1, 512, sgate[:], normB[:], Wk, pre_gate=True)


class Reg:
    def __init__(self, tile, a, b):
        self.tile, self.a, self.b = tile, a, b
        self.t = tile.t[:, a:b]

    def __getitem__(self, k):
        return View(self.tile, self.t[k])

    def v(self, ap):
        return View(self.tile, ap)


def gdn_mixer(s, C, D, L, PS):
    nc = s.nc
    idf, idb = C["idf"], C["idb"]
    win = s.sb("win", [128, 8, 1552], BF16)
    s.dma("pool", win[:], D["w_in"].v(D["w_in"].t.rearrange("(c p) n -> p c n", p=128)))
    cw = s.sb("cw", [128, 8, 5], F32)
    s.dma("sp", cw[:], D["convw"][:, :, :])
    negA = s.sb("negA", [128, 2, 4], F32)
    s.dma("sp", negA[:], D["alog"].v(D["alog"].t.rearrange("(o z) r -> o z r", o=1).partition_broadcast(128)))
    s.act(negA[:], negA[:], AF.Exp)
    s.ts("dve", negA[:], negA[:], -1.0, ALU.mult)
    dtbB = s.sb("dtbB", [128, 2, 4], F32)
    s.dma("sp", dtbB[:], D["dtb"].v(D["dtb"].t.rearrange("(o z) r -> o z r", o=1).partition_broadcast(128)))
    normB = s.sb("normB", [128, 4, 128], F32)
    for h in range(4):
        s.dma("sp", normB[:, h, :], D["normw"].v(D["normw"].t[0:1, :].partition_broadcast(128)))
    xTb = [s.sb(f"xTb{i}", [128, 8, 516], BF16) for i in range(2)]
    pre = s.sb("pre", [128, 516], F32)
    acc = s.sb("cacc", [128, 512], F32)
    qT = [s.sb(f"qT{c}", [128, 512], F32) for c in range(2)]
    kT = [s.sb(f"kT{c}", [128, 512], F32) for c in range(2)]
    vT = [s.sb(f"vT{c}", [128, 512], F32) for c in range(4)]
    qTb = [s.sb(f"qTb{c}", [128, 512], BF16) for c in range(2)]
    kTb = [s.sb(f"kTb{c}", [128, 512], BF16) for c in range(2)]
    sqt = s.sb("sqt", [128, 512], F32)
    rn = s.sb("rn", [128, 512], F32)
    ab = s.sb("ab", [128, 8], F32)
    g = s.sb("g", [128, 4], F32)
    beta = s.sb("beta", [128, 4], F32)
    nbeta = s.sb("nbeta", [128, 4], F32)
    bg = s.sb("bg", [128, 4], F32)
    gB = s.sb("gB", [128, 4, 128], F32)
    ones3 = s.sb("ones3", [128, 4, 128], F32)
    s.memset("pool", ones3[:], 1.0)
    E12 = s.sb("E12", [128, 12], F32)
    ngc = s.sb("ngc", [128, 4], F32)
    gcs = s.sb("gcs", [128, 4], F32)
    k_tm = s.sb("k_tm", [128, 2, 128], F32)
    v_tm = s.sb("v_tm", [128, 4, 128], F32)
    sgate = s.sb("sgate", [128, 512], F32)
    osb_all = s.sb("osb_all", [128, 512], F32)
    def quad(name, shape, dt):
        return [s.sb(f"{name}{i}", shape, dt) for i in range(4)]
    Einc = quad("Einc", [128, 128], F32); En = quad("En", [128, 128], F32)
    attnT = quad("attnT", [128, 128], BF16)
    Mb = [quad(f"Mb{i}", [128, 128], BF16) for i in range(2)]
    MTb = [quad(f"MTb{i}", [128, 128], BF16) for i in range(2)]
    P32 = quad("P32", [128, 128], F32); Pbf = quad("Pbf", [128, 128], BF16)
    kbg = quad("kbg", [128, 128], BF16); vb = quad("vb", [128, 128], BF16); kdec = quad("kdec", [128, 128], BF16)
    u_sb = quad("u_sb", [128, 128], F32); wTb = quad("wTb", [128, 128], BF16)
    vnew = quad("vnew", [128, 128], BF16); oAs = quad("oAs", [128, 128], F32)
    def reg(bank, a, b, nm):
        return Reg(PS[bank], a, b)
    pG = [reg(5, 0, 128, "pG0"), reg(5, 128, 256, "pG1")]
    pQK = [reg(5, 256, 384, "pQK0"), reg(5, 384, 512, "pQK1")]
    HB = [6, 7, 0, 1]
    pA = [reg(HB[h], 0, 128, "pA") for h in range(4)]
    pB = [reg(HB[h], 128, 256, "pB") for h in range(4)]
    pC = [reg(HB[h], 256, 384, "pC") for h in range(4)]
    pD = [reg(HB[h], 384, 512, "pD") for h in range(4)]
    pP1 = [reg(2 + (h % 2), 0, 128, "pP1") for h in range(4)]
    pP2 = [reg(2 + (h % 2), 128, 256, "pP2") for h in range(4)]
    Wk = out_work(s, 512)
    NBLK = L // 512
    for d in range(2):
        S = [s.sb(f"S{d}{h}", [128, 128], F32) for h in range(4)]
        Sb = [[s.sb(f"Sb{d}{h}{i}", [128, 128], BF16) for i in range(2)] for h in range(4)]
        si = [0] * 4
        for h in range(4):
            s.memset("dve", S[h][:], 0.0)
            s.memset("dve", Sb[h][0][:], 0.0)
        U, SU, mneg, mstr = C["U128"][d], C["SU128"][d], C["maskneg"][d], C["maskstrict"][d]
        abcol = 1536 + 8 * d
        blocks = range(NBLK) if d == 0 else range(NBLK - 1, -1, -1)
        for bi, blk in enumerate(blocks):
            xb = xTb[bi % 2]
            load_block_halo(s, xb, D, blk, NBLK)
            for cc in range(8):
                out = (qT[cc] if cc < 2 else kT[cc - 2] if cc < 4 else vT[cc - 4])[:]
                conv_chunk(s, PS[2], PS[3], win, cc * 128, xb, pre, acc, cw, cc, out)
            for (src, dstb, scl) in ((qT[0], qTb[0], float(128 ** -0.5)), (qT[1], qTb[1], float(128 ** -0.5)), (kT[0], kTb[0], 1.0), (kT[1], kTb[1], 1.0)):
                s.act(sqt[:], src[:], AF.Square)
                s.mm(PS[2][:, :], C["ones"][:], sqt[:])
                s.act(rn[:], PS[2][:, :], AF.Ln, bias=NORM_EPS)
                s.act(rn[:], rn[:], AF.Exp, scale=-0.5)
                s.stt("dve", src[:], src[:], scl, rn[:], ALU.mult, ALU.mult)
                s.cp("act", dstb[:], src[:])
            tiles = range(4) if d == 0 else range(3, -1, -1)
            for tl in tiles:
                tsl = slice(tl * 128, (tl + 1) * 128)
                xsl = slice(2 + tl * 128, 2 + (tl + 1) * 128)
                tok0 = blk * 512 + tl * 128
                for dc in range(8):
                    s.mm(PS[3][:, 0:8], xb[:, dc, xsl], win[:, dc, abcol:abcol + 8], start=(dc == 0), stop=(dc == 7))
                s.cp("act", ab[:], PS[3][:, 0:8])
                s.tt("dve", g[:], ab[:, 0:4], dtbB[:, d, :], ALU.add)
                s.act(g[:], g[:], AF.Exp)
                s.act(g[:], g[:], AF.Ln, bias=1.0)
                s.tt("dve", g[:], g[:], negA[:, d, :], ALU.mult)
                s.act(beta[:], ab[:, 4:8], AF.Exp, scale=-1.0)
                s.ts("dve", beta[:], beta[:], 1.0, ALU.add)
                s.op("dve", lambda: nc.vector.reciprocal(out=beta.t[:], in_=beta.t[:]), [beta[:]], [beta[:]])
                s.ts("dve", nbeta[:], beta[:], -1.0, ALU.mult)
                s.tt("pool", gB[:], ones3[:], g.v(g.t[:, :].unsqueeze(2).to_broadcast([128, 4, 128])), ALU.mult)
                if d == 1:
                    for dc in range(8):
                        s.mm(PS[2][:, :], xb[:, dc, xsl], win[:, dc, 1024:1536], start=(dc == 0), stop=(dc == 7))
                    s.act(sgate[:], PS[2][:, :], AF.Silu)
                for c in range(2):
                    s.tr(PS[3][:, 64 + c * 128:192 + c * 128], kT[c][:, tsl], idf[:])
                s.cp("act", k_tm.v(k_tm.t[:, :, :].rearrange("p c k -> p (c k)")), PS[3][:, 64:320])
                for c in range(4):
                    s.tr(PS[4][:, c * 128:(c + 1) * 128], vT[c][:, tsl], idf[:])
                s.cp("act", v_tm.v(v_tm.t[:, :, :].rearrange("p c k -> p (c k)")), PS[4][:, :])
                s.mm(PS[3][:, 32:36], U[:], g[:])
                s.mm(PS[3][:, 36:40], SU[:], g[:])
                s.mm(PS[3][:, 40:44], C["ones"][:], g[:])
                s.act(E12[:], PS[3][:, 32:44], AF.Exp)
                s.act(ngc[:], PS[3][:, 32:36], AF.Copy, scale=-1.0)
                s.act(gcs[:], PS[3][:, 32:36], AF.Copy)
                s.tt("dve", bg[:], beta[:], E12[:, 0:4], ALU.mult)
                for hq in range(2):
                    s.mm(pG[hq][:, :], kTb[hq][:, tsl], kTb[hq][:, tsl])
                    s.mm(pQK[hq][:, :], kTb[hq][:, tsl], qTb[hq][:, tsl])
                HQ = [0, 0, 1, 1]
                cM = [None] * 4
                cMT = [None] * 4
                for hv in range(4):
                    hq = HQ[hv]
                    s.mm(pP1[hv][:, :], gB[:, hv, :], U[:], start=True, stop=False)
                    s.mm(pP1[hv][:, :], idf[:], mneg[:], start=False, stop=True)
                    s.act(Einc[hv][:], pP1[hv][:, :], AF.Exp, bias=ngc[:, hv:hv + 1])
                    s.mm(pP2[hv][:, :], gB[:, hv, :], U[:], start=True, stop=False)
                    s.mm(pP2[hv][:, :], idf[:], mstr[:], start=False, stop=True)
                    s.act(En[hv][:], pP2[hv][:, :], AF.Exp, bias=gcs[:, hv:hv + 1], scale=-1.0)
                for hv in range(4):
                    hq = HQ[hv]
                    s.tt("dve", attnT[hv][:], pQK[hq][:, :], Einc[hv][:], ALU.mult)
                    s.stt("dve", Mb[0][hv][:], En[hv][:], nbeta[:, hv:hv + 1], pG[hq][:, :], ALU.mult, ALU.mult)
                    cM[hv], cMT[hv] = Mb[0][hv], MTb[0][hv]
                for hv in range(4):
                    ptb = pA[hv].v(pA[hv].t[:, :].bitcast(BF16))
                    s.tr(pA[hv].v(ptb.ap[:, 0:128]), cM[hv][:], idb[:])
                for hv in range(4):
                    ptb = pA[hv].v(pA[hv].t[:, :].bitcast(BF16))
                    s.cp("act", cMT[hv][:], pA[hv].v(ptb.ap[:, 0:128]))
                for hv in range(4):
                    s.tt("dve", P32[hv][:], idf[:], cMT[hv][:], ALU.add)
                    s.cp("pool", Pbf[hv][:], P32[hv][:])
                for l in range(1, 7):
                    for hv in range(4):
                        s.mm(pB[hv][:, :], cMT[hv][:], cM[hv][:])
                        if l < 6:
                            s.mm(pC[hv][:, :], cM[hv][:], cMT[hv][:])
                    for hv in range(4):
                        s.cp("act", Mb[l % 2][hv][:], pB[hv][:, :])
                        if l < 6:
                            s.cp("dve", MTb[l % 2][hv][:], pC[hv][:, :])
                        cM[hv], cMT[hv] = Mb[l % 2][hv], MTb[l % 2][hv]
                    for hv in range(4):
                        s.mm(pD[hv][:, :], cM[hv][:], Pbf[hv][:])
                    for hv in range(4):
                        s.tt("dve", P32[hv][:], P32[hv][:], pD[hv][:, :], ALU.add)
                        s.cp("pool", Pbf[hv][:], P32[hv][:])
                for hv in range(4):
                    hq = HQ[hv]
                    s.ts("pool", vb[hv][:], v_tm[:, hv, :], beta[:, hv:hv + 1], ALU.mult)
                    s.ts("pool", kbg[hv][:], k_tm[:, hq, :], bg[:, hv:hv + 1], ALU.mult)
                    s.ts("pool", kdec[hv][:], k_tm[:, hq, :], E12[:, 4 + hv:5 + hv], ALU.mult)
                for hv in range(4):
                    s.mm(pB[hv][:, :], Pbf[hv][:], vb[hv][:])
                    s.mm(pC[hv][:, :], kbg[hv][:], Pbf[hv][:])
                for hv in range(4):
                    s.cp("act", u_sb[hv][:], pB[hv][:, :])
                    s.cp("act", wTb[hv][:], pC[hv][:, :])
                for hv in range(4):
                    cur = Sb[hv][si[hv] % 2]
                    s.mm(pD[hv][:, :], wTb[hv][:], cur[:])
                    s.mm(pA[hv][:, :], qTb[HQ[hv]][:, tsl], cur[:])
                for hv in range(4):
                    s.tt("dve", vnew[hv][:], u_sb[hv][:], pD[hv][:, :], ALU.subtract)
                for hv in range(4):
                    s.mm(pB[hv][:, :], attnT[hv][:], vnew[hv][:])
                    s.mm(pC[hv][:, :], kdec[hv][:], vnew[hv][:])
                for hv in range(4):
                    s.cp("act", oAs[hv][:], pB[hv][:, :])
                for hv in range(4):
                    s.stt("dve", osb_all[:, hv * 128:(hv + 1) * 128], pA[hv][:, :], E12[:, hv:hv + 1], oAs[hv][:], ALU.mult, ALU.add)
                    s.stt("dve", S[hv][:], S[hv][:], E12[:, 8 + hv:9 + hv], pC[hv][:, :], ALU.mult, ALU.add)
                    si[hv] += 1
                for hv in range(4):
                    s.cp("act", Sb[hv][si[hv] % 2][:], S[hv][:])
                out_stage(s, C, d, osb_all[:], PS[4][:, :], D, tok0, 512, 4, 128, sgate[:],
                          normB.v(normB.t[:, :, :].rearrange("p h v -> p (h v)")), Wk)


def PS1_to_sb(s, P, scratch):
    return P[:, :]
import ml_dtypes
from concourse.bass_utils import run_bass_kernel_spmd

NCORE = 8
_DEBUG_HOOK = None
SEQ = 16384
NT = 4096
GT_TOK = 512
BF = ml_dtypes.bfloat16


def _run(build, in_maps, outs):
    nc = bass.Bass("TRN2", target_bir_lowering=False)
    s = Sched(nc)
    D = {}
    for k, v in in_maps[0].items():
        dt = BF16 if v.dtype == BF else F32
        D[k] = s.dram(nc.dram_tensor(k, list(v.shape), dt, kind="ExternalInput").ap(), k)
    for k, (shape, dt) in outs.items():
        D[k] = s.dram(nc.dram_tensor(k, list(shape), dt, kind="ExternalOutput").ap(), k)
    build(s, nc, D)
    s.finish([D[k] for k in outs])
    res = run_bass_kernel_spmd(nc, in_maps, core_ids=list(range(len(in_maps))))
    return res.results


def prep_phase(s, nc, D, C, PS, nt):
    idf = C["idf"]
    xt = [s.sb(f"pxt{i}", [128, 1024], F32) for i in range(2)]
    xoT = [s.sb(f"pxoT{i}", [128, 8, 128], BF16) for i in range(2)]
    for tl in range(nt // 128):
        tok0 = tl * 128
        xc = xt[tl % 2]
        s.dma("sp", xc[:], D["xres"][tok0:tok0 + 128, :])
        for dc in range(8):
            s.tr(PS[dc // 4][:, (dc % 4) * 128:(dc % 4 + 1) * 128], xc[:, dc * 128:(dc + 1) * 128], idf[:])
        xo = xoT[tl % 2]
        for hh in range(2):
            s.cp("act" if hh == 0 else "dve", xo[:, hh * 4:(hh + 1) * 4, :], PS[hh].v(PS[hh].t[:, :].rearrange("p (c t) -> p c t", c=4)))
        s.dma("sp", D["xTout"].v(D["xTout"].t[:, tok0:tok0 + 128].rearrange("(c p) t -> p c t", p=128)), xo[:])


def _c(a):
    return np.ascontiguousarray(a)


def _mixer_inputs(layer, inp, xT_seq):
    maps = []
    for c in range(NCORE):
        sq, q = c // 4, c % 4
        m = {"xT": xT_seq[sq]}
        if layer == 0:
            w = inp["gdn_w_in"][0]
            qc = np.arange(q * 256, (q + 1) * 256); kc = 1024 + qc
            vc = 2048 + np.arange(q * 512, (q + 1) * 512); zc = 4096 + np.arange(q * 512, (q + 1) * 512)
            hv = np.arange(q * 4, q * 4 + 4)
            abc = 6144 + np.concatenate([hv, 32 + hv, 16 + hv, 48 + hv])
            cols = np.concatenate([qc, kc, vc, zc, abc]); ccols = np.concatenate([qc, kc, vc])
            m.update({"w_in": _c(w[:, cols]),
                      "convw": _c(inp["gdn_conv_w"][0][:, ccols].reshape(5, 8, 128).transpose(2, 1, 0)),
                      "alog": _c(inp["gdn_a_log"][0][:, hv]), "dtb": _c(inp["gdn_dt_bias"][0][:, hv]),
                      "normw": _c(inp["gdn_norm_w"][0][None])})
        elif layer == 1:
            w = inp["m2_w_in"][0]; g_ = q; x0 = 2048
            cols = np.concatenate([np.arange(x0 + g_ * 512, x0 + (g_ + 1) * 512), np.arange(x0 + 2048 + g_ * 128, x0 + 2048 + (g_ + 1) * 128),
                                   np.arange(x0 + 2560 + g_ * 128, x0 + 2560 + (g_ + 1) * 128), np.arange(g_ * 512, (g_ + 1) * 512),
                                   np.arange(5120 + g_ * 8, 5120 + g_ * 8 + 8), np.arange(5152 + g_ * 8, 5152 + g_ * 8 + 8)])
            ccols = np.concatenate([np.arange(g_ * 512, (g_ + 1) * 512), np.arange(2048 + g_ * 128, 2048 + (g_ + 1) * 128),
                                    np.arange(2560 + g_ * 128, 2560 + (g_ + 1) * 128)])
            m.update({"w_in": _c(w[:, cols]),
                      "convw": _c(inp["m2_conv_w"][0][:, ccols].reshape(5, 6, 128).transpose(2, 1, 0)),
                      "convb": _c(inp["m2_conv_b"][0][ccols].reshape(6, 128).T),
                      "alog": _c(inp["m2_a_log"][0][:, g_ * 8:(g_ + 1) * 8]), "dtb": _c(inp["m2_dt_bias"][0][:, g_ * 8:(g_ + 1) * 8]),
                      "dsk": _c(inp["m2_d"][0][None, g_ * 8:(g_ + 1) * 8]), "normw": _c(inp["m2_norm_w"][0][None, g_ * 512:(g_ + 1) * 512])})
        elif layer == 2:
            w = inp["hg_w_in"][0]; cs = slice(q * 256, (q + 1) * 256)
            lg = inp["hg_lb_logits"][:, cs]
            m.update({"w_in": _c(np.concatenate([w[:, k * 1024:(k + 1) * 1024][:, cs] for k in range(5)], axis=1)),
                      "lbrow": _c(lg[None]), "lbcol": _c(lg.reshape(4, 2, 128).transpose(2, 0, 1)),
                      "normw": _c(inp["hg_norm_w"][0][None])})
        else:
            w = inp["gla_w_in"][0]; ks = slice(q * 128, (q + 1) * 128); vs = slice(q * 256, (q + 1) * 256)
            m.update({"w_in": _c(np.concatenate([w[:, 0:512][:, ks], w[:, 512:1024][:, ks], w[:, 1024:2048][:, vs],
                                                 w[:, 2048:3072][:, vs], w[:, 3072:3104]], axis=1)),
                      "wgk": _c(inp["gla_w_gk"][0][:, :, ks]), "bgk": _c(inp["gla_b_gk"][0][:, ks]),
                      "normw": _c(inp["gla_norm_w"][0][None])})
        maps.append(m)
    return maps


MIX_W = [2048, 2048, 1024, 1024]
MIX_WOUT = ["gdn_w_out", "m2_w_out", "hg_w_out", "gla_w_out"]


def _token_inputs(layer, inp, oT_tok, xres):
    maps = []
    p = inp["p"][layer].reshape(2 * SEQ, 256)
    wrt = _c(np.concatenate([inp["moe_w_group"][layer], inp["moe_w_router"][layer]], axis=1).reshape(8, 128, 36).transpose(1, 0, 2))
    brt = _c(np.concatenate([inp["moe_b_group"][layer], inp["moe_b_router"][layer]])[None])
    shared = {"w_out": _c(inp[MIX_WOUT[layer]][0]), "lng": _c(inp["ln_g"][layer]), "lnb": _c(inp["ln_b"][layer]),
              "wrt": wrt, "brt": brt,
              "w_gate": _c(inp["moe_w_gate"][layer].reshape(32, 1024, 256)), "w_up": _c(inp["moe_w_up"][layer].reshape(32, 1024, 256)),
              "w_down": _c(inp["moe_w_down"][layer].reshape(32, 256, 1024)),
              "pe_g": _c(inp["pe_w_gate"][layer]), "pe_p": _c(inp["pe_w_proj"][layer])}
    for c in range(NCORE):
        m = {"oT": oT_tok[c], "xres": xres[c], "pl": _c(p[c * NT:(c + 1) * NT])}
        m.update(shared)
        maps.append(m)
    return maps


def kernel_unfused(**inp):
    inp = {k: np.asarray(v) for k, v in inp.items()}
    x = inp["x"].reshape(2 * SEQ, 1024)
    xres = [_c(x[c * NT:(c + 1) * NT]) for c in range(NCORE)]

    def b_prep(s, nc, D):
        C = make_consts(s)
        PS = [s.ps(f"ps{i}", [128, 512], F32) for i in range(8)]
        prep_phase(s, nc, D, C, PS, NT)
    r = _run(b_prep, [{"xres": xres[c]} for c in range(NCORE)], {"xTout": ([1024, NT], BF16)})
    xT = [np.asarray(r[c]["xTout"]) for c in range(NCORE)]

    for layer in range(4):
        xT_seq = [_c(np.concatenate(xT[4 * sq:4 * sq + 4], axis=1)) for sq in range(2)]
        Wq = MIX_W[layer] // 4

        def b_mix(s, nc, D, layer=layer, Wq=Wq):
            C = make_consts(s)
            make_masks(s, C)
            PS = [s.ps(f"ps{i}", [128, 512], F32) for i in range(8)]
            D["of"] = s.dram(nc.dram_tensor("of", [SEQ, Wq], F32, kind="Internal").ap(), "of")
            if layer == 0:
                make_masks_b(s, C); gdn_mixer(s, C, D, SEQ, PS)
            elif layer == 1:
                make_masks_b(s, C); ssd_mixer(s, C, D, SEQ, PS)
            elif layer == 2:
                hg_mixer(s, C, D, SEQ, PS)
            else:
                gla_mixer(s, C, D, SEQ, PS)
        r = _run(b_mix, _mixer_inputs(layer, inp, xT_seq), {"oT": ([Wq, SEQ], BF16)})
        oT = [np.asarray(r[c]["oT"]) for c in range(NCORE)]
        oT_tok = []
        for c in range(NCORE):
            sq, q = c // 4, c % 4
            oT_tok.append(_c(np.concatenate([oT[4 * sq + qq][:, q * NT:(q + 1) * NT] for qq in range(4)], axis=0)))
        last = layer == 3

        def b_tok(s, nc, D, layer=layer, last=last):
            C = make_consts(s)
            D["x1res"] = s.dram(nc.dram_tensor("x1res", [NT, 1024], F32, kind="Internal").ap(), "x1res")
            token_phase(s, C, D, NT, GT_TOK, MIX_W[layer], last)
        outs = {"xout": ([NT, 1024], F32)}
        if not last:
            outs["xTout"] = ([1024, NT], BF16)
        r = _run(b_tok, _token_inputs(layer, inp, oT_tok, xres), outs)
        xres = [np.asarray(r[c]["xout"]) for c in range(NCORE)]
        if _DEBUG_HOOK is not None:
            _DEBUG_HOOK(layer, xres, oT)
        if not last:
            xT = [np.asarray(r[c]["xTout"]) for c in range(NCORE)]
    out = np.concatenate(xres, axis=0).reshape(2, SEQ, 1024).astype(np.float32)
    return out

GROUPS = [[0, 1, 2, 3], [4, 5, 6, 7]]


def zero_dram(s, dtile, zt):
    n = 1
    for d in dtile.t.shape:
        n *= d
    k = n // (128 * 8192)
    names = " ".join(f"d{i}" for i in range(len(dtile.t.shape)))
    flat = dtile.t.rearrange(f"{names} -> ({names})").rearrange("(k p n) -> k p n", p=128, n=8192)
    for i in range(k):
        s.dma("sp", dtile.v(flat[i]), zt[:, :])


def build_fused(s, nc, D, Lz):
    C = make_consts(s)
    make_masks(s, C)
    make_masks_b(s, C)
    PS = [s.ps(f"ps{i}", [128, 512], F32) for i in range(8)]
    rv = nc.sync.partition_id() % 4

    def dr(name, shape, dt):
        return s.dram(nc.dram_tensor(name, list(shape), dt).ap(), name)
    xTm = dr("xTm", [1024, NT], BF16)
    obx = dr("obx", [4, 1024, NT], BF16)
    x1res = dr("x1res", [NT, 1024], F32)
    xres_pp = [dr("xresA", [NT, 1024], F32), dr("xresB", [NT, 1024], F32)]
    CC_BYTES = 4 * 1024 * 1024

    def make_exch(name, R, N):
        Rc = CC_BYTES // (4 * N * 2)
        nch = R // Rc
        return {"Rc": Rc, "n": nch, "ib": [dr(f"{name}_ib{c}", [4, Rc, N], BF16) for c in range(nch)],
                "ob": [dr(f"{name}_ob{c}", [4, Rc, N], BF16) for c in range(nch)]}
    EX = {"x": make_exch("ex", 1024, NT), 512: make_exch("eo512", 512, SEQ), 256: make_exch("eo256", 256, SEQ)}
    mark = s.mark()
    s.start_phases()
    zt = s.sb("zt", [128, 8192], BF16)
    s.memset("pool", zt[:], 0.0)
    for e in EX.values():
        for t in e["ib"]:
            zero_dram(s, t, zt)
    s.reset_to(mark)

    def exchange(e, src):
        Rc = e["Rc"]
        for c in range(e["n"]):
            ib, ob = e["ib"][c], e["ob"][c]
            s.dma("sp", ib.v(ib.t[bass.ds(rv, 1), :, :]), src.v(src.t[c * Rc:(c + 1) * Rc, :].rearrange("(o r) n -> o r n", o=1)))
            s.allreduce(ob[:, :, :], ib[:, :, :], GROUPS)

    def exchange_x():
        e = EX["x"]
        exchange(e, xTm)
        for c in range(e["n"]):
            s.dma("sp", obx[:, c * e["Rc"]:(c + 1) * e["Rc"], :], e["ob"][c][:, :, :])

    prep_phase(s, nc, {"xres": D["xres"], "xTout": xTm}, C, PS, NT)
    exchange_x()
    if "dbg_obx" in D:
        s.dma("sp", D["dbg_obx"][:, :, :], obx[:, :, :])
    s.reset_to(mark)
    cur_x = D["xres"]
    for layer in range(4):
        W = MIX_W[layer]
        Wq = W // 4
        last = layer == 3
        oTloc = dr(f"oTloc{layer}", [Wq, SEQ], BF16)
        of = dr(f"of{layer}", [SEQ, Wq], F32)
        oTtok = dr(f"oTtok{layer}", [W, NT], BF16)
        Dm = {k[len(f"M{layer}_"):]: v for k, v in D.items() if k.startswith(f"M{layer}_")}
        Dm.update({"xT": obx, "of": of, "oT": oTloc})
        [gdn_mixer, ssd_mixer, hg_mixer, gla_mixer][layer](s, C, Dm, SEQ, PS)
        e = EX[Wq]
        exchange(e, oTloc)
        ot3 = oTtok.t.rearrange("(q w) t -> q w t", q=4)
        for c in range(e["n"]):
            s.dma("sp", oTtok.v(ot3[:, c * e["Rc"]:(c + 1) * e["Rc"], :]), e["ob"][c].v(e["ob"][c].t[:, :, bass.ds(rv * NT, NT)]))
        if layer == 1 and "dbg_oTtok0" in D:
            s.dma("sp", D["dbg_oTtok0"][:, :], oTtok[:, :])
            s.dma("sp", D["dbg_oTloc0"][:, :], oTloc[:, :])
        s.reset_to(mark)
        Dt = {k[len(f"T{layer}_"):]: v for k, v in D.items() if k.startswith(f"T{layer}_")}
        nxt = D["out"] if last else xres_pp[layer % 2]
        Dt.update({"oT": oTtok, "xres": cur_x, "x1res": x1res, "xout": nxt, "xTout": xTm})
        token_phase(s, C, Dt, NT, GT_TOK, W, last, PS=PS)
        cur_x = nxt
        if layer == 1 and "dbg_x0" in D:
            s.dma("sp", D["dbg_x0"][:, :], nxt[:, :])
        if not last:
            exchange_x()
        s.reset_to(mark)


def kernel(**inp):
    inp = {k: np.asarray(v) for k, v in inp.items()}
    x = inp["x"].reshape(2 * SEQ, 1024)
    maps = [{"xres": _c(x[c * NT:(c + 1) * NT])} for c in range(NCORE)]
    for layer in range(4):
        mm = _mixer_inputs(layer, inp, [None, None])
        tm = _token_inputs(layer, inp, [None] * NCORE, [None] * NCORE)
        for c in range(NCORE):
            for k, v in mm[c].items():
                if k != "xT":
                    maps[c][f"M{layer}_{k}"] = v
            for k, v in tm[c].items():
                if k not in ("oT", "xres"):
                    maps[c][f"T{layer}_{k}"] = v

    def b(s, nc, D):
        build_fused(s, nc, D, None)
    outs = {"out": ([NT, 1024], F32)}
    if _DEBUG_HOOK is not None:
        outs.update({"dbg_obx": ([4, 1024, NT], BF16), "dbg_oTtok0": ([2048, NT], BF16), "dbg_oTloc0": ([512, SEQ], BF16), "dbg_x0": ([NT, 1024], F32)})
    r = _run(b, maps, outs)
    if _DEBUG_HOOK is not None:
        _DEBUG_HOOK(r)
    out = np.concatenate([np.asarray(r[c]["out"]) for c in range(NCORE)], axis=0).reshape(2, SEQ, 1024).astype(np.float32)
    return out
```

```python
import numpy as np
import concourse.bass as bass
import concourse.mybir as mybir

F32 = mybir.dt.float32
BF16 = mybir.dt.bfloat16
AF = mybir.ActivationFunctionType
ALU = mybir.AluOpType
AX = mybir.AxisListType


class View:
    __slots__ = ("tile", "ap")

    def __init__(self, tile, ap):
        self.tile = tile
        self.ap = ap


class Tile:
    def __init__(self, sch, tensor, name):
        self.sch = sch
        self.t = tensor
        self.name = name
        self.w = []
        self.r = []
        self.dsem = None
        self.dcnt = 0
        self.is_dram = False
        self.is_psum = False

    def __getitem__(self, k):
        return View(self, self.t[k])

    def v(self, ap):
        return View(self, ap)


class Sched:
    def __init__(self, nc):
        self.nc = nc
        self.E = {"pe": nc.tensor, "act": nc.scalar, "dve": nc.vector,
                  "pool": nc.gpsimd, "sp": nc.sync}
        self.sem = {k: nc.alloc_semaphore("sem_" + k) for k in ("pe", "act", "dve", "pool")}
        self.cnt = {k: 0 for k in self.sem}
        self.semid = {id(v): k for k, v in self.sem.items()}
        self.seen = {k: {} for k in self.E}
        self.nsem = 4
        self.ntile = 0
        self.last_out_tokens = []

    def sb(self, name, shape, dt):
        self.ntile += 1
        if not hasattr(self, "off"):
            self.off = 16384
            self.cap = 228800
        esz = 4 if dt == F32 else 2
        n = 1
        for d in shape[1:]:
            n *= d
        nbytes = (n * esz + 63) // 64 * 64
        if self.off + nbytes > self.cap:
            raise RuntimeError(f"SBUF overflow allocating {name}: {self.off}+{nbytes}")
        t = self.nc.alloc_sbuf_tensor_at(f"{name}_{self.ntile}", list(shape), dt, offset=self.off)
        self.off += nbytes
        return Tile(self, t, name)

    def mark(self):
        return getattr(self, "off", 16384)

    def reset_to(self, off):
        self.barrier()
        self.off = off
        ph = getattr(self, "phase", 0)
        keep = []
        for t in getattr(self, "_dsems", []):
            if getattr(t, "phase", 0) == ph and ph != 0:
                self._free_dsems.append((t.dsem, t.dcnt))
                t.dsem = None
            else:
                keep.append(t)
        self._dsems = keep
        self._recycled = getattr(self, "_recycled", []) + [x for x in self._free_dsems]
        self.phase = ph + 1

    def start_phases(self):
        self.phase = 1

    def barrier(self):
        dsems = getattr(self, "_dsems", [])
        for k in self.sem:
            pass
        for eng, e in self.E.items():
            seen = self.seen[eng]
            for k, sm in self.sem.items():
                if self.cnt[k] > 0 and seen.get(id(sm), 0) < self.cnt[k]:
                    e.wait_ge(sm, self.cnt[k]); seen[id(sm)] = self.cnt[k]
            for t in dsems:
                if t.dcnt > 0 and seen.get(id(t.dsem), 0) < t.dcnt:
                    e.wait_ge(t.dsem, t.dcnt); seen[id(t.dsem)] = t.dcnt
            for (sm, c) in getattr(self, "_free_dsems", []):
                if c > 0 and seen.get(id(sm), 0) < c:
                    e.wait_ge(sm, c); seen[id(sm)] = c
            if getattr(self, "ccsem", None) is not None and self.cccnt > 0 and seen.get(id(self.ccsem), 0) < self.cccnt:
                e.wait_ge(self.ccsem, self.cccnt); seen[id(self.ccsem)] = self.cccnt

    def ps(self, name, shape, dt):
        self.ntile += 1
        t = Tile(self, self.nc.alloc_psum_tensor(f"{name}_{self.ntile}", list(shape), dt), name)
        t.is_psum = True
        return t

    def dram(self, ap, name):
        t = Tile(self, ap, name)
        t.is_dram = True
        return t

    def _dsem(self, tile):
        if tile.dsem is None:
            if not hasattr(self, "_dsems"):
                self._dsems = []
                self._free_dsems = []
            if self._free_dsems:
                tile.dsem, tile.dcnt = self._free_dsems.pop()
            else:
                tile.dsem = self.nc.alloc_semaphore(f"d_{tile.name}_{self.nsem}")
                self.nsem += 1
                tile.dcnt = 0
            tile.phase = getattr(self, "phase", 0)
            self._dsems.append(tile)
        return tile.dsem

    def _waits(self, eng, outs, ins):
        need = {}
        own = self.sem.get(eng)
        for v in ins:
            for (s, val) in v.tile.w:
                need[id(s)] = (s, max(val, need.get(id(s), (s, 0))[1]))
            if v.tile.is_psum:
                for (s, val) in v.tile.r:
                    if s is not own:
                        need[id(s)] = (s, max(val, need.get(id(s), (s, 0))[1]))
        for v in outs:
            for (s, val) in v.tile.w + v.tile.r:
                need[id(s)] = (s, max(val, need.get(id(s), (s, 0))[1]))
        e = self.E[eng]
        seen = self.seen[eng]
        for sid, (s, val) in need.items():
            k = self.semid.get(sid)
            if k is not None and val > self.cnt[k]:
                if own is not None and s is own:
                    continue
                raise RuntimeError(f"wait on future inc of {k}: {val} > {self.cnt[k]}")
            if seen.get(sid, 0) < val:
                e.wait_ge(s, val)
                seen[sid] = val

    def _commit(self, outs, ins, tok):
        for v in ins:
            v.tile.r.append(tok)
            if len(v.tile.r) > 12:
                d = {}
                for (s, val) in v.tile.r:
                    d[id(s)] = (s, max(val, d.get(id(s), (s, 0))[1]))
                v.tile.r = list(d.values())
        for v in outs:
            if v.tile.is_dram:
                d = {}
                for (s_, val) in v.tile.w + [tok]:
                    d[id(s_)] = (s_, max(val, d.get(id(s_), (s_, 0))[1]))
                v.tile.w = list(d.values())
            else:
                v.tile.w = [tok]
            v.tile.r = []

    def op(self, eng, fn, outs, ins, inc=True):
        import os
        b = os.environ.get("OPBUDGET")
        self.nops = getattr(self, "nops", 0) + 1
        if b is not None and self.nops > int(b):
            return None
        self._waits(eng, outs, ins)
        ins_ = fn()
        s = self.sem[eng]
        if inc:
            ins_.then_inc(s, 1)
            self.cnt[eng] += 1
            tok = (s, self.cnt[eng])
        else:
            tok = (s, self.cnt[eng] + 1)
        self._commit(outs, ins, tok)
        return ins_

    def dma(self, q, out, in_, sbuf_side=None):
        self._waits(q, [out], [in_])
        st = sbuf_side if sbuf_side is not None else (in_.tile if out.tile.is_dram else out.tile)
        s = self._dsem(st)
        ins_ = self.E[q].dma_start(out=out.ap, in_=in_.ap)
        ins_.then_inc(s, 16)
        st.dcnt += 16
        tok = (s, st.dcnt)
        self._commit([out], [in_], tok)
        return tok

    def mm(self, out, lhsT, rhs, start=True, stop=True, inc=None):
        if inc is None:
            inc = stop
        return self.op("pe", lambda: self.nc.tensor.matmul(out.ap, lhsT=lhsT.ap, rhs=rhs.ap, start=start, stop=stop),
                       [out], [lhsT, rhs], inc=inc)

    def tr(self, out, in_, ident, inc=True):
        return self.op("pe", lambda: self.nc.tensor.transpose(out=out.ap, in_=in_.ap, identity=ident.ap),
                       [out], [in_, ident], inc=inc)

    def act(self, out, in_, func, bias=None, scale=None, accum=None):
        ins = [in_]
        kw = {}
        if bias is not None:
            if isinstance(bias, View):
                ins.append(bias); kw["bias"] = bias.ap
            else:
                kw["bias"] = bias
        if scale is not None:
            if isinstance(scale, View):
                ins.append(scale); kw["scale"] = scale.ap
            else:
                kw["scale"] = scale
        outs = [out]
        if accum is not None:
            outs.append(accum); kw["accum_out"] = accum.ap
        return self.op("act", lambda: self.nc.scalar.activation(out=out.ap, in_=in_.ap, func=func, **kw), outs, ins)

    def _e(self, eng):
        return self.E[eng]

    def tt(self, eng, out, a, b, op):
        return self.op(eng, lambda: self._e(eng).tensor_tensor(out=out.ap, in0=a.ap, in1=b.ap, op=op), [out], [a, b])

    def ts(self, eng, out, a, s1, op0, s2=None, op1=None):
        ins = [a]
        a1 = s1.ap if isinstance(s1, View) else s1
        a2 = s2.ap if isinstance(s2, View) else s2
        if isinstance(s1, View): ins.append(s1)
        if isinstance(s2, View): ins.append(s2)
        if op1 is None:
            return self.op(eng, lambda: self._e(eng).tensor_scalar(out=out.ap, in0=a.ap, scalar1=a1, scalar2=None, op0=op0), [out], ins)
        return self.op(eng, lambda: self._e(eng).tensor_scalar(out=out.ap, in0=a.ap, scalar1=a1, scalar2=a2, op0=op0, op1=op1), [out], ins)

    def stt(self, eng, out, a, sc, b, op0, op1):
        ins = [a, b]
        s_ = sc.ap if isinstance(sc, View) else sc
        if isinstance(sc, View): ins.append(sc)
        return self.op(eng, lambda: self._e(eng).scalar_tensor_tensor(out=out.ap, in0=a.ap, scalar=s_, in1=b.ap, op0=op0, op1=op1), [out], ins)

    def cp(self, eng, out, a):
        if eng == "act":
            return self.op("act", lambda: self.nc.scalar.copy(out=out.ap, in_=a.ap), [out], [a])
        return self.op(eng, lambda: self._e(eng).tensor_copy(out=out.ap, in_=a.ap), [out], [a])

    def memset(self, eng, out, val):
        return self.op(eng, lambda: self._e(eng).memset(out.ap, val), [out], [])

    def allreduce(self, out, in_, groups):
        if getattr(self, "ccsem", None) is None:
            self.ccsem = self.nc.alloc_semaphore("ccsem")
            self.cccnt = 0
        self._waits("pool", [out], [in_])
        ins_ = self.nc.gpsimd.collective_compute("AllReduce", ALU.add, replica_groups=groups, ins=[in_.ap], outs=[out.ap])
        ins_.then_inc(self.ccsem)
        self.cccnt += 1
        tok = (self.ccsem, self.cccnt)
        self._commit([out], [in_], tok)

    def finish(self, dram_tiles):
        e = self.E["sp"]
        for t in dram_tiles:
            for (s, val) in t.w:
                e.wait_ge(s, val)

ALPHA = float((2 * 4) ** 0.25)
LN_EPS = 1e-5
DM = 1024
NEG_BIG = -30000.0


def make_consts(s):
    nc = s.nc
    C = {}
    idf = s.sb("identf", [128, 128], F32)
    s.memset("pool", idf[:], 1.0)
    s.op("pool", lambda: nc.gpsimd.affine_select(out=idf.t[:], in_=idf.t[:], pattern=[[-1, 128]],
                                                 compare_op=ALU.is_equal, fill=0.0, base=0, channel_multiplier=1),
         [idf[:]], [idf[:]])
    idb = s.sb("identb", [128, 128], BF16)
    s.cp("pool", idb[:], idf[:])
    C["idf"] = idf
    C["idb"] = idb
    return C


def layernorm_tile(s, y, xn, out, gam, bet, st6, mv, rstd, nmr):
    nc = s.nc
    for hf in range(2):
        s.op("dve", lambda hf=hf: nc.vector.bn_stats(out=st6.t[:, hf * 6:(hf + 1) * 6], in_=y.t[:, hf * 512:(hf + 1) * 512]),
             [st6[:]], [y[:]])
    s.op("dve", lambda: nc.vector.bn_aggr(out=mv.t[:], in_=st6.t[:]), [mv[:]], [st6[:]])
    s.act(rstd[:], mv[:, 1:2], AF.Ln, bias=LN_EPS)
    s.act(rstd[:], rstd[:], AF.Exp, scale=-0.5)
    s.stt("dve", nmr[:], mv[:, 0:1], -1.0, rstd[:], ALU.mult, ALU.mult)
    s.act(xn[:], y[:], AF.Identity, bias=nmr[:], scale=rstd[:])
    s.tt("pool", xn[:], xn[:], gam[:], ALU.mult)
    s.tt("pool", out[:], xn[:], bet[:], ALU.add)


def token_phase(s, C, D, NT, GT, W, last, upto=9, PS=None, after_group=None):
    nc = s.nc
    WC = W // 128
    idf, idb = C["idf"], C["idb"]
    lng = [s.sb(f"lng{i}", [128, DM], F32) for i in range(2)]
    lnb = [s.sb(f"lnb{i}", [128, DM], F32) for i in range(2)]
    for i in range(2):
        s.dma("sp", lng[i][:], D["lng"].v(D["lng"].t[i:i + 1, :].partition_broadcast(128)))
        s.dma("sp", lnb[i][:], D["lnb"].v(D["lnb"].t[i:i + 1, :].partition_broadcast(128)))
    wr = s.sb("wr", [128, 8, 36], F32)
    s.dma("sp", wr[:], D["wrt"][:, :, :])
    wrh = s.sb("wrh", [128, 8, 36], BF16)
    wrl = s.sb("wrl", [128, 8, 36], BF16)
    wrt_ = s.sb("wrt_", [128, 8, 36], F32)
    s.cp("dve", wrh[:], wr[:])
    s.tt("dve", wrt_[:], wr[:], wrh[:], ALU.subtract)
    s.cp("dve", wrl[:], wrt_[:])
    brt = s.sb("brt", [128, 36], F32)
    s.dma("sp", brt[:], D["brt"].v(D["brt"].t[0:1, :].partition_broadcast(128)))
    split_w = (W <= 1024)
    wbig_n = (WC * DM + 10 * DM) if split_w else max(WC * DM, 10 * DM)
    wbig = s.sb("wbig", [128, wbig_n], BF16)
    wo_t = s.sb("wo_t", [128, 8], BF16) if False else None
    sel = s.sb("sel", [32, 32, 128], BF16)
    s.memset("pool", sel[:], 1.0)
    s.op("pool", lambda: nc.gpsimd.affine_select(out=sel.t[:], in_=sel.t[:], pattern=[[-1, 32], [0, 128]],
                                                 compare_op=ALU.is_equal, fill=0.0, base=0, channel_multiplier=1),
         [sel[:]], [sel[:]])

    if PS is None:
        PS = [s.ps(f"ps{i}", [128, 512], F32) for i in range(8)]

    xT1 = s.sb("x1T", [128, 8, GT], BF16)
    acc = s.sb("acc", [128, GT // 128, DM], F32)
    combT = [s.sb(f"combT{i}", [32, GT], BF16) for i in range(2)]
    oTb = [s.sb(f"oTb{i}", [128, WC, 256], BF16) for i in range(2)]
    xt = [s.sb(f"xt{i}", [128, DM], F32) for i in range(2)]
    y = s.sb("y", [128, DM], F32)
    xn = s.sb("xn", [128, DM], F32)
    x1 = [s.sb(f"x1_{i}", [128, DM], F32) for i in range(2)]
    xTf = s.sb("xTf", [128, 8, 128], F32)
    xTl = s.sb("xTl", [128, 8, 128], BF16)
    st6 = s.sb("st6", [128, 12], F32)
    mv = s.sb("mv", [128, 2], F32)
    rstd = s.sb("rstd", [128, 1], F32)
    nmr = s.sb("nmr", [128, 1], F32)
    lg = s.sb("lg", [128, 36], F32)
    sm = {k: s.sb("sm_" + k, [128, n], F32) for k, n in
          [("gmax", 1), ("goh", 4), ("gex", 4), ("gsum", 1), ("gp", 1), ("es", 8), ("m1", 1), ("k1", 8),
           ("e2", 8), ("m2", 1), ("k2", 8), ("d", 1), ("w1", 1), ("w2", 1), ("wi", 8), ("comb", 32), ("lo", 32)]}
    combh = s.sb("combh", [128, 32], BF16)
    combl = s.sb("combl", [128, 32], BF16)
    wg = [s.sb(f"wg{i}", [128, 8, 256], BF16) for i in range(2)]
    wu = [s.sb(f"wu{i}", [128, 8, 256], BF16) for i in range(2)]
    wd = [s.sb(f"wd{i}", [128, 2, DM], BF16) for i in range(2)]
    cb = [s.sb(f"cb{i}", [128, 512], F32) for i in range(2)]
    sg = [s.sb(f"sg{i}", [128, 512], F32) for i in range(2)]
    tmp = [s.sb(f"tmp{i}", [128, 512], F32) for i in range(2)]
    hT = [[s.sb(f"hT{i}{f}", [128, 512], BF16) for f in range(2)] for i in range(2)]
    x2 = s.sb("x2", [128, DM], F32)
    x2T = s.sb("x2T", [128, 8, 128], BF16)
    pt = s.sb("pt", [128, 256], F32)
    pT = s.sb("pT", [128, 2, 128], BF16)
    sgt = s.sb("sgt", [128, DM], F32)
    xo = [s.sb(f"xo{i}", [128, DM], F32) for i in range(1)]
    xoT = [s.sb(f"xoT{i}", [128, 8, 128], BF16) for i in range(1)]

    NG = NT // GT
    TPG = GT // 128
    pre_bf = "wbf_gate" in D
    if pre_bf:
        for e in range(32):
            s.dma("pool", D["wbf_gate"][e], D["w_gate"][e])
            s.dma("pool", D["wbf_up"][e], D["w_up"][e])
            s.dma("pool", D["wbf_down"][e], D["w_down"][e])
    for g in range(NG):
        t0g = g * GT
        wo0 = 10 * DM if split_w else 0
        wo = wbig.v(wbig.t[:, wo0:wo0 + WC * DM].rearrange("p (c n) -> p c n", c=WC))
        if g == 0 or not split_w:
            s.dma("pool", wo, D["w_out"].v(D["w_out"].t.rearrange("(c p) n -> p c n", p=128)))
        for tl in range(TPG):
            tok0 = t0g + tl * 128
            if tl % 2 == 0:
                ob = oTb[(tl // 2) % 2]
                nb = min(256, GT - tl * 128)
                s.dma("sp", ob[:, :, 0:nb], D["oT"].v(D["oT"].t[:, tok0:tok0 + nb].rearrange("(c p) t -> p c t", p=128)))
            xc = xt[tl % 2]
            s.dma("sp", xc[:], D["xres"][tok0:tok0 + 128, :])
            o_off = (tl % 2) * 128
            for hf in range(2):
                for wc in range(WC):
                    s.mm(PS[hf][:, :], ob[:, wc, o_off:o_off + 128], wo.tile.v(wo.ap[:, wc, hf * 512:(hf + 1) * 512]),
                         start=(wc == 0), stop=(wc == WC - 1))
            for hf in range(2):
                s.stt("dve", y[:, hf * 512:(hf + 1) * 512], xc[:, hf * 512:(hf + 1) * 512], ALPHA, PS[hf][:, :], ALU.mult, ALU.add)
            x1c = x1[tl % 2]
            layernorm_tile(s, y, xn, x1c, lng[0], lnb[0], st6, mv, rstd, nmr)
            s.dma("sp", D["x1res"][tok0:tok0 + 128, :], x1c[:])
            if upto == 1:
                s.dma("sp", D["xout"][tok0:tok0 + 128, :], x1c[:])
                continue
            for dc in range(8):
                pt_ = PS[2 + dc // 4]
                s.tr(pt_[:, (dc % 4) * 128:(dc % 4 + 1) * 128], x1c[:, dc * 128:(dc + 1) * 128], idf[:])
            for hh in range(2):
                s.cp("act", xTf.v(xTf.t[:, hh * 4:(hh + 1) * 4, :].rearrange("p c t -> p (c t)")), PS[2 + hh][:, :])
            s.cp("dve", xT1[:, :, tl * 128:(tl + 1) * 128], xTf[:])
            if upto == 2:
                s.dma("sp", D["xout"][tok0:tok0 + 128, :], xTf.v(xTf.t[:, :, :].rearrange("p c t -> p (c t)")))
                continue
            xh = xT1[:, :, tl * 128:(tl + 1) * 128]
            s.tt("dve", xTf[:], xTf[:], xh, ALU.subtract)
            s.cp("dve", xTl[:], xTf[:])
            k_ = 0
            for (xa, wa) in ((xh, wrh), (xTl[:], wrh), (xh, wrl)):
                for dc in range(8):
                    s.mm(PS[4][:, 0:36], xa.tile.v(xa.ap[:, dc, :]), wa[:, dc, :], start=(k_ == 0), stop=(k_ == 23))
                    k_ += 1
            s.tt("dve", lg[:], PS[4][:, 0:36], brt[:], ALU.add)
            routing(s, lg, sm)
            if upto == 3:
                s.dma("sp", D["xout"][tok0:tok0 + 128, 0:32], sm["comb"][:])
                continue
            s.cp("dve", combh[:], sm["comb"][:])
            s.tt("dve", sm["lo"][:], sm["comb"][:], combh[:], ALU.subtract)
            s.cp("dve", combl[:], sm["lo"][:])
            pb = PS[5].v(PS[5].t[:, :].bitcast(BF16))
            s.tr(PS[5].v(pb.ap[0:32, 0:128]), combh[:], idb[:])
            s.tr(PS[5].v(pb.ap[0:32, 128:256]), combl[:], idb[:])
            s.cp("act", combT[0][:, tl * 128:(tl + 1) * 128], PS[5].v(pb.ap[0:32, 0:128]))
            s.cp("act", combT[1][:, tl * 128:(tl + 1) * 128], PS[5].v(pb.ap[0:32, 128:256]))
        if upto <= 3:
            continue
        if upto == 4:
            s.dma("sp", D["xout"][0:32, 0:GT], combT[0][:])
            continue
        NB = GT // 512 if GT >= 512 else 1
        BW = min(512, GT)
        assert NB == 1

        def load_gu(e):
            b = e % 2
            if pre_bf:
                s.dma("sp", wg[b][:], D["wbf_gate"].v(D["wbf_gate"].t[e].rearrange("(c p) f -> p c f", p=128)))
                s.dma("sp", wu[b][:], D["wbf_up"].v(D["wbf_up"].t[e].rearrange("(c p) f -> p c f", p=128)))
            else:
                s.dma("pool", wg[b][:], D["w_gate"].v(D["w_gate"].t[e].rearrange("(c p) f -> p c f", p=128)))
                s.dma("pool", wu[b][:], D["w_up"].v(D["w_up"].t[e].rearrange("(c p) f -> p c f", p=128)))

        def load_d(e):
            b = e % 2
            if pre_bf:
                s.dma("sp", wd[b][:], D["wbf_down"].v(D["wbf_down"].t[e].rearrange("(c p) n -> p c n", p=128)))
            else:
                s.dma("pool", wd[b][:], D["w_down"].v(D["w_down"].t[e].rearrange("(c p) n -> p c n", p=128)))

        bs = slice(0, BW)

        def comb(e):
            k = e % 2
            s.mm(PS[0][:, 0:BW], sel[:, e, :], combT[0][:, bs], start=True, stop=False)
            s.mm(PS[0][:, 0:BW], sel[:, e, :], combT[1][:, bs], start=False, stop=True)
            s.cp("act", cb[k][:, 0:BW], PS[0][:, 0:BW])

        def gate_up(e, fc):
            b, k = e % 2, e % 2
            gP, uP = PS[1 + 2 * fc], PS[2 + 2 * fc]
            for dc in range(8):
                s.mm(gP[:, 0:BW], wg[b][:, dc, fc * 128:(fc + 1) * 128], xT1[:, dc, bs], start=(dc == 0), stop=(dc == 7))
            for dc in range(8):
                s.mm(uP[:, 0:BW], wu[b][:, dc, fc * 128:(fc + 1) * 128], xT1[:, dc, bs], start=(dc == 0), stop=(dc == 7))
            s.act(sg[fc][:, 0:BW], gP[:, 0:BW], AF.Silu)
            s.tt("dve", tmp[fc][:, 0:BW], sg[fc][:, 0:BW], uP[:, 0:BW], ALU.mult)
            s.tt("pool", hT[k][fc][:, 0:BW], tmp[fc][:, 0:BW], cb[k][:, 0:BW], ALU.mult)

        def down(e):
            b, k = e % 2, e % 2
            for tt_ in range(BW // 128):
                for hf in range(2):
                    dP = PS[5 + (tt_ * 2 + hf) % 3]
                    for fc in range(2):
                        s.mm(dP[:, :], hT[k][fc][:, tt_ * 128:(tt_ + 1) * 128], wd[b][:, fc, hf * 512:(hf + 1) * 512],
                             start=(fc == 0), stop=(fc == 1))
                    a_ = acc[:, tt_, hf * 512:(hf + 1) * 512]
                    if e == 0:
                        s.cp("act", a_, dP[:, :])
                    else:
                        s.tt("dve", a_, a_, dP[:, :], ALU.add)

        load_gu(0); load_d(0); load_gu(1); load_d(1)
        comb(0)
        gate_up(0, 0)
        gate_up(0, 1)
        load_gu(2)
        for e in range(32):
            if e + 1 < 32:
                comb(e + 1)
                gate_up(e + 1, 0)
            down(e)
            if e + 2 < 32:
                load_d(e + 2)
            if e + 1 < 32:
                gate_up(e + 1, 1)
                if e + 3 < 32:
                    load_gu(e + 3)
        if upto == 5:
            for tl in range(TPG):
                s.dma("sp", D["xout"][t0g + tl * 128:t0g + (tl + 1) * 128, :], acc[:, tl, :])
            continue
        wpg = wbig.v(wbig.t[:, 0:8 * DM].rearrange("p (c n) -> p c n", c=8))
        wpp = wbig.v(wbig.t[:, 8 * DM:10 * DM].rearrange("p (c n) -> p c n", c=2))
        if g == 0 or not split_w:
            s.dma("pool", wpg, D["pe_g"].v(D["pe_g"].t.rearrange("(c p) n -> p c n", p=128)))
            s.dma("pool", wpp, D["pe_p"].v(D["pe_p"].t.rearrange("(c p) n -> p c n", p=128)))
        for tl in range(TPG):
            tok0 = t0g + tl * 128
            xc = xt[tl % 2]
            s.dma("sp", xc[:], D["x1res"][tok0:tok0 + 128, :])
            s.dma("sp", pt[:], D["pl"][tok0:tok0 + 128, :])
            s.stt("dve", y[:], xc[:], ALPHA, acc[:, tl, :], ALU.mult, ALU.add)
            layernorm_tile(s, y, xn, x2, lng[1], lnb[1], st6, mv, rstd, nmr)
            for dc in range(8):
                pt_ = PS[dc // 4]
                s.tr(pt_[:, (dc % 4) * 128:(dc % 4 + 1) * 128], x2[:, dc * 128:(dc + 1) * 128], idf[:])
            for hh in range(2):
                s.cp("act" if hh == 0 else "dve", x2T.v(x2T.t[:, hh * 4:(hh + 1) * 4, :].rearrange("p c t -> p (c t)")), PS[hh][:, :])
            for pc in range(2):
                s.tr(PS[2][:, pc * 128:(pc + 1) * 128], pt[:, pc * 128:(pc + 1) * 128], idf[:])
            s.cp("act", pT.v(pT.t[:, :, :].rearrange("p c t -> p (c t)")), PS[2][:, 0:256])
            for hf in range(2):
                for dc in range(8):
                    s.mm(PS[3 + hf][:, :], x2T[:, dc, :], wbig.v(wpg.ap[:, dc, hf * 512:(hf + 1) * 512]), start=(dc == 0), stop=(dc == 7))
                for pc in range(2):
                    s.mm(PS[5 + hf][:, :], pT[:, pc, :], wbig.v(wpp.ap[:, pc, hf * 512:(hf + 1) * 512]), start=(pc == 0), stop=(pc == 1))
            xoc = xo[0]
            for hf in range(2):
                hs = slice(hf * 512, (hf + 1) * 512)
                s.act(sgt[:, hs], PS[3 + hf][:, :], AF.Sigmoid)
                s.tt("dve", sgt[:, hs], sgt[:, hs], PS[5 + hf][:, :], ALU.mult)
                s.tt("pool", xoc[:, hs], sgt[:, hs], x2[:, hs], ALU.add)
            s.dma("sp", D["xout"][tok0:tok0 + 128, :], xoc[:])
            if not last:
                for dc in range(8):
                    pt_ = PS[dc // 4]
                    s.tr(pt_[:, (dc % 4) * 128:(dc % 4 + 1) * 128], xoc[:, dc * 128:(dc + 1) * 128], idf[:])
                xoTc = xoT[0]
                for hh in range(2):
                    s.cp("act" if hh == 0 else "dve", xoTc.v(xoTc.t[:, hh * 4:(hh + 1) * 4, :].rearrange("p c t -> p (c t)")), PS[hh][:, :])
                s.dma("sp", D["xTout"].v(D["xTout"].t[:, tok0:tok0 + 128].rearrange("(c p) t -> p c t", p=128)), xoTc[:])
        if after_group is not None and not last:
            after_group(g)


def routing(s, lg, sm):
    nc = s.nc
    gl = lg[:, 0:4]
    s.op("dve", lambda: nc.vector.reduce_max(out=sm["gmax"].t[:], in_=lg.t[:, 0:4], axis=AX.X), [sm["gmax"][:]], [lg[:]])
    s.ts("dve", sm["goh"][:], gl, sm["gmax"][:], ALU.is_equal)
    s.ts("dve", sm["gex"][:], gl, sm["gmax"][:], ALU.subtract)
    s.act(sm["gex"][:], sm["gex"][:], AF.Exp)
    s.op("dve", lambda: nc.vector.reduce_sum(out=sm["gsum"].t[:], in_=sm["gex"].t[:], axis=AX.X), [sm["gsum"][:]], [sm["gex"][:]])
    s.op("dve", lambda: nc.vector.reciprocal(out=sm["gp"].t[:], in_=sm["gsum"].t[:]), [sm["gp"][:]], [sm["gsum"][:]])
    s.ts("dve", sm["es"][:], lg[:, 4:12], sm["goh"][:, 0:1], ALU.mult)
    for g in range(1, 4):
        s.stt("dve", sm["es"][:], lg[:, 4 + 8 * g:12 + 8 * g], sm["goh"][:, g:g + 1], sm["es"][:], ALU.mult, ALU.add)
    s.op("dve", lambda: nc.vector.reduce_max(out=sm["m1"].t[:], in_=sm["es"].t[:], axis=AX.X), [sm["m1"][:]], [sm["es"][:]])
    s.ts("dve", sm["k1"][:], sm["es"][:], sm["m1"][:], ALU.is_equal)
    s.stt("dve", sm["e2"][:], sm["k1"][:], NEG_BIG, sm["es"][:], ALU.mult, ALU.add)
    s.op("dve", lambda: nc.vector.reduce_max(out=sm["m2"].t[:], in_=sm["e2"].t[:], axis=AX.X), [sm["m2"][:]], [sm["e2"][:]])
    s.ts("dve", sm["k2"][:], sm["e2"][:], sm["m2"][:], ALU.is_equal)
    s.tt("dve", sm["d"][:], sm["m2"][:], sm["m1"][:], ALU.subtract)
    s.act(sm["d"][:], sm["d"][:], AF.Exp)
    s.ts("dve", sm["d"][:], sm["d"][:], 1.0, ALU.add)
    s.op("dve", lambda: nc.vector.reciprocal(out=sm["w1"].t[:], in_=sm["d"].t[:]), [sm["w1"][:]], [sm["d"][:]])
    s.ts("dve", sm["w2"][:], sm["w1"][:], -1.0, ALU.mult, 1.0, ALU.add)
    s.tt("dve", sm["w1"][:], sm["w1"][:], sm["gp"][:], ALU.mult)
    s.tt("dve", sm["w2"][:], sm["w2"][:], sm["gp"][:], ALU.mult)
    s.ts("dve", sm["wi"][:], sm["k1"][:], sm["w1"][:], ALU.mult)
    s.stt("dve", sm["wi"][:], sm["k2"][:], sm["w2"][:], sm["wi"][:], ALU.mult, ALU.add)
    for g in range(4):
        s.ts("dve", sm["comb"][:, 8 * g:8 * g + 8], sm["wi"][:], sm["goh"][:, g:g + 1], ALU.mult)

NORM_EPS = 1e-6


def xT_view(D, lo, hi):
    t = D["xT"]
    if len(t.t.shape) == 2:
        return t.v(t.t[:, lo:hi].rearrange("(c p) t -> p c t", p=128))
    slot = lo // 4096
    assert (hi - 1) // 4096 == slot
    return t.v(t.t[slot, :, lo - slot * 4096:hi - slot * 4096].rearrange("(c p) t -> p c t", p=128))


def make_masks(s, C):
    nc = s.nc

    def tri(name, pat, cm, op):
        t = s.sb(name, [128, 128], F32)
        s.memset("pool", t[:], 1.0)
        s.op("pool", lambda: nc.gpsimd.affine_select(out=t.t[:], in_=t.t[:], pattern=[[pat, 128]], compare_op=op,
                                                     fill=0.0, base=0, channel_multiplier=cm), [t[:]], [t[:]])
        return t
    C["U128"] = [tri("U128f", 1, -1, ALU.is_ge), tri("U128b", -1, 1, ALU.is_ge)]
    C["SU128"] = [tri("SU128f", -1, 1, ALU.is_gt), tri("SU128b", 1, -1, ALU.is_gt)]
    U64 = [tri("U64f", 1, -1, ALU.is_ge), tri("U64b", -1, 1, ALU.is_ge)]
    SU64 = [tri("SU64f", -1, 1, ALU.is_gt), tri("SU64b", 1, -1, ALU.is_gt)]
    s.memset("pool", U64[0][0:64, 64:128], 0.0)
    s.memset("pool", U64[1][64:128, 0:64], 0.0)
    s.memset("pool", SU64[0][64:128, 0:64], 0.0)
    s.memset("pool", SU64[1][0:64, 64:128], 0.0)
    C["U64"] = U64
    C["SU64"] = SU64
    ones = s.sb("ones", [128, 128], F32)
    s.memset("pool", ones[:], 1.0)
    C["ones"] = ones
    C["U64m"] = U64
    return C


def formA_tile(s, C, d, PSg, PSa, PSo, qT, kT, k_tm, g_tm, v_tm, V, St, W):
    nc = s.nc
    U, SU = C["U64"][d], C["SU64"][d]
    gcT = PSg.tile.v(PSg.ap[:, 0:128])
    dec = PSg.tile.v(PSg.ap[:, 128:256])
    s.mm(gcT, g_tm, U[:])
    s.mm(dec, SU[:], g_tm)
    Eg, Eng, Ed = W["Eg"], W["Eng"], W["Ed"]
    s.act(Eg[:], gcT, AF.Exp)
    s.act(Eng[:], gcT, AF.Exp, scale=-1.0)
    s.act(Ed[:], dec, AF.Exp)
    QdT, KdT, Kl = W["QdT"], W["KdT"], W["Kl"]
    s.tt("dve", QdT[:], qT, Eg[:], ALU.mult)
    s.tt("pool", KdT[:], kT, Eng[:], ALU.mult)
    s.tt("pool", Kl[:], k_tm, Ed[:], ALU.mult)
    AT = PSa.tile.v(PSa.ap[:, 0:128])
    dS = PSa.tile.v(PSa.ap[:, 128:128 + V])
    s.mm(AT, KdT[:], QdT[:])
    ATm = W["ATm"]
    s.tt("dve", ATm[:], AT, U[:], ALU.mult)
    first, second = (slice(0, 64), slice(64, 128)) if d == 0 else (slice(64, 128), slice(0, 64))
    e_first, e_second = (63, 127) if d == 0 else (64, 0)
    S = St["S"]
    Sb = St["Sb"]
    cur = Sb[St["i"] % 2]
    s.mm(PSo.tile.v(PSo.ap[first, :]), ATm[:, first], v_tm, start=True, stop=False)
    s.mm(PSo.tile.v(PSo.ap[first, :]), QdT[:, first], cur[:], start=False, stop=True)
    s.mm(dS, Kl[first, :], v_tm.tile.v(v_tm.ap[first, :]))
    s.stt("dve", S[:], S[:], Eg[:, e_first:e_first + 1], dS, ALU.mult, ALU.add)
    St["i"] += 1
    nxt = Sb[St["i"] % 2]
    s.cp("act", nxt[:], S[:])
    s.mm(PSo.tile.v(PSo.ap[second, :]), ATm[:, second], v_tm, start=True, stop=False)
    s.mm(PSo.tile.v(PSo.ap[second, :]), QdT[:, second], nxt[:], start=False, stop=True)
    s.mm(dS, Kl[second, :], v_tm.tile.v(v_tm.ap[second, :]))
    s.stt("dve", S[:], S[:], Eg[:, e_second:e_second + 1], dS, ALU.mult, ALU.add)
    St["i"] += 1
    nxt2 = Sb[St["i"] % 2]
    s.cp("act", nxt2[:], S[:])


def formA_work(s, V):
    W = {}
    for n in ("Eg", "Eng", "Ed"):
        W[n] = s.sb(n, [128, 128], F32)
    for n in ("QdT", "KdT", "Kl", "ATm"):
        W[n] = s.sb(n, [128, 128], BF16)
    return W


def out_stage(s, C, d, PSo_all, PSt, D, tok0, ncol, nh, V, sgate, normB, Wk, col0=0, pre_gate=False):
    nc = s.nc
    osb = Wk["osb"][Wk["oi"] % 2]
    Wk["oi"] += 1
    if d == 0:
        s.cp("act", osb[:, 0:ncol], PSo_all)
        s.dma("sp", D["of"].v(D["of"].t[tok0:tok0 + 128, col0:col0 + ncol]), osb[:, 0:ncol])
        return
    ofl = Wk["ofl"]
    s.dma("sp", ofl[:, 0:ncol], D["of"].v(D["of"].t[tok0:tok0 + 128, col0:col0 + ncol]))
    s.tt("dve", osb[:, 0:ncol], ofl[:, 0:ncol], PSo_all, ALU.add)
    ss, rs, sq = Wk["ss"], Wk["rs"], Wk["sq"]
    if pre_gate:
        s.tt("dve", osb[:, 0:ncol], osb[:, 0:ncol], sgate, ALU.mult)
    s.memset("pool", ss[:], 0.0)
    for h in range(nh):
        s.act(sq[:, 0:V], osb[:, h * V:(h + 1) * V], AF.Square, accum=ss[:, h:h + 1])
    s.act(rs[:, 0:nh], ss[:, 0:nh], AF.Ln, bias=NORM_EPS, scale=1.0 / V)
    s.act(rs[:, 0:nh], rs[:, 0:nh], AF.Exp, scale=-0.5)
    og = Wk["og"]
    for h in range(nh):
        if pre_gate:
            s.ts("dve", osb[:, h * V:(h + 1) * V], osb[:, h * V:(h + 1) * V], rs[:, h:h + 1], ALU.mult)
        else:
            s.stt("dve", osb[:, h * V:(h + 1) * V], osb[:, h * V:(h + 1) * V], rs[:, h:h + 1], sgate.tile.v(sgate.ap[:, h * V:(h + 1) * V]),
                  ALU.mult, ALU.mult)
    s.tt("pool", og[:, 0:ncol], osb[:, 0:ncol], normB, ALU.mult)
    ptb = PSt.tile.v(PSt.ap.bitcast(BF16))
    nchunk = ncol // 128
    oTs = Wk["oTs"][Wk["oi"] % 2]
    for c in range(nchunk):
        s.tr(PSt.tile.v(ptb.ap[:, c * 128:(c + 1) * 128]), og[:, c * 128:(c + 1) * 128], C["idb"][:])
    s.cp("act", oTs[:, 0:nchunk, :], PSt.tile.v(ptb.ap[:, 0:nchunk * 128].rearrange("p (c t) -> p c t", c=nchunk)))
    s.dma("sp", D["oT"].v(D["oT"].t[col0:col0 + ncol, tok0:tok0 + 128].rearrange("(c p) t -> p c t", p=128)), oTs[:, 0:nchunk, :])


def out_work(s, maxcol):
    Wk = {"oi": 0}
    Wk["osb"] = [s.sb(f"osb{i}", [128, maxcol], F32) for i in range(2)]
    Wk["ofl"] = s.sb("ofl", [128, maxcol], F32)
    Wk["og"] = s.sb("og", [128, maxcol], BF16)
    Wk["sq"] = s.sb("sq", [128, 512], F32)
    Wk["ss"] = s.sb("ss", [128, 8], F32)
    Wk["rs"] = s.sb("rs", [128, 8], F32)
    Wk["oTs"] = [s.sb(f"oTs{i}", [128, maxcol // 128, 128], BF16) for i in range(2)]
    return Wk


def hg_mixer(s, C, D, L, PS):
    nc = s.nc
    nh, V = 2, 128
    win = s.sb("win", [128, 8, 1280], BF16)
    s.dma("pool", win[:], D["w_in"].v(D["w_in"].t.rearrange("(c p) n -> p c n", p=128)))
    def lb_from_logits(src_view, n, nm):
        lgt = s.sb(nm + "lg", [128, 4, n], F32)
        s.dma("sp", lgt[:], src_view)
        s.act(lgt[:], lgt[:], AF.Exp)
        num = s.sb(nm + "num", [128, n], F32)
        den = s.sb(nm + "den", [128, n], F32)
        s.tt("dve", num[:], lgt[:, 1, :], lgt[:, 2, :], ALU.add)
        s.tt("dve", den[:], lgt[:, 0, :], lgt[:, 3, :], ALU.add)
        s.tt("dve", den[:], den[:], num[:], ALU.add)
        s.op("dve", lambda: nc.vector.reciprocal(out=den.t[:], in_=den.t[:]), [den[:]], [den[:]])
        s.tt("dve", num[:], num[:], den[:], ALU.mult)
        return num
    lbB = lb_from_logits(D["lbrow"].v(D["lbrow"].t[0:1, :, :].partition_broadcast(128)), 256, "lbr")
    omlB = s.sb("omlB", [128, 256], F32)
    s.ts("dve", omlB[:], lbB[:], -1.0, ALU.mult, 1.0, ALU.add)
    lbc = lb_from_logits(D["lbcol"][:, :, :], 2, "lbc")
    omlc = s.sb("omlc", [128, 2], F32)
    nomlc = s.sb("nomlc", [128, 2], F32)
    s.ts("dve", omlc[:], lbc[:], -1.0, ALU.mult, 1.0, ALU.add)
    s.ts("dve", nomlc[:], omlc[:], -1.0, ALU.mult)
    normB = s.sb("normB", [128, 2, 128], F32)
    for h in range(2):
        s.dma("sp", normB[:, h, :], D["normw"].v(D["normw"].t[0:1, :].partition_broadcast(128)))
    xTb = [s.sb(f"xTb{i}", [128, 8, 512], BF16) for i in range(2)]
    qTs = [s.sb(f"qTs{h}", [128, 512], F32) for h in range(2)]
    kTs = [s.sb(f"kTs{h}", [128, 512], F32) for h in range(2)]
    sig = s.sb("sig", [128, 256], F32)
    t1 = s.sb("t1", [128, 256], F32)
    t2 = s.sb("t2", [128, 256], F32)
    ktm = s.sb("ktm", [128, 256], F32)
    gtm = s.sb("gtm", [128, 256], F32)
    vtm = s.sb("vtm", [128, 256], BF16)
    sgate = s.sb("sgate", [128, 256], F32)
    W = formA_work(s, V)
    Wk = out_work(s, 256)
    NBLK = L // 512
    for d in range(2):
        St = []
        for h in range(nh):
            S = s.sb(f"S{d}{h}", [128, V], F32)
            Sb = [s.sb(f"Sb{d}{h}{i}", [128, V], BF16) for i in range(2)]
            s.memset("dve", S[:], 0.0)
            s.memset("dve", Sb[0][:], 0.0)
            St.append({"S": S, "Sb": Sb, "i": 0})
        fcol = 256 + 256 * d
        blocks = range(NBLK) if d == 0 else range(NBLK - 1, -1, -1)
        for bi, blk in enumerate(blocks):
            xb = xTb[bi % 2]
            s.dma("sp", xb[:], xT_view(D, blk * 512, (blk + 1) * 512))
            for h in range(nh):
                for dc in range(8):
                    s.mm(PS[0][:, :], win[:, dc, h * 128:(h + 1) * 128], xb[:, dc, :], start=(dc == 0), stop=(dc == 7))
                s.act(qTs[h][:], PS[0][:, :], AF.Silu)
                for dc in range(8):
                    s.mm(PS[1][:, :], win[:, dc, fcol + h * 128:fcol + (h + 1) * 128], xb[:, dc, :], start=(dc == 0), stop=(dc == 7))
                s.act(kTs[h][:], PS[1][:, :], AF.Sigmoid)
                s.ts("dve", kTs[h][:], kTs[h][:], nomlc[:, h:h + 1], ALU.mult, omlc[:, h:h + 1], ALU.add)
            tiles = range(4) if d == 0 else range(3, -1, -1)
            for tl in tiles:
                tsl = slice(tl * 128, (tl + 1) * 128)
                tok0 = blk * 512 + tl * 128
                for dc in range(8):
                    s.mm(PS[2][:, 0:256], xb[:, dc, tsl], win[:, dc, fcol:fcol + 256], start=(dc == 0), stop=(dc == 7))
                for dc in range(8):
                    s.mm(PS[2][:, 256:512], xb[:, dc, tsl], win[:, dc, 768:1024], start=(dc == 0), stop=(dc == 7))
                s.act(sig[:], PS[2][:, 0:256], AF.Sigmoid)
                s.cp("act", vtm[:], PS[2][:, 256:512])
                s.tt("dve", t1[:], sig[:], omlB[:], ALU.mult)
                s.tt("pool", ktm[:], omlB[:], t1[:], ALU.subtract)
                s.tt("pool", t2[:], t1[:], lbB[:], ALU.add)
                s.act(gtm[:], t2[:], AF.Ln)
                if d == 1:
                    for dc in range(8):
                        s.mm(PS[3][:, 0:256], xb[:, dc, tsl], win[:, dc, 1024:1280], start=(dc == 0), stop=(dc == 7))
                    s.act(sgate[:], PS[3][:, 0:256], AF.Silu)
                for h in range(nh):
                    hs = slice(h * 128, (h + 1) * 128)
                    formA_tile(s, C, d, PS[4][:, h * 256:(h + 1) * 256], PS[5][:, h * 256:(h + 1) * 256], PS[6][:, h * V:(h + 1) * V],
                               qTs[h][:, tsl], kTs[h][:, tsl], ktm[:, hs], gtm[:, hs], vtm[:, hs], V, St[h], W)
                out_stage(s, C, d, PS[6][:, 0:256], PS[7][:, :], D, tok0, 256, nh, V, sgate[:],
                          normB.v(normB.t[:, :, :].rearrange("p h v -> p (h v)")), Wk)


def gla_mixer(s, C, D, L, PS):
    nc = s.nc
    nh, V = 1, 256
    win = s.sb("win", [128, 8, 800], BF16)
    s.dma("pool", win[:], D["w_in"].v(D["w_in"].t.rearrange("(c p) n -> p c n", p=128)))
    wgk = s.sb("wgk", [16, 2, 128], F32)
    s.dma("sp", wgk[:], D["wgk"].v(D["wgk"].t.rearrange("z r k -> r z k")))
    bgkB = s.sb("bgkB", [128, 2, 128], F32)
    s.dma("sp", bgkB[:], D["bgk"].v(D["bgk"].t.rearrange("(o z) k -> o z k", o=1).partition_broadcast(128)))
    normB = s.sb("normB", [128, 256], F32)
    s.dma("sp", normB[:], D["normw"].v(D["normw"].t[0:1, :].partition_broadcast(128)))
    xTb = [s.sb(f"xTb{i}", [128, 8, 512], BF16) for i in range(2)]
    qTs = s.sb("qTs", [128, 512], F32)
    kTs = s.sb("kTs", [128, 512], F32)
    rTs = s.sb("rTs", [16, 512], F32)
    z = s.sb("z", [128, 128], F32)
    ktm = s.sb("ktm", [128, 128], F32)
    gtm = s.sb("gtm", [128, 128], F32)
    vtm = s.sb("vtm", [128, 256], BF16)
    sgate = s.sb("sgate", [128, 256], F32)
    W = formA_work(s, V)
    Wk = out_work(s, 256)
    NBLK = L // 512
    for d in range(2):
        S = s.sb(f"S{d}", [128, V], F32)
        Sb = [s.sb(f"Sb{d}{i}", [128, V], BF16) for i in range(2)]
        s.memset("dve", S[:], 0.0)
        s.memset("dve", Sb[0][:], 0.0)
        St = {"S": S, "Sb": Sb, "i": 0}
        rcol = 768 + 16 * d
        blocks = range(NBLK) if d == 0 else range(NBLK - 1, -1, -1)
        for bi, blk in enumerate(blocks):
            xb = xTb[bi % 2]
            s.dma("sp", xb[:], xT_view(D, blk * 512, (blk + 1) * 512))
            for dc in range(8):
                s.mm(PS[0][:, :], win[:, dc, 0:128], xb[:, dc, :], start=(dc == 0), stop=(dc == 7))
            s.act(qTs[:], PS[0][:, :], AF.Copy, scale=float(128 ** -0.5))
            for dc in range(8):
                s.mm(PS[1][:, :], win[:, dc, 128:256], xb[:, dc, :], start=(dc == 0), stop=(dc == 7))
            s.cp("act", kTs[:], PS[1][:, :])
            for dc in range(8):
                s.mm(PS[3][0:16, :], win[:, dc, rcol:rcol + 16], xb[:, dc, :], start=(dc == 0), stop=(dc == 7))
            s.cp("act", rTs[:], PS[3][0:16, :])
            tiles = range(4) if d == 0 else range(3, -1, -1)
            for tl in tiles:
                tsl = slice(tl * 128, (tl + 1) * 128)
                tok0 = blk * 512 + tl * 128
                for dc in range(8):
                    s.mm(PS[2][:, 0:384], xb[:, dc, tsl], win[:, dc, 128:512], start=(dc == 0), stop=(dc == 7))
                s.cp("act", ktm[:], PS[2][:, 0:128])
                s.cp("act", vtm[:], PS[2][:, 128:384])
                s.mm(PS[2][:, 384:512], rTs[:, tsl], wgk[:, d, :])
                s.tt("dve", z[:], PS[2][:, 384:512], bgkB[:, d, :], ALU.add)
                s.act(z[:], z[:], AF.Exp, scale=-1.0)
                s.act(z[:], z[:], AF.Ln, bias=1.0)
                s.ts("dve", gtm[:], z[:], -1.0 / 16.0, ALU.mult)
                if d == 1:
                    for dc in range(8):
                        s.mm(PS[3][:, 0:256], xb[:, dc, tsl], win[:, dc, 512:768], start=(dc == 0), stop=(dc == 7))
                    s.act(sgate[:], PS[3][:, 0:256], AF.Silu)
                formA_tile(s, C, d, PS[4][:, 0:256], PS[5][:, 0:384], PS[6][:, 0:256],
                           qTs[:, tsl], kTs[:, tsl], ktm[:], gtm[:], vtm[:], V, St, W)
                out_stage(s, C, d, PS[6][:, 0:256], PS[7][:, :], D, tok0, 256, nh, V, sgate[:], normB[:], Wk)

MASKBIG = 30000.0


def make_masks_b(s, C):
    C["maskneg"] = []
    for d in range(2):
        m = s.sb(f"maskneg{d}", [128, 128], F32)
        s.ts("pool", m[:], C["U128"][d][:], -1.0, ALU.add, MASKBIG, ALU.mult)
        C["maskneg"].append(m)
    C["maskstrict"] = []
    for d in range(2):
        m = s.sb(f"maskstr{d}", [128, 128], F32)
        s.ts("pool", m[:], C["SU128"][d][:], -1.0, ALU.add, -MASKBIG, ALU.mult)
        C["maskstrict"].append(m)


def load_block_halo(s, xb, D, blk, NBLK, TB=512):
    lo, hi = blk * TB - 2, blk * TB + TB + 2
    a, b = 0, TB + 4
    if blk == 0:
        s.memset("pool", xb[:, :, 0:2], 0.0)
        lo, a = 0, 2
    if blk == NBLK - 1:
        s.memset("pool", xb[:, :, TB + 2:TB + 4], 0.0)
        hi, b = blk * TB + TB, TB + 2
    cur = lo
    while cur < hi:
        nxt = min(hi, (cur // 4096 + 1) * 4096)
        s.dma("sp", xb[:, :, a + (cur - lo):a + (nxt - lo)], xT_view(D, cur, nxt))
        cur = nxt


def conv_chunk(s, PSa, PSb, win, col0, xb, pre, acc, cw, cc, out, bias=None):
    for half, P in ((0, PSa), (1, PSb)):
        for dc in range(8):
            s.mm(P[:, 0:258], win[:, dc, col0:col0 + 128], xb[:, dc, half * 258:(half + 1) * 258], start=(dc == 0), stop=(dc == 7))
        s.cp("act", pre[:, half * 258:(half + 1) * 258], P[:, 0:258])
    s.ts("dve", acc[:], pre[:, 0:512], cw[:, cc, 0:1], ALU.mult)
    for k in range(1, 5):
        s.stt("dve", acc[:], pre[:, k:k + 512], cw[:, cc, k:k + 1], acc[:], ALU.mult, ALU.add)
    if bias is None:
        s.act(out, acc[:], AF.Silu)
    else:
        s.act(out, acc[:], AF.Silu, bias=bias)


def ssd_mixer(s, C, D, L, PS):
    nc = s.nc
    win = s.sb("win", [128, 8, 1296], BF16)
    s.dma("pool", win[:], D["w_in"].v(D["w_in"].t.rearrange("(c p) n -> p c n", p=128)))
    cw = s.sb("cw", [128, 6, 5], F32)
    s.dma("sp", cw[:], D["convw"][:, :, :])
    cbias = s.sb("cbias", [128, 6], F32)
    s.dma("sp", cbias[:], D["convb"][:, :])
    negA = s.sb("negA", [128, 2, 8], F32)
    s.dma("sp", negA[:], D["alog"].v(D["alog"].t.rearrange("(o z) r -> o z r", o=1).partition_broadcast(128)))
    s.act(negA[:], negA[:], AF.Exp)
    s.ts("dve", negA[:], negA[:], -1.0, ALU.mult)
    dtbB = s.sb("dtbB", [128, 2, 8], F32)
    s.dma("sp", dtbB[:], D["dtb"].v(D["dtb"].t.rearrange("(o z) r -> o z r", o=1).partition_broadcast(128)))
    dskB = s.sb("dskB", [128, 8], F32)
    s.dma("sp", dskB[:], D["dsk"].v(D["dsk"].t[0:1, :].partition_broadcast(128)))
    normB = s.sb("normB", [128, 512], F32)
    s.dma("sp", normB[:], D["normw"].v(D["normw"].t[0:1, :].partition_broadcast(128)))
    xTb = [s.sb(f"xTb{i}", [128, 8, 516], BF16) for i in range(2)]
    pre = s.sb("pre", [128, 516], F32)
    acc = s.sb("cacc", [128, 512], F32)
    xsT = [s.sb(f"xsT{c}", [128, 512], F32) for c in range(4)]
    BT = s.sb("BT", [128, 512], BF16)
    CT = s.sb("CT", [128, 512], BF16)
    dt = s.sb("dt", [128, 8], F32)
    g = s.sb("g", [128, 8], F32)
    gB = s.sb("gB", [128, 8, 128], F32)
    ones3 = s.sb("ones3", [128, 8, 128], F32)
    s.memset("pool", ones3[:], 1.0)
    E24 = s.sb("E24", [128, 24], F32)
    ngc = s.sb("ngc", [128, 8], F32)
    xs_tm = s.sb("xs_tm", [128, 8, 64], F32)
    u = s.sb("u", [128, 8, 64], F32)
    u_bf = s.sb("u_bf", [128, 512], BF16)
    udec = s.sb("udec", [128, 8, 64], BF16)
    B_tm = s.sb("B_tm", [128, 128], BF16)
    Er = [s.sb(f"Er{i}", [128, 128], F32) for i in range(2)]
    STr = [s.sb(f"STr{i}", [128, 128], BF16) for i in range(2)]
    tmpB = s.sb("tmpB", [128, 8, 64], F32)
    ysb = s.sb("ysb", [128, 512], F32)
    skip = s.sb("skip", [128, 8, 64], F32)
    sgate = s.sb("sgate", [128, 512], F32)
    Wk = out_work(s, 512)
    NBLK = L // 512
    idf = C["idf"]
    for d in range(2):
        S = s.sb(f"S{d}", [128, 8, 64], F32)
        Sb = [s.sb(f"Sb{d}{i}", [128, 512], BF16) for i in range(2)]
        s.memset("dve", S[:], 0.0)
        s.memset("dve", Sb[0][:], 0.0)
        si = 0
        U, SU, mneg = C["U128"][d], C["SU128"][d], C["maskneg"][d]
        dtcol = 1280 + 8 * d
        blocks = range(NBLK) if d == 0 else range(NBLK - 1, -1, -1)
        for bi, blk in enumerate(blocks):
            xb = xTb[bi % 2]
            load_block_halo(s, xb, D, blk, NBLK)
            for cc in range(6):
                out = xsT[cc][:] if cc < 4 else (BT[:] if cc == 4 else CT[:])
                conv_chunk(s, PS[0], PS[1], win, cc * 128, xb, pre, acc, cw, cc, out, bias=cbias[:, cc:cc + 1])
            tiles = range(4) if d == 0 else range(3, -1, -1)
            for tl in tiles:
                tsl = slice(tl * 128, (tl + 1) * 128)
                xsl = slice(2 + tl * 128, 2 + (tl + 1) * 128)
                tok0 = blk * 512 + tl * 128
                for dc in range(8):
                    s.mm(PS[3][:, 0:8], xb[:, dc, xsl], win[:, dc, dtcol:dtcol + 8], start=(dc == 0), stop=(dc == 7))
                s.tt("dve", dt[:], PS[3][:, 0:8], dtbB[:, d, :], ALU.add)
                s.act(dt[:], dt[:], AF.Exp)
                s.act(dt[:], dt[:], AF.Ln, bias=1.0)
                s.tt("dve", g[:], dt[:], negA[:, d, :], ALU.mult)
                s.tt("pool", gB[:], ones3[:], g.v(g.t[:, :].unsqueeze(2).to_broadcast([128, 8, 128])), ALU.mult)
                if d == 1:
                    for dc in range(8):
                        s.mm(PS[2][:, :], xb[:, dc, xsl], win[:, dc, 768:1280], start=(dc == 0), stop=(dc == 7))
                    s.act(sgate[:], PS[2][:, :], AF.Silu)
                for c in range(4):
                    s.tr(PS[4][:, c * 128:(c + 1) * 128], xsT[c][:, tsl], idf[:])
                s.cp("act", xs_tm.v(xs_tm.t[:, :, :].rearrange("p r q -> p (r q)")), PS[4][:, :])
                p5b = PS[5].v(PS[5].t[:, :].bitcast(BF16))
                s.tr(PS[5].v(p5b.ap[:, 768:896]), BT[:, tsl], C["idb"][:])
                s.cp("act", B_tm[:], PS[5].v(p5b.ap[:, 768:896]))
                dtb_ = dt.v(dt.t[:, :].unsqueeze(2).to_broadcast([128, 8, 64]))
                s.tt("dve", u[:], xs_tm[:], dtb_, ALU.mult)
                s.cp("pool", u_bf.v(u_bf.t[:, :].rearrange("p (r q) -> p r q", r=8)), u[:])
                s.mm(PS[3][:, 32:40], U[:], g[:])
                s.mm(PS[3][:, 40:48], SU[:], g[:])
                s.mm(PS[3][:, 48:56], C["ones"][:], g[:])
                s.act(E24[:], PS[3][:, 32:56], AF.Exp)
                s.act(ngc[:], PS[3][:, 32:40], AF.Copy, scale=-1.0)
                s.mm(PS[5][:, 0:128], BT[:, tsl], CT[:, tsl])
                for r in range(8):
                    Pr = PS[1 + (r % 2)][:, 0:128]
                    s.mm(Pr, gB[:, r, :], U[:], start=True, stop=False)
                    s.mm(Pr, idf[:], mneg[:], start=False, stop=True)
                    s.act(Er[r % 2][:], Pr, AF.Exp, bias=ngc[:, r:r + 1])
                    s.tt("dve", STr[r % 2][:], PS[5][:, 0:128], Er[r % 2][:], ALU.mult)
                    s.mm(PS[6][:, r * 64:(r + 1) * 64], STr[r % 2][:], u_bf[:, r * 64:(r + 1) * 64])
                cur = Sb[si % 2]
                s.mm(PS[7][:, :], CT[:, tsl], cur[:])
                s.cp("act", tmpB.v(tmpB.t[:, :, :].rearrange("p r q -> p (r q)")), PS[7][:, :])
                egc_b = E24.v(E24.t[:, 0:8].unsqueeze(2).to_broadcast([128, 8, 64]))
                s.tt("dve", tmpB[:], tmpB[:], egc_b, ALU.mult)
                s.tt("dve", ysb[:], tmpB.v(tmpB.t[:, :, :].rearrange("p r q -> p (r q)")), PS[6][:, :], ALU.add)
                edec_b = E24.v(E24.t[:, 8:16].unsqueeze(2).to_broadcast([128, 8, 64]))
                s.tt("pool", udec[:], u[:], edec_b, ALU.mult)
                s.mm(PS[0][:, :], B_tm[:], udec.v(udec.t[:, :, :].rearrange("p r q -> p (r q)")))
                egl_b = E24.v(E24.t[:, 16:24].unsqueeze(2).to_broadcast([128, 8, 64]))
                s.tt("dve", S[:], S[:], egl_b, ALU.mult)
                s.tt("dve", S.v(S.t[:, :, :].rearrange("p r q -> p (r q)")), S.v(S.t[:, :, :].rearrange("p r q -> p (r q)")), PS[0][:, :], ALU.add)
                si += 1
                s.cp("act", Sb[si % 2][:], S.v(S.t[:, :, :].rearrange("p r q -> p (r q)")))
                if d == 1:
                    dsk_b = dskB.v(dskB.t[:, :].unsqueeze(2).to_broadcast([128, 8, 64]))
                    s.tt("pool", skip[:], xs_tm[:], dsk_b, ALU.mult)
                    s.tt("pool", ysb[:], ysb[:], skip.v(skip.t[:, :, :].rearrange("p r q -> p (r q)")), ALU.add)
                out_stage(s, C, d, ysb[:], PS[1][:, :], D, tok0, 512, 1, 512, sgate[:], normB[:], Wk, pre_gate=True)


class Reg:
    def __init__(self, tile, a, b):
        self.tile, self.a, self.b = tile, a, b
        self.t = tile.t[:, a:b]

    def __getitem__(self, k):
        return View(self.tile, self.t[k])

    def v(self, ap):
        return View(self.tile, ap)


def gdn_mixer(s, C, D, L, PS):
    nc = s.nc
    idf, idb = C["idf"], C["idb"]
    win = s.sb("win", [128, 8, 1552], BF16)
    s.dma("pool", win[:], D["w_in"].v(D["w_in"].t.rearrange("(c p) n -> p c n", p=128)))
    cw = s.sb("cw", [128, 8, 5], F32)
    s.dma("sp", cw[:], D["convw"][:, :, :])
    negA = s.sb("negA", [128, 2, 4], F32)
    s.dma("sp", negA[:], D["alog"].v(D["alog"].t.rearrange("(o z) r -> o z r", o=1).partition_broadcast(128)))
    s.act(negA[:], negA[:], AF.Exp)
    s.ts("dve", negA[:], negA[:], -1.0, ALU.mult)
    dtbB = s.sb("dtbB", [128, 2, 4], F32)
    s.dma("sp", dtbB[:], D["dtb"].v(D["dtb"].t.rearrange("(o z) r -> o z r", o=1).partition_broadcast(128)))
    normB = s.sb("normB", [128, 4, 128], F32)
    for h in range(4):
        s.dma("sp", normB[:, h, :], D["normw"].v(D["normw"].t[0:1, :].partition_broadcast(128)))
    xTb = [s.sb(f"xTb{i}", [128, 8, 516], BF16) for i in range(2)]
    pre = s.sb("pre", [128, 516], F32)
    acc = s.sb("cacc", [128, 512], F32)
    qT = [s.sb(f"qT{c}", [128, 512], F32) for c in range(2)]
    kT = [s.sb(f"kT{c}", [128, 512], F32) for c in range(2)]
    vT = [s.sb(f"vT{c}", [128, 512], F32) for c in range(4)]
    qTb = [s.sb(f"qTb{c}", [128, 512], BF16) for c in range(2)]
    kTb = [s.sb(f"kTb{c}", [128, 512], BF16) for c in range(2)]
    sqt = s.sb("sqt", [128, 512], F32)
    rn = s.sb("rn", [128, 512], F32)
    ab = s.sb("ab", [128, 8], F32)
    g = s.sb("g", [128, 4], F32)
    beta = s.sb("beta", [128, 4], F32)
    nbeta = s.sb("nbeta", [128, 4], F32)
    bg = s.sb("bg", [128, 4], F32)
    gB = s.sb("gB", [128, 4, 128], F32)
    ones3 = s.sb("ones3", [128, 4, 128], F32)
    s.memset("pool", ones3[:], 1.0)
    E12 = s.sb("E12", [128, 12], F32)
    ngc = s.sb("ngc", [128, 4], F32)
    gcs = s.sb("gcs", [128, 4], F32)
    k_tm = s.sb("k_tm", [128, 2, 128], F32)
    v_tm = s.sb("v_tm", [128, 4, 128], F32)
    sgate = s.sb("sgate", [128, 512], F32)
    osb_all = s.sb("osb_all", [128, 512], F32)
    def quad(name, shape, dt):
        return [s.sb(f"{name}{i}", shape, dt) for i in range(4)]
    Einc = quad("Einc", [128, 128], F32); En = quad("En", [128, 128], F32)
    attnT = quad("attnT", [128, 128], BF16)
    Mb = [quad(f"Mb{i}", [128, 128], BF16) for i in range(2)]
    MTb = [quad(f"MTb{i}", [128, 128], BF16) for i in range(2)]
    P32 = quad("P32", [128, 128], F32); Pbf = quad("Pbf", [128, 128], BF16)
    kbg = quad("kbg", [128, 128], BF16); vb = quad("vb", [128, 128], BF16); kdec = quad("kdec", [128, 128], BF16)
    u_sb = quad("u_sb", [128, 128], F32); wTb = quad("wTb", [128, 128], BF16)
    vnew = quad("vnew", [128, 128], BF16); oAs = quad("oAs", [128, 128], F32)
    def reg(bank, a, b, nm):
        return Reg(PS[bank], a, b)
    pG = [reg(5, 0, 128, "pG0"), reg(5, 128, 256, "pG1")]
    pQK = [reg(5, 256, 384, "pQK0"), reg(5, 384, 512, "pQK1")]
    HB = [6, 7, 0, 1]
    pA = [reg(HB[h], 0, 128, "pA") for h in range(4)]
    pB = [reg(HB[h], 128, 256, "pB") for h in range(4)]
    pC = [reg(HB[h], 256, 384, "pC") for h in range(4)]
    pD = [reg(HB[h], 384, 512, "pD") for h in range(4)]
    pP1 = [reg(2 + (h % 2), 0, 128, "pP1") for h in range(4)]
    pP2 = [reg(2 + (h % 2), 128, 256, "pP2") for h in range(4)]
    Wk = out_work(s, 512)
    NBLK = L // 512
    for d in range(2):
        S = [s.sb(f"S{d}{h}", [128, 128], F32) for h in range(4)]
        Sb = [[s.sb(f"Sb{d}{h}{i}", [128, 128], BF16) for i in range(2)] for h in range(4)]
        si = [0] * 4
        for h in range(4):
            s.memset("dve", S[h][:], 0.0)
            s.memset("dve", Sb[h][0][:], 0.0)
        U, SU, mneg, mstr = C["U128"][d], C["SU128"][d], C["maskneg"][d], C["maskstrict"][d]
        abcol = 1536 + 8 * d
        blocks = range(NBLK) if d == 0 else range(NBLK - 1, -1, -1)
        for bi, blk in enumerate(blocks):
            xb = xTb[bi % 2]
            load_block_halo(s, xb, D, blk, NBLK)
            for cc in range(8):
                out = (qT[cc] if cc < 2 else kT[cc - 2] if cc < 4 else vT[cc - 4])[:]
                conv_chunk(s, PS[2], PS[3], win, cc * 128, xb, pre, acc, cw, cc, out)
            for (src, dstb, scl) in ((qT[0], qTb[0], float(128 ** -0.5)), (qT[1], qTb[1], float(128 ** -0.5)), (kT[0], kTb[0], 1.0), (kT[1], kTb[1], 1.0)):
                s.act(sqt[:], src[:], AF.Square)
                s.mm(PS[2][:, :], C["ones"][:], sqt[:])
                s.act(rn[:], PS[2][:, :], AF.Ln, bias=NORM_EPS)
                s.act(rn[:], rn[:], AF.Exp, scale=-0.5)
                s.stt("dve", src[:], src[:], scl, rn[:], ALU.mult, ALU.mult)
                s.cp("act", dstb[:], src[:])
            tiles = range(4) if d == 0 else range(3, -1, -1)
            for tl in tiles:
                tsl = slice(tl * 128, (tl + 1) * 128)
                xsl = slice(2 + tl * 128, 2 + (tl + 1) * 128)
                tok0 = blk * 512 + tl * 128
                for dc in range(8):
                    s.mm(PS[3][:, 0:8], xb[:, dc, xsl], win[:, dc, abcol:abcol + 8], start=(dc == 0), stop=(dc == 7))
                s.cp("act", ab[:], PS[3][:, 0:8])
                s.tt("dve", g[:], ab[:, 0:4], dtbB[:, d, :], ALU.add)
                s.act(g[:], g[:], AF.Exp)
                s.act(g[:], g[:], AF.Ln, bias=1.0)
                s.tt("dve", g[:], g[:], negA[:, d, :], ALU.mult)
                s.act(beta[:], ab[:, 4:8], AF.Exp, scale=-1.0)
                s.ts("dve", beta[:], beta[:], 1.0, ALU.add)
                s.op("dve", lambda: nc.vector.reciprocal(out=beta.t[:], in_=beta.t[:]), [beta[:]], [beta[:]])
                s.ts("dve", nbeta[:], beta[:], -1.0, ALU.mult)
                s.tt("pool", gB[:], ones3[:], g.v(g.t[:, :].unsqueeze(2).to_broadcast([128, 4, 128])), ALU.mult)
                if d == 1:
                    for dc in range(8):
                        s.mm(PS[2][:, :], xb[:, dc, xsl], win[:, dc, 1024:1536], start=(dc == 0), stop=(dc == 7))
                    s.act(sgate[:], PS[2][:, :], AF.Silu)
                for c in range(2):
                    s.tr(PS[3][:, 64 + c * 128:192 + c * 128], kT[c][:, tsl], idf[:])
                s.cp("act", k_tm.v(k_tm.t[:, :, :].rearrange("p c k -> p (c k)")), PS[3][:, 64:320])
                for c in range(4):
                    s.tr(PS[4][:, c * 128:(c + 1) * 128], vT[c][:, tsl], idf[:])
                s.cp("act", v_tm.v(v_tm.t[:, :, :].rearrange("p c k -> p (c k)")), PS[4][:, :])
                s.mm(PS[3][:, 32:36], U[:], g[:])
                s.mm(PS[3][:, 36:40], SU[:], g[:])
                s.mm(PS[3][:, 40:44], C["ones"][:], g[:])
                s.act(E12[:], PS[3][:, 32:44], AF.Exp)
                s.act(ngc[:], PS[3][:, 32:36], AF.Copy, scale=-1.0)
                s.act(gcs[:], PS[3][:, 32:36], AF.Copy)
                s.tt("dve", bg[:], beta[:], E12[:, 0:4], ALU.mult)
                for hq in range(2):
                    s.mm(pG[hq][:, :], kTb[hq][:, tsl], kTb[hq][:, tsl])
                    s.mm(pQK[hq][:, :], kTb[hq][:, tsl], qTb[hq][:, tsl])
                HQ = [0, 0, 1, 1]
                cM = [None] * 4
                cMT = [None] * 4
                for hv in range(4):
                    hq = HQ[hv]
                    s.mm(pP1[hv][:, :], gB[:, hv, :], U[:], start=True, stop=False)
                    s.mm(pP1[hv][:, :], idf[:], mneg[:], start=False, stop=True)
                    s.act(Einc[hv][:], pP1[hv][:, :], AF.Exp, bias=ngc[:, hv:hv + 1])
                    s.mm(pP2[hv][:, :], gB[:, hv, :], U[:], start=True, stop=False)
                    s.mm(pP2[hv][:, :], idf[:], mstr[:], start=False, stop=True)
                    s.act(En[hv][:], pP2[hv][:, :], AF.Exp, bias=gcs[:, hv:hv + 1], scale=-1.0)
                for hv in range(4):
                    hq = HQ[hv]
                    s.tt("dve", attnT[hv][:], pQK[hq][:, :], Einc[hv][:], ALU.mult)
                    s.stt("dve", Mb[0][hv][:], En[hv][:], nbeta[:, hv:hv + 1], pG[hq][:, :], ALU.mult, ALU.mult)
                    cM[hv], cMT[hv] = Mb[0][hv], MTb[0][hv]
                for hv in range(4):
                    ptb = pA[hv].v(pA[hv].t[:, :].bitcast(BF16))
                    s.tr(pA[hv].v(ptb.ap[:, 0:128]), cM[hv][:], idb[:])
                for hv in range(4):
                    ptb = pA[hv].v(pA[hv].t[:, :].bitcast(BF16))
                    s.cp("act", cMT[hv][:], pA[hv].v(ptb.ap[:, 0:128]))
                for hv in range(4):
                    s.tt("dve", P32[hv][:], idf[:], cMT[hv][:], ALU.add)
                    s.cp("pool", Pbf[hv][:], P32[hv][:])
                for l in range(1, 7):
                    for hv in range(4):
                        s.mm(pB[hv][:, :], cMT[hv][:], cM[hv][:])
                        if l < 6:
                            s.mm(pC[hv][:, :], cM[hv][:], cMT[hv][:])
                    for hv in range(4):
                        s.cp("act", Mb[l % 2][hv][:], pB[hv][:, :])
                        if l < 6:
                            s.cp("dve", MTb[l % 2][hv][:], pC[hv][:, :])
                        cM[hv], cMT[hv] = Mb[l % 2][hv], MTb[l % 2][hv]
                    for hv in range(4):
                        s.mm(pD[hv][:, :], cM[hv][:], Pbf[hv][:])
                    for hv in range(4):
                        s.tt("dve", P32[hv][:], P32[hv][:], pD[hv][:, :], ALU.add)
                        s.cp("pool", Pbf[hv][:], P32[hv][:])
                for hv in range(4):
                    hq = HQ[hv]
                    s.ts("pool", vb[hv][:], v_tm[:, hv, :], beta[:, hv:hv + 1], ALU.mult)
                    s.ts("pool", kbg[hv][:], k_tm[:, hq, :], bg[:, hv:hv + 1], ALU.mult)
                    s.ts("pool", kdec[hv][:], k_tm[:, hq, :], E12[:, 4 + hv:5 + hv], ALU.mult)
                for hv in range(4):
                    s.mm(pB[hv][:, :], Pbf[hv][:], vb[hv][:])
                    s.mm(pC[hv][:, :], kbg[hv][:], Pbf[hv][:])
                for hv in range(4):
                    s.cp("act", u_sb[hv][:], pB[hv][:, :])
                    s.cp("act", wTb[hv][:], pC[hv][:, :])
                for hv in range(4):
                    cur = Sb[hv][si[hv] % 2]
                    s.mm(pD[hv][:, :], wTb[hv][:], cur[:])
                    s.mm(pA[hv][:, :], qTb[HQ[hv]][:, tsl], cur[:])
                for hv in range(4):
                    s.tt("dve", vnew[hv][:], u_sb[hv][:], pD[hv][:, :], ALU.subtract)
                for hv in range(4):
                    s.mm(pB[hv][:, :], attnT[hv][:], vnew[hv][:])
                    s.mm(pC[hv][:, :], kdec[hv][:], vnew[hv][:])
                for hv in range(4):
                    s.cp("act", oAs[hv][:], pB[hv][:, :])
                for hv in range(4):
                    s.stt("dve", osb_all[:, hv * 128:(hv + 1) * 128], pA[hv][:, :], E12[:, hv:hv + 1], oAs[hv][:], ALU.mult, ALU.add)
                    s.stt("dve", S[hv][:], S[hv][:], E12[:, 8 + hv:9 + hv], pC[hv][:, :], ALU.mult, ALU.add)
                    si[hv] += 1
                for hv in range(4):
                    s.cp("act", Sb[hv][si[hv] % 2][:], S[hv][:])
                out_stage(s, C, d, osb_all[:], PS[4][:, :], D, tok0, 512, 4, 128, sgate[:],
                          normB.v(normB.t[:, :, :].rearrange("p h v -> p (h v)")), Wk)


def PS1_to_sb(s, P, scratch):
    return P[:, :]
import ml_dtypes
from concourse.bass_utils import run_bass_kernel_spmd

NCORE = 8
_DEBUG_HOOK = None
_PRE_BF = False
SEQ = 16384
NT = 4096
GT_TOK = 512
BF = ml_dtypes.bfloat16


def _run(build, in_maps, outs):
    nc = bass.Bass("TRN2", target_bir_lowering=False)
    s = Sched(nc)
    D = {}
    for k, v in in_maps[0].items():
        dt = BF16 if v.dtype == BF else F32
        D[k] = s.dram(nc.dram_tensor(k, list(v.shape), dt, kind="ExternalInput").ap(), k)
    for k, (shape, dt) in outs.items():
        D[k] = s.dram(nc.dram_tensor(k, list(shape), dt, kind="ExternalOutput").ap(), k)
    build(s, nc, D)
    s.finish([D[k] for k in outs])
    res = run_bass_kernel_spmd(nc, in_maps, core_ids=list(range(len(in_maps))))
    return res.results


def prep_phase(s, nc, D, C, PS, nt):
    idf = C["idf"]
    xt = [s.sb(f"pxt{i}", [128, 1024], F32) for i in range(2)]
    xoT = [s.sb(f"pxoT{i}", [128, 8, 128], BF16) for i in range(2)]
    for tl in range(nt // 128):
        tok0 = tl * 128
        xc = xt[tl % 2]
        s.dma("sp", xc[:], D["xres"][tok0:tok0 + 128, :])
        for dc in range(8):
            s.tr(PS[dc // 4][:, (dc % 4) * 128:(dc % 4 + 1) * 128], xc[:, dc * 128:(dc + 1) * 128], idf[:])
        xo = xoT[tl % 2]
        for hh in range(2):
            s.cp("act" if hh == 0 else "dve", xo[:, hh * 4:(hh + 1) * 4, :], PS[hh].v(PS[hh].t[:, :].rearrange("p (c t) -> p c t", c=4)))
        s.dma("sp", D["xTout"].v(D["xTout"].t[:, tok0:tok0 + 128].rearrange("(c p) t -> p c t", p=128)), xo[:])


def _c(a):
    return np.ascontiguousarray(a)


def _mixer_inputs(layer, inp, xT_seq):
    maps = []
    for c in range(NCORE):
        sq, q = c // 4, c % 4
        m = {"xT": xT_seq[sq]}
        if layer == 0:
            w = inp["gdn_w_in"][0]
            qc = np.arange(q * 256, (q + 1) * 256); kc = 1024 + qc
            vc = 2048 + np.arange(q * 512, (q + 1) * 512); zc = 4096 + np.arange(q * 512, (q + 1) * 512)
            hv = np.arange(q * 4, q * 4 + 4)
            abc = 6144 + np.concatenate([hv, 32 + hv, 16 + hv, 48 + hv])
            cols = np.concatenate([qc, kc, vc, zc, abc]); ccols = np.concatenate([qc, kc, vc])
            m.update({"w_in": _c(w[:, cols]),
                      "convw": _c(inp["gdn_conv_w"][0][:, ccols].reshape(5, 8, 128).transpose(2, 1, 0)),
                      "alog": _c(inp["gdn_a_log"][0][:, hv]), "dtb": _c(inp["gdn_dt_bias"][0][:, hv]),
                      "normw": _c(inp["gdn_norm_w"][0][None])})
        elif layer == 1:
            w = inp["m2_w_in"][0]; g_ = q; x0 = 2048
            cols = np.concatenate([np.arange(x0 + g_ * 512, x0 + (g_ + 1) * 512), np.arange(x0 + 2048 + g_ * 128, x0 + 2048 + (g_ + 1) * 128),
                                   np.arange(x0 + 2560 + g_ * 128, x0 + 2560 + (g_ + 1) * 128), np.arange(g_ * 512, (g_ + 1) * 512),
                                   np.arange(5120 + g_ * 8, 5120 + g_ * 8 + 8), np.arange(5152 + g_ * 8, 5152 + g_ * 8 + 8)])
            ccols = np.concatenate([np.arange(g_ * 512, (g_ + 1) * 512), np.arange(2048 + g_ * 128, 2048 + (g_ + 1) * 128),
                                    np.arange(2560 + g_ * 128, 2560 + (g_ + 1) * 128)])
            m.update({"w_in": _c(w[:, cols]),
                      "convw": _c(inp["m2_conv_w"][0][:, ccols].reshape(5, 6, 128).transpose(2, 1, 0)),
                      "convb": _c(inp["m2_conv_b"][0][ccols].reshape(6, 128).T),
                      "alog": _c(inp["m2_a_log"][0][:, g_ * 8:(g_ + 1) * 8]), "dtb": _c(inp["m2_dt_bias"][0][:, g_ * 8:(g_ + 1) * 8]),
                      "dsk": _c(inp["m2_d"][0][None, g_ * 8:(g_ + 1) * 8]), "normw": _c(inp["m2_norm_w"][0][None, g_ * 512:(g_ + 1) * 512])})
        elif layer == 2:
            w = inp["hg_w_in"][0]; cs = slice(q * 256, (q + 1) * 256)
            lg = inp["hg_lb_logits"][:, cs]
            m.update({"w_in": _c(np.concatenate([w[:, k * 1024:(k + 1) * 1024][:, cs] for k in range(5)], axis=1)),
                      "lbrow": _c(lg[None]), "lbcol": _c(lg.reshape(4, 2, 128).transpose(2, 0, 1)),
                      "normw": _c(inp["hg_norm_w"][0][None])})
        else:
            w = inp["gla_w_in"][0]; ks = slice(q * 128, (q + 1) * 128); vs = slice(q * 256, (q + 1) * 256)
            m.update({"w_in": _c(np.concatenate([w[:, 0:512][:, ks], w[:, 512:1024][:, ks], w[:, 1024:2048][:, vs],
                                                 w[:, 2048:3072][:, vs], w[:, 3072:3104]], axis=1)),
                      "wgk": _c(inp["gla_w_gk"][0][:, :, ks]), "bgk": _c(inp["gla_b_gk"][0][:, ks]),
                      "normw": _c(inp["gla_norm_w"][0][None])})
        maps.append(m)
    return maps


MIX_W = [2048, 2048, 1024, 1024]
MIX_WOUT = ["gdn_w_out", "m2_w_out", "hg_w_out", "gla_w_out"]


def _token_inputs(layer, inp, oT_tok, xres):
    maps = []
    p = inp["p"][layer].reshape(2 * SEQ, 256)
    wrt = _c(np.concatenate([inp["moe_w_group"][layer], inp["moe_w_router"][layer]], axis=1).reshape(8, 128, 36).transpose(1, 0, 2))
    brt = _c(np.concatenate([inp["moe_b_group"][layer], inp["moe_b_router"][layer]])[None])
    shared = {"w_out": _c(inp[MIX_WOUT[layer]][0]), "lng": _c(inp["ln_g"][layer]), "lnb": _c(inp["ln_b"][layer]),
              "wrt": wrt, "brt": brt,
              "w_gate": _c(inp["moe_w_gate"][layer].reshape(32, 1024, 256)), "w_up": _c(inp["moe_w_up"][layer].reshape(32, 1024, 256)),
              "w_down": _c(inp["moe_w_down"][layer].reshape(32, 256, 1024)),
              "pe_g": _c(inp["pe_w_gate"][layer]), "pe_p": _c(inp["pe_w_proj"][layer])}
    for c in range(NCORE):
        m = {"oT": oT_tok[c], "xres": xres[c], "pl": _c(p[c * NT:(c + 1) * NT])}
        m.update(shared)
        maps.append(m)
    return maps


def kernel_unfused(**inp):
    inp = {k: np.asarray(v) for k, v in inp.items()}
    x = inp["x"].reshape(2 * SEQ, 1024)
    xres = [_c(x[c * NT:(c + 1) * NT]) for c in range(NCORE)]

    def b_prep(s, nc, D):
        C = make_consts(s)
        PS = [s.ps(f"ps{i}", [128, 512], F32) for i in range(8)]
        prep_phase(s, nc, D, C, PS, NT)
    r = _run(b_prep, [{"xres": xres[c]} for c in range(NCORE)], {"xTout": ([1024, NT], BF16)})
    xT = [np.asarray(r[c]["xTout"]) for c in range(NCORE)]

    for layer in range(4):
        xT_seq = [_c(np.concatenate(xT[4 * sq:4 * sq + 4], axis=1)) for sq in range(2)]
        Wq = MIX_W[layer] // 4

        def b_mix(s, nc, D, layer=layer, Wq=Wq):
            C = make_consts(s)
            make_masks(s, C)
            PS = [s.ps(f"ps{i}", [128, 512], F32) for i in range(8)]
            D["of"] = s.dram(nc.dram_tensor("of", [SEQ, Wq], F32, kind="Internal").ap(), "of")
            if layer == 0:
                make_masks_b(s, C); gdn_mixer(s, C, D, SEQ, PS)
            elif layer == 1:
                make_masks_b(s, C); ssd_mixer(s, C, D, SEQ, PS)
            elif layer == 2:
                hg_mixer(s, C, D, SEQ, PS)
            else:
                gla_mixer(s, C, D, SEQ, PS)
        r = _run(b_mix, _mixer_inputs(layer, inp, xT_seq), {"oT": ([Wq, SEQ], BF16)})
        oT = [np.asarray(r[c]["oT"]) for c in range(NCORE)]
        oT_tok = []
        for c in range(NCORE):
            sq, q = c // 4, c % 4
            oT_tok.append(_c(np.concatenate([oT[4 * sq + qq][:, q * NT:(q + 1) * NT] for qq in range(4)], axis=0)))
        last = layer == 3

        def b_tok(s, nc, D, layer=layer, last=last):
            C = make_consts(s)
            D["x1res"] = s.dram(nc.dram_tensor("x1res", [NT, 1024], F32, kind="Internal").ap(), "x1res")
            if _PRE_BF:
                D["wbf_gate"] = s.dram(nc.dram_tensor("wbf_gate", [32, 1024, 256], BF16).ap(), "wbf_gate")
                D["wbf_up"] = s.dram(nc.dram_tensor("wbf_up", [32, 1024, 256], BF16).ap(), "wbf_up")
                D["wbf_down"] = s.dram(nc.dram_tensor("wbf_down", [32, 256, 1024], BF16).ap(), "wbf_down")
            token_phase(s, C, D, NT, GT_TOK, MIX_W[layer], last)
        outs = {"xout": ([NT, 1024], F32)}
        if not last:
            outs["xTout"] = ([1024, NT], BF16)
        r = _run(b_tok, _token_inputs(layer, inp, oT_tok, xres), outs)
        xres = [np.asarray(r[c]["xout"]) for c in range(NCORE)]
        if _DEBUG_HOOK is not None:
            _DEBUG_HOOK(layer, xres, oT)
        if not last:
            xT = [np.asarray(r[c]["xTout"]) for c in range(NCORE)]
    out = np.concatenate(xres, axis=0).reshape(2, SEQ, 1024).astype(np.float32)
    return out

GROUPS = [[0, 1, 2, 3], [4, 5, 6, 7]]


def zero_dram(s, dtile, zt):
    n = 1
    for d in dtile.t.shape:
        n *= d
    k = n // (128 * 8192)
    names = " ".join(f"d{i}" for i in range(len(dtile.t.shape)))
    flat = dtile.t.rearrange(f"{names} -> ({names})").rearrange("(k p n) -> k p n", p=128, n=8192)
    for i in range(k):
        s.dma("sp", dtile.v(flat[i]), zt[:, :])


def build_fused(s, nc, D, Lz):
    C = make_consts(s)
    make_masks(s, C)
    make_masks_b(s, C)
    PS = [s.ps(f"ps{i}", [128, 512], F32) for i in range(8)]
    rv = nc.sync.partition_id() % 4

    def dr(name, shape, dt):
        return s.dram(nc.dram_tensor(name, list(shape), dt).ap(), name)
    xTm = dr("xTm", [1024, NT], BF16)
    obx = dr("obx", [4, 1024, NT], BF16)
    x1res = dr("x1res", [NT, 1024], F32)
    xres_pp = [dr("xresA", [NT, 1024], F32), dr("xresB", [NT, 1024], F32)]
    wbf = [dr("wbf_gate", [32, 1024, 256], BF16), dr("wbf_up", [32, 1024, 256], BF16), dr("wbf_down", [32, 256, 1024], BF16)]
    CC_BYTES = 4 * 1024 * 1024

    def make_exch(name, R, N):
        Rc = CC_BYTES // (4 * N * 2)
        nch = R // Rc
        return {"Rc": Rc, "n": nch, "ib": [dr(f"{name}_ib{c}", [4, Rc, N], BF16) for c in range(nch)],
                "ob": [dr(f"{name}_ob{c}", [4, Rc, N], BF16) for c in range(nch)]}
    EX = {"x": make_exch("ex", 1024, NT), 512: make_exch("eo512", 512, SEQ), 256: make_exch("eo256", 256, SEQ)}
    EXT = {"ib": [dr(f"ext_ib{c}", [4, 1024, GT_TOK], BF16) for c in range(NT // GT_TOK)],
           "ob": [dr(f"ext_ob{c}", [4, 1024, GT_TOK], BF16) for c in range(NT // GT_TOK)]}
    mark = s.mark()
    s.start_phases()
    zt = s.sb("zt", [128, 8192], BF16)
    s.memset("pool", zt[:], 0.0)
    for e in list(EX.values()) + [EXT]:
        for t in e["ib"]:
            zero_dram(s, t, zt)
    s.reset_to(mark)

    def exchange(e, src):
        Rc = e["Rc"]
        for c in range(e["n"]):
            ib, ob = e["ib"][c], e["ob"][c]
            s.dma("sp", ib.v(ib.t[bass.ds(rv, 1), :, :]), src.v(src.t[c * Rc:(c + 1) * Rc, :].rearrange("(o r) n -> o r n", o=1)))
            s.allreduce(ob[:, :, :], ib[:, :, :], GROUPS)

    def exchange_x():
        e = EX["x"]
        exchange(e, xTm)
        for c in range(e["n"]):
            s.dma("sp", obx[:, c * e["Rc"]:(c + 1) * e["Rc"], :], e["ob"][c][:, :, :])

    prep_phase(s, nc, {"xres": D["xres"], "xTout": xTm}, C, PS, NT)
    exchange_x()
    if "dbg_obx" in D:
        s.dma("sp", D["dbg_obx"][:, :, :], obx[:, :, :])
    s.reset_to(mark)
    cur_x = D["xres"]
    for layer in range(4):
        W = MIX_W[layer]
        Wq = W // 4
        last = layer == 3
        oTloc = dr(f"oTloc{layer}", [Wq, SEQ], BF16)
        of = dr(f"of{layer}", [SEQ, Wq], F32)
        oTtok = dr(f"oTtok{layer}", [W, NT], BF16)
        Dm = {k[len(f"M{layer}_"):]: v for k, v in D.items() if k.startswith(f"M{layer}_")}
        Dm.update({"xT": obx, "of": of, "oT": oTloc})
        [gdn_mixer, ssd_mixer, hg_mixer, gla_mixer][layer](s, C, Dm, SEQ, PS)
        e = EX[Wq]
        exchange(e, oTloc)
        ot3 = oTtok.t.rearrange("(q w) t -> q w t", q=4)
        for c in range(e["n"]):
            s.dma("sp", oTtok.v(ot3[:, c * e["Rc"]:(c + 1) * e["Rc"], :]), e["ob"][c].v(e["ob"][c].t[:, :, bass.ds(rv * NT, NT)]))
        if layer == 1 and "dbg_oTtok0" in D:
            s.dma("sp", D["dbg_oTtok0"][:, :], oTtok[:, :])
            s.dma("sp", D["dbg_oTloc0"][:, :], oTloc[:, :])
        s.reset_to(mark)
        Dt = {k[len(f"T{layer}_"):]: v for k, v in D.items() if k.startswith(f"T{layer}_")}
        nxt = D["out"] if last else xres_pp[layer % 2]
        Dt.update({"oT": oTtok, "xres": cur_x, "x1res": x1res, "xout": nxt, "xTout": xTm})
        def after_group(g):
            ib, ob = EXT["ib"][g], EXT["ob"][g]
            ts_ = slice(g * GT_TOK, (g + 1) * GT_TOK)
            s.dma("sp", ib.v(ib.t[bass.ds(rv, 1), :, :]), xTm.v(xTm.t[:, ts_].rearrange("(o r) n -> o r n", o=1)))
            s.allreduce(ob[:, :, :], ib[:, :, :], GROUPS)
            s.dma("sp", obx[:, :, ts_], ob[:, :, :])
        token_phase(s, C, Dt, NT, GT_TOK, W, last, PS=PS)
        cur_x = nxt
        if not last:
            exchange_x()
        if layer == 1 and "dbg_x0" in D:
            s.dma("sp", D["dbg_x0"][:, :], nxt[:, :])
        s.reset_to(mark)


def kernel(**inp):
    inp = {k: np.asarray(v) for k, v in inp.items()}
    x = inp["x"].reshape(2 * SEQ, 1024)
    maps = [{"xres": _c(x[c * NT:(c + 1) * NT])} for c in range(NCORE)]
    for layer in range(4):
        mm = _mixer_inputs(layer, inp, [None, None])
        tm = _token_inputs(layer, inp, [None] * NCORE, [None] * NCORE)
        for c in range(NCORE):
            for k, v in mm[c].items():
                if k != "xT":
                    maps[c][f"M{layer}_{k}"] = v
            for k, v in tm[c].items():
                if k not in ("oT", "xres"):
                    maps[c][f"T{layer}_{k}"] = v

    def b(s, nc, D):
        build_fused(s, nc, D, None)
    outs = {"out": ([NT, 1024], F32)}
    if _DEBUG_HOOK is not None:
        outs.update({"dbg_obx": ([4, 1024, NT], BF16), "dbg_oTtok0": ([2048, NT], BF16), "dbg_oTloc0": ([512, SEQ], BF16), "dbg_x0": ([NT, 1024], F32)})
    r = _run(b, maps, outs)
    if _DEBUG_HOOK is not None:
        _DEBUG_HOOK(r)
    out = np.concatenate([np.asarray(r[c]["out"]) for c in range(NCORE)], axis=0).reshape(2, SEQ, 1024).astype(np.float32)
    return out
```
